# Optimizing a Trainium2 kernel written in Bass

```python
import math
import jax, jax.numpy as jnp
from jax import lax
import numpy as np

D_MODEL = 1024
BATCH = 16
SEQ = 2048
DEPTH = 4

GRID_W = 64
CTX_LEN = 256
HEAD_DIM = 64
A_HEADS = 4
A_KV_HEADS = 2
Q_BLOCK = 128
ROPE_THETA = 10000.0
B_HEADS = 4
WIN_R = 8
WIN_C = 16
C_HEADS = 4
C_CONV = 3
D_HEADS = 4
D_KDIM = 32
D_GATE_RANK = 16
GLA_TAU = 16.0
CHUNK = 64
N_BRANCH = 4
N_GROUPS = 4
EXP_PER_GROUP = 8
N_EXPERTS = N_GROUPS * EXP_PER_GROUP
EXP_HIDDEN = 512
TOP_K = 2
EPS = 1e-6
DN_ALPHA = (2.0 * DEPTH) ** 0.25
DN_BETA = (8.0 * DEPTH) ** -0.25

A_QW = A_HEADS * HEAD_DIM
A_KVW = A_KV_HEADS * HEAD_DIM
B_W = B_HEADS * HEAD_DIM
C_W = C_HEADS * HEAD_DIM
D_KW = D_HEADS * D_KDIM
D_VW = D_HEADS * HEAD_DIM
IN_SPLITS = (
    ('a_q', A_QW), ('a_k', A_KVW), ('a_v', A_KVW),
    ('b_q', B_W), ('b_k', B_W), ('b_v', B_W),
    ('c_qkv', 3 * C_W), ('c_beta', 2 * C_HEADS), ('c_a', 2 * C_HEADS), ('c_g', C_W),
    ('d_q', D_KW), ('d_k', D_KW), ('d_v', D_VW), ('d_lr', 2 * D_GATE_RANK), ('d_g', D_VW),
    ('gates', N_BRANCH * D_MODEL),
)
IN_W = sum(w for _, w in IN_SPLITS)

kernel_name = 'hybrid_latent_diffusion_block'

F32 = jnp.float32


def split_in(proj):
    offs = np.cumsum([w for _, w in IN_SPLITS])[:-1].tolist()
    return dict(zip([n for n, _ in IN_SPLITS], jnp.split(proj, offs, axis=-1)))


def split_heads(x, h):
    b, n, _ = x.shape
    return x.reshape(b, n, h, -1).transpose(0, 2, 1, 3)


def merge_heads(x):
    b, h, n, d = x.shape
    return x.transpose(0, 2, 1, 3).reshape(b, n, h * d)


def rms_norm(x, g):
    xf = x.astype(F32)
    y = xf * lax.rsqrt(jnp.mean(xf * xf, axis=-1, keepdims=True) + EPS)
    return (y * g.astype(F32)).astype(x.dtype)


def layer_norm(x, g, b):
    xf = x.astype(F32)
    mu = jnp.mean(xf, axis=-1, keepdims=True)
    var = jnp.mean(jnp.square(xf - mu), axis=-1, keepdims=True)
    return ((xf - mu) * lax.rsqrt(var + EPS) * g.astype(F32) + b.astype(F32)).astype(x.dtype)


def l2_normalize(x):
    return x * lax.rsqrt(jnp.sum(x * x, axis=-1, keepdims=True) + EPS)


def axial_rope_tables(n, dtype):
    t = jnp.arange(n, dtype=jnp.int32)
    row = (t // GRID_W).astype(F32)
    col = (t % GRID_W).astype(F32)
    nf = HEAD_DIM // 4
    inv = ROPE_THETA ** (-jnp.arange(nf, dtype=F32) / nf)
    ang_r = row[:, None] * inv
    ang_c = col[:, None] * inv
    return tuple(a.astype(dtype) for a in (jnp.cos(ang_r), jnp.sin(ang_r), jnp.cos(ang_c), jnp.sin(ang_c)))


def _rope_half(x, cos, sin):
    x1, x2 = jnp.split(x, 2, axis=-1)
    return jnp.concatenate([x1 * cos - x2 * sin, x1 * sin + x2 * cos], axis=-1)


def axial_rope(x, rope):
    cos_r, sin_r, cos_c, sin_c = rope
    xr, xc = jnp.split(x, 2, axis=-1)
    return jnp.concatenate([_rope_half(xr, cos_r, sin_r), _rope_half(xc, cos_c, sin_c)], axis=-1)


def gqa_mixer(q, k, v, qc, kc, vc, q_gain, k_gain, rope):
    scale = HEAD_DIM ** -0.5
    rep = A_HEADS // A_KV_HEADS
    q = axial_rope(rms_norm(split_heads(q, A_HEADS), q_gain), rope)
    k = axial_rope(rms_norm(split_heads(k, A_KV_HEADS), k_gain), rope)
    v = split_heads(v, A_KV_HEADS)
    qc = rms_norm(split_heads(qc, A_HEADS), q_gain)
    kc = rms_norm(split_heads(kc, A_KV_HEADS), k_gain)
    vc = split_heads(vc, A_KV_HEADS)
    b, _, s, _ = q.shape
    nc = qc.shape[2]
    k_all = jnp.concatenate([k, kc], axis=2)
    v_all = jnp.concatenate([v, vc], axis=2)
    nb = s // Q_BLOCK
    qb = jnp.moveaxis(q.reshape(b, A_KV_HEADS, rep, nb, Q_BLOCK, HEAD_DIM), 3, 0)

    def block(qi):
        sc = jnp.einsum('bgrqd,bgkd->bgrqk', qi, k_all).astype(F32) * scale
        p = jax.nn.softmax(sc, axis=-1).astype(v_all.dtype)
        return jnp.einsum('bgrqk,bgkd->bgrqd', p, v_all)

    o = lax.map(block, qb)
    o = jnp.moveaxis(o, 0, 3).reshape(b, A_HEADS, s, HEAD_DIM)
    qcg = qc.reshape(b, A_KV_HEADS, rep, nc, HEAD_DIM)
    scc = jnp.einsum('bgrqd,bgkd->bgrqk', qcg, kc).astype(F32) * scale
    pc = jax.nn.softmax(scc, axis=-1).astype(vc.dtype)
    oc = jnp.einsum('bgrqk,bgkd->bgrqd', pc, vc).reshape(b, A_HEADS, nc, HEAD_DIM)
    return merge_heads(o), merge_heads(oc)


def na_mixer(q, k, v, qc, kc, vc, rpb):
    scale = HEAD_DIM ** -0.5
    q, k, v = split_heads(q, B_HEADS), split_heads(k, B_HEADS), split_heads(v, B_HEADS)
    qc, kc, vc = split_heads(qc, B_HEADS), split_heads(kc, B_HEADS), split_heads(vc, B_HEADS)
    b, h, s, d = q.shape
    rows = s // GRID_W
    wr = min(WIN_R, rows)
    nl = wr * GRID_W
    qg = q.reshape(b, h, rows, GRID_W, d)
    kg = k.reshape(b, h, rows, GRID_W, d)
    vg = v.reshape(b, h, rows, GRID_W, d)
    r_idx = jnp.arange(rows, dtype=jnp.int32)
    row_start = jnp.clip(r_idx - wr // 2, 0, rows - wr)
    cols = jnp.arange(GRID_W, dtype=jnp.int32)
    col_start = jnp.clip(cols - WIN_C // 2, 0, GRID_W - WIN_C)
    col_ok = (cols[None, :] >= col_start[:, None]) & (cols[None, :] < col_start[:, None] + WIN_C)
    dc_idx = jnp.clip(cols[None, :] - cols[:, None] + WIN_C - 1, 0, 2 * WIN_C - 2)
    mask = jnp.broadcast_to(col_ok[:, None, :], (GRID_W, wr, GRID_W)).reshape(GRID_W, nl)

    def row_block(inp):
        q_r, r, rs = inp
        k_r = lax.dynamic_slice_in_dim(kg, rs, wr, axis=2).reshape(b, h, nl, d)
        v_r = lax.dynamic_slice_in_dim(vg, rs, wr, axis=2).reshape(b, h, nl, d)
        dr_idx = rs + jnp.arange(wr, dtype=jnp.int32) - r + WIN_R - 1
        bias = rpb[:, dr_idx[None, :, None], dc_idx[:, None, :]].reshape(h, GRID_W, nl)
        s_loc = jnp.einsum('bhqd,bhkd->bhqk', q_r, k_r).astype(F32) * scale + bias.astype(F32)
        s_loc = jnp.where(mask, s_loc, -jnp.inf)
        s_ctx = jnp.einsum('bhqd,bhkd->bhqk', q_r, kc).astype(F32) * scale
        p = jax.nn.softmax(jnp.concatenate([s_loc, s_ctx], axis=-1), axis=-1).astype(v.dtype)
        return (jnp.einsum('bhqk,bhkd->bhqd', p[..., :nl], v_r)
                + jnp.einsum('bhqk,bhkd->bhqd', p[..., nl:], vc))

    o = lax.map(row_block, (jnp.moveaxis(qg, 2, 0), r_idx, row_start))
    o = jnp.moveaxis(o, 0, 2).reshape(b, h, s, d)
    scc = jnp.einsum('bhqd,bhkd->bhqk', qc, kc).astype(F32) * scale
    oc = jnp.einsum('bhqk,bhkd->bhqd', jax.nn.softmax(scc, axis=-1).astype(vc.dtype), vc)
    return merge_heads(o), merge_heads(oc)


def _to_chunks(x):
    b, h, n = x.shape[:3]
    return jnp.moveaxis(x.reshape(b, h, n // CHUNK, CHUNK, *x.shape[3:]), 2, 0)


def _from_chunks(o):
    nc, b, h, c, dv = o.shape
    return jnp.moveaxis(o, 0, 2).reshape(b, h, nc * c, dv)


def gated_delta_chunked(q, k, v, beta, log_a, s0):
    dk = q.shape[-1]
    tri_incl = jnp.tril(jnp.ones((CHUNK, CHUNK), bool))
    tri_strict = jnp.tril(jnp.ones((CHUNK, CHUNK), bool), -1)
    eye = jnp.eye(CHUNK, dtype=F32)

    def step(s, inp):
        qc, kc, vc, bc, gc = inp
        g = jnp.cumsum(gc, axis=-1)
        decay = jnp.exp(jnp.where(tri_incl, g[..., :, None] - g[..., None, :], -jnp.inf))
        kk = jnp.einsum('bhid,bhjd->bhij', kc, kc)
        lmat = jnp.where(tri_strict, bc[..., :, None] * kk * decay, 0.0)
        rhs = jnp.concatenate([kc * (bc * jnp.exp(g))[..., None], vc * bc[..., None]], axis=-1)
        sol = lax.linalg.triangular_solve(eye + lmat, rhs, left_side=True, lower=True, unit_diagonal=True)
        w, u0 = sol[..., :dk], sol[..., dk:]
        u = u0 - jnp.einsum('bhcd,bhde->bhce', w, s)
        qk = jnp.einsum('bhid,bhjd->bhij', qc, kc) * decay
        o = (jnp.einsum('bhcd,bhde->bhce', qc * jnp.exp(g)[..., None], s)
             + jnp.einsum('bhij,bhje->bhie', qk, u))
        g_last = g[..., -1:]
        s_new = (s * jnp.exp(g_last)[..., None]
                 + jnp.einsum('bhcd,bhce->bhde', kc * jnp.exp(g_last - g)[..., None], u))
        return s_new, o

    s_fin, o = lax.scan(step, s0, tuple(_to_chunks(t) for t in (q, k, v, beta, log_a)))
    return _from_chunks(o), s_fin


def gla_chunked(q, k, v, log_a, s0):
    tri_incl = jnp.tril(jnp.ones((CHUNK, CHUNK), bool))[:, :, None]

    def step(s, inp):
        qc, kc, vc, gc = inp
        g = jnp.cumsum(gc, axis=2)
        diff = g[:, :, :, None, :] - g[:, :, None, :, :]
        decay = jnp.exp(jnp.where(tri_incl, diff, -jnp.inf))
        a = jnp.einsum('bhid,bhijd,bhjd->bhij', qc, decay, kc)
        o = (jnp.einsum('bhcd,bhde->bhce', qc * jnp.exp(g), s)
             + jnp.einsum('bhij,bhje->bhie', a, vc))
        g_last = g[:, :, -1:, :]
        s_new = (s * jnp.exp(g_last[:, :, 0, :])[..., None]
                 + jnp.einsum('bhcd,bhce->bhde', kc * jnp.exp(g_last - g), vc))
        return s_new, o

    s_fin, o = lax.scan(step, s0, tuple(_to_chunks(t) for t in (q, k, v, log_a)))
    return _from_chunks(o), s_fin


def _orient(t, d):
    return t if d == 0 else jnp.flip(t, axis=2)


def bidirectional_scan(scan_fn, lat, lat_dir, ctx, ctx_dir, s0):
    out_l, out_c = 0.0, 0.0
    for d in range(2):
        oc, sc = scan_fn(*[_orient(t, d) for t in ctx], *[_orient(g[d], d) for g in ctx_dir], s0)
        ol, _ = scan_fn(*[_orient(t, d) for t in lat], *[_orient(g[d], d) for g in lat_dir], sc)
        out_l = out_l + _orient(ol, d)
        out_c = out_c + _orient(oc, d)
    return out_l, out_c


def _gated_out(o, gate, gain, n_heads):
    o = rms_norm(o, gain) * jax.nn.silu(split_heads(gate.astype(F32), n_heads))
    return merge_heads(o).astype(gate.dtype)


def short_conv(x, w):
    return lax.conv_general_dilated(x, w[:, None, :].astype(x.dtype), window_strides=(1,),
                                    padding=((C_CONV // 2, C_CONV // 2),),
                                    dimension_numbers=('NWC', 'WIO', 'NWC'),
                                    feature_group_count=x.shape[-1])


def _gdn_prep(qkv, beta_l, a_l, conv_w, a_log, dt_bias):
    b, n, _ = qkv.shape
    qkv = jax.nn.silu(short_conv(qkv, conv_w)).astype(F32)
    q, k, v = jnp.split(qkv, 3, axis=-1)
    q = l2_normalize(split_heads(q, C_HEADS)) * HEAD_DIM ** -0.5
    k = l2_normalize(split_heads(k, C_HEADS))
    v = split_heads(v, C_HEADS)
    beta = jax.nn.sigmoid(beta_l.astype(F32)).reshape(b, n, 2, C_HEADS).transpose(2, 0, 3, 1)
    a = a_l.astype(F32).reshape(b, n, 2, C_HEADS).transpose(2, 0, 3, 1)
    log_a = -jnp.exp(a_log.astype(F32))[:, None, :, None] * jax.nn.softplus(a + dt_bias.astype(F32)[:, None, :, None])
    return q, k, v, beta, log_a


def gdn_mixer(lat, ctx, conv_w, a_log, dt_bias, out_gain):
    ql, kl, vl, bl, gl = _gdn_prep(lat[0], lat[1], lat[2], conv_w, a_log, dt_bias)
    qc, kc, vc, bc, gc = _gdn_prep(ctx[0], ctx[1], ctx[2], conv_w, a_log, dt_bias)
    s0 = jnp.zeros((ql.shape[0], C_HEADS, HEAD_DIM, HEAD_DIM), F32)
    ol, oc = bidirectional_scan(gated_delta_chunked, (ql, kl, vl), (bl, gl), (qc, kc, vc), (bc, gc), s0)
    return _gated_out(ol, lat[3], out_gain, C_HEADS), _gated_out(oc, ctx[3], out_gain, C_HEADS)


def _gla_prep(q, k, v, lr, gw, gb):
    b, n, _ = q.shape
    q = split_heads(q.astype(F32), D_HEADS) * D_KDIM ** -0.5
    k = split_heads(k.astype(F32), D_HEADS)
    v = split_heads(v.astype(F32), D_HEADS)
    z = jnp.einsum('bnzr,zrk->bnzk', lr.astype(F32).reshape(b, n, 2, D_GATE_RANK), gw.astype(F32)) + gb.astype(F32)
    log_a = (jax.nn.log_sigmoid(z) / GLA_TAU).reshape(b, n, 2, D_HEADS, D_KDIM).transpose(2, 0, 3, 1, 4)
    return q, k, v, log_a


def gla_mixer(lat, ctx, gw, gb, out_gain):
    ql, kl, vl, gl = _gla_prep(lat[0], lat[1], lat[2], lat[3], gw, gb)
    qc, kc, vc, gc = _gla_prep(ctx[0], ctx[1], ctx[2], ctx[3], gw, gb)
    s0 = jnp.zeros((ql.shape[0], D_HEADS, D_KDIM, HEAD_DIM), F32)
    ol, oc = bidirectional_scan(gla_chunked, (ql, kl, vl), (gl,), (qc, kc, vc), (gc,), s0)
    return _gated_out(ol, lat[4], out_gain, D_HEADS), _gated_out(oc, ctx[4], out_gain, D_HEADS)


def merge_branches(branches, gates, w_br, w_o):
    o = jnp.stack(branches, axis=2)
    up = jnp.einsum('bnzc,zcd->bnzd', o, w_br)
    g = jax.nn.sigmoid(gates.reshape(*gates.shape[:-1], N_BRANCH, D_MODEL))
    return jnp.sum(g * up, axis=2) @ w_o


def mixer_sublayer(h, hc, w_in, w_br, w_o, a_qg, a_kg, rpb, c_conv, c_a_log, c_dt_bias, c_og,
                   d_gw, d_gb, d_og, rope, with_ctx):
    p = split_in(h @ w_in)
    pc = split_in(hc @ w_in)
    oa, oca = gqa_mixer(p['a_q'], p['a_k'], p['a_v'], pc['a_q'], pc['a_k'], pc['a_v'], a_qg, a_kg, rope)
    ob, ocb = na_mixer(p['b_q'], p['b_k'], p['b_v'], pc['b_q'], pc['b_k'], pc['b_v'], rpb)
    oc_, occ = gdn_mixer((p['c_qkv'], p['c_beta'], p['c_a'], p['c_g']),
                         (pc['c_qkv'], pc['c_beta'], pc['c_a'], pc['c_g']), c_conv, c_a_log, c_dt_bias, c_og)
    od, ocd = gla_mixer((p['d_q'], p['d_k'], p['d_v'], p['d_lr'], p['d_g']),
                        (pc['d_q'], pc['d_k'], pc['d_v'], pc['d_lr'], pc['d_g']), d_gw, d_gb, d_og)
    y = merge_branches((oa, ob, oc_, od), p['gates'], w_br, w_o)
    yc = merge_branches((oca, ocb, occ, ocd), pc['gates'], w_br, w_o) if with_ctx else None
    return y, yc


def hier_moe(h, w_rg, b_rg, w_re, b_re, w_up, w_gate, w_down):
    shp = h.shape
    t = h.reshape(-1, shp[-1])
    lg = (t @ w_rg).astype(F32) + b_rg.astype(F32)
    grp = jnp.argmax(lg, axis=-1)
    p_grp = jnp.take_along_axis(jax.nn.softmax(lg, axis=-1), grp[:, None], axis=-1)
    le = jnp.einsum('td,gde->tge', t, w_re).astype(F32) + b_re.astype(F32)
    le = jnp.take_along_axis(le, grp[:, None, None], axis=1)[:, 0]
    top_p, top_i = lax.top_k(jax.nn.softmax(le, axis=-1), TOP_K)
    wts = p_grp * top_p / jnp.sum(top_p, axis=-1, keepdims=True)
    eid = grp[:, None] * EXP_PER_GROUP + top_i
    dense = jnp.einsum('tk,tke->te', wts, jax.nn.one_hot(eid, N_EXPERTS, dtype=F32)).astype(h.dtype)
    y = jnp.zeros_like(t)
    for e in range(N_EXPERTS):
        a = jax.nn.silu(t @ w_gate[e]) * (t @ w_up[e])
        y = y + dense[:, e:e + 1] * (a @ w_down[e])
    return y.reshape(shp)


def setup_inputs(seed: int = 0) -> dict:
    key = jax.random.key(seed)
    ks = jax.random.split(key, 32)
    L, D = DEPTH, D_MODEL

    def nrm(k, shape, s):
        return jax.random.normal(k, shape, F32) * s

    dt = jnp.exp(jax.random.uniform(ks[12], (L, 2, C_HEADS), F32, math.log(1e-3), math.log(1e-1)))
    return {
        'x': nrm(ks[0], (BATCH, SEQ, D), 1.0),
        'c': nrm(ks[1], (BATCH, D), 1.0),
        'ctx': nrm(ks[2], (BATCH, CTX_LEN, D), 1.0),
        'c_ctx': nrm(ks[3], (D,), 1.0),
        'w_ada': nrm(ks[4], (L, D, 6 * D), 0.5 * D ** -0.5),
        'b_ada': nrm(ks[5], (L, 6 * D), 0.02),
        'w_in': nrm(ks[6], (L, D, IN_W), D ** -0.5),
        'a_q_gain': 1.0 + nrm(ks[7], (L, HEAD_DIM), 0.02),
        'a_k_gain': 1.0 + nrm(ks[8], (L, HEAD_DIM), 0.02),
        'b_rpb': nrm(ks[9], (L, B_HEADS, 2 * WIN_R - 1, 2 * WIN_C - 1), 0.1),
        'c_conv': nrm(ks[10], (L, C_CONV, 3 * C_W), C_CONV ** -0.5),
        'c_a_log': jnp.log(jax.random.uniform(ks[11], (L, 2, C_HEADS), F32, 1.0, 16.0)),
        'c_dt_bias': dt + jnp.log(-jnp.expm1(-dt)),
        'c_out_gain': 1.0 + nrm(ks[13], (L, HEAD_DIM), 0.02),
        'd_gate_w': nrm(ks[14], (L, 2, D_GATE_RANK, D_KW), D_GATE_RANK ** -0.5),
        'd_gate_b': nrm(ks[15], (L, 2, D_KW), 0.1),
        'd_out_gain': 1.0 + nrm(ks[16], (L, HEAD_DIM), 0.02),
        'w_branch': nrm(ks[17], (L, N_BRANCH, A_QW, D), A_QW ** -0.5),
        'w_out': nrm(ks[18], (L, D, D), D ** -0.5 * DN_BETA),
        'ln1_g': 1.0 + nrm(ks[19], (L, D), 0.02),
        'ln1_b': nrm(ks[20], (L, D), 0.02),
        'ln2_g': 1.0 + nrm(ks[21], (L, D), 0.02),
        'ln2_b': nrm(ks[22], (L, D), 0.02),
        'w_router_g': nrm(ks[23], (L, D, N_GROUPS), D ** -0.5),
        'b_router_g': nrm(ks[24], (L, N_GROUPS), 0.01),
        'w_router_e': nrm(ks[25], (L, N_GROUPS, D, EXP_PER_GROUP), D ** -0.5),
        'b_router_e': nrm(ks[26], (L, N_GROUPS, EXP_PER_GROUP), 0.01),
        'w_up': nrm(ks[27], (L, N_EXPERTS, D, EXP_HIDDEN), D ** -0.5),
        'w_gate': nrm(ks[28], (L, N_EXPERTS, D, EXP_HIDDEN), D ** -0.5),
        'w_down': nrm(ks[29], (L, N_EXPERTS, EXP_HIDDEN, D), EXP_HIDDEN ** -0.5 * DN_BETA),
    }


def reference(x, c, ctx, c_ctx, w_ada, b_ada, w_in, a_q_gain, a_k_gain, b_rpb, c_conv, c_a_log, c_dt_bias,
              c_out_gain, d_gate_w, d_gate_b, d_out_gain, w_branch, w_out, ln1_g, ln1_b, ln2_g, ln2_b,
              w_router_g, b_router_g, w_router_e, b_router_e, w_up, w_gate, w_down):
    rope = axial_rope_tables(x.shape[1], x.dtype)
    sc = jax.nn.silu(c)
    scc = jax.nn.silu(c_ctx)
    xc = ctx
    for l in range(DEPTH):
        with_ctx = l < DEPTH - 1
        mod = (sc @ w_ada[l] + b_ada[l])[:, None, :]
        modc = scc @ w_ada[l] + b_ada[l]
        sh1, s1, g1, sh2, s2, g2 = jnp.split(mod, 6, axis=-1)
        sh1c, s1c, g1c, sh2c, s2c, g2c = jnp.split(modc, 6, axis=-1)
        h = x * (1.0 + s1) + sh1
        hc = xc * (1.0 + s1c) + sh1c
        y, yc = mixer_sublayer(h, hc, w_in[l], w_branch[l], w_out[l], a_q_gain[l], a_k_gain[l], b_rpb[l],
                               c_conv[l], c_a_log[l], c_dt_bias[l], c_out_gain[l],
                               d_gate_w[l], d_gate_b[l], d_out_gain[l], rope, with_ctx)
        x = layer_norm(DN_ALPHA * x + g1 * y, ln1_g[l], ln1_b[l])
        h = x * (1.0 + s2) + sh2
        x = layer_norm(DN_ALPHA * x + g2 * hier_moe(h, w_router_g[l], b_router_g[l], w_router_e[l], b_router_e[l],
                                                   w_up[l], w_gate[l], w_down[l]), ln2_g[l], ln2_b[l])
        if with_ctx:
            xc = layer_norm(DN_ALPHA * xc + g1c * yc, ln1_g[l], ln1_b[l])
            hc = xc * (1.0 + s2c) + sh2c
            xc = layer_norm(DN_ALPHA * xc + g2c * hier_moe(hc, w_router_g[l], b_router_g[l], w_router_e[l],
                                                          b_router_e[l], w_up[l], w_gate[l], w_down[l]),
                            ln2_g[l], ln2_b[l])
    return x
```

```python
import numpy as np
import concourse.bass as bass
import concourse.mybir as mybir
from concourse.bass_utils import run_bass_kernel_spmd
from contextlib import ExitStack

F32 = mybir.dt.float32
BF16 = mybir.dt.bfloat16
AF = mybir.ActivationFunctionType
ALU = mybir.AluOpType
AX = mybir.AxisListType

D = 1024
TL = 2048
TC = 256
T = TL + TC
DEPTH = 4
INW = 7216
ALPHA = (2.0 * DEPTH) ** 0.25
EPS = 1e-6
NEG = -30000.0


class Res:
    __slots__ = ("w", "rs")

    def __init__(self):
        self.w = None
        self.rs = []


ENGS = ("pe", "act", "dve", "pool", "sp")


class Sched:
    def __init__(self, nc, ndma_sems=16):
        self.nc = nc
        self.eobj = {"pe": nc.tensor, "act": nc.scalar, "dve": nc.vector, "pool": nc.gpsimd, "sp": nc.sync}
        self.esem = {e: nc.alloc_semaphore("prog_" + e) for e in ENGS}
        self.ecount = {e: 0 for e in ENGS}
        self.dsems = {}
        self.dcount = {}
        self.ndma = ndma_sems
        self.seen = {e: {} for e in ENGS}
        self.nops = 0

    def _wait(self, eng, tok):
        sem, val = tok[0], tok[1]
        k = id(sem)
        if self.seen[eng].get(k, 0) >= val:
            return
        self.seen[eng][k] = val
        self.eobj[eng].wait_ge(sem, val)

    def op(self, eng, fn, reads=(), writes=(), dma=False):
        toks = []
        for r in reads:
            if r.w is not None:
                toks.append(r.w)
        for w in writes:
            if w.w is not None:
                toks.append(w.w)
            toks.extend(w.rs)
        for t in toks:
            if t[2] == eng and not t[3] and eng == "pe":
                continue
            self._wait(eng, t)
        if dma:
            if eng not in self.dsems:
                self.dsems[eng] = [self.nc.alloc_semaphore("dma_%s_%d" % (eng, i)) for i in range(self.ndma)]
                self.dcount[eng] = 0
            j = self.dcount[eng]
            self.dcount[eng] = j + 1
            sem = self.dsems[eng][j % self.ndma]
            if j >= self.ndma:
                self._wait(eng, (sem, 16 * (j // self.ndma)))
            val = 16 * (j // self.ndma + 1)
            ins = fn(self.eobj[eng])
            ins.then_inc(sem, 16)
            tok = (sem, val, eng, True)
        else:
            self.ecount[eng] += 1
            ins = fn(self.eobj[eng])
            ins.then_inc(self.esem[eng], 1)
            tok = (self.esem[eng], self.ecount[eng], eng, False)
        for r in reads:
            r.rs.append(tok)
        for w in writes:
            w.w = tok
            w.rs = []
        self.nops += 1
        return tok

    def dma(self, out, in_, reads=(), writes=(), eng="sp", **kw):
        return self.op(eng, lambda e: e.dma_start(out=out, in_=in_, **kw), reads, writes, dma=True)

    def barrier(self):
        toks = []
        for e in ENGS:
            if self.ecount[e] > 0:
                toks.append((self.esem[e], self.ecount[e], e, False))
        for q, sems in self.dsems.items():
            n = self.dcount[q]
            for i, s in enumerate(sems):
                uses = (n - i + self.ndma - 1) // self.ndma if n > i else 0
                if uses > 0:
                    toks.append((s, 16 * uses, q, True))
        for e in ENGS:
            for t in toks:
                if t[2] == e and not t[3]:
                    continue
                self._wait(e, t)

    def finish(self, toks):
        for t in toks:
            self._wait("sp", t)


CONST_COLS = {}


def _build_consts():
    cols = []

    mcols = []

    def add(name, arr):
        arr = np.asarray(arr, np.float32)
        assert arr.shape[0] == 128
        if name in ("gla_mask", "sel8", "selp", "gdn_ms", "gdn_nms", "gdn_qi", "ident4", "hmask32", "sblk", "hm64", "wmask"):
            CONST_COLS[name] = (1, sum(a.shape[1] for a in mcols), arr.shape[1])
            mcols.append(arr)
        else:
            CONST_COLS[name] = (0, sum(a.shape[1] for a in cols), arr.shape[1])
            cols.append(arr)

    add("ident", np.eye(128))
    add("onesln", np.full((128, 128), 1.0 / 1024))
    bd = np.zeros((128, 128))
    bd[:64, :64] = 1.0 / 64
    bd[64:, 64:] = 1.0 / 64
    add("bd64", bd)
    bdo = np.zeros((128, 128))
    bdo[:64, :64] = 1.0
    bdo[64:, 64:] = 1.0
    add("bd64one", bdo)
    add("ones", np.ones((128, 128)))
    cv = np.zeros((128, 8))
    cv[:, 0] = EPS
    cv[:, 1] = 1.0
    cv[0:4, 2] = 1.0
    cv[4:8, 3] = 1.0
    add("cvec", cv)
    gm = np.zeros((128, 512))
    tri = np.tril(np.ones((64, 64)))
    for h in range(4):
        gm[0:64, h * 64:(h + 1) * 64] = tri.T
        gm[0:64, 256 + h * 64:256 + (h + 1) * 64] = tri
    add("gla_mask", gm)
    sel8 = np.zeros((128, 512))
    for dh in range(8):
        sel8[dh, dh * 64:(dh + 1) * 64] = 1.0
    add("sel8", sel8)
    selp = np.zeros((128, 512))
    for d in range(2):
        for hc in range(2):
            o = (d * 2 + hc) * 128
            selp[d * 4 + 2 * hc, o:o + 64] = 1.0
            selp[d * 4 + 2 * hc + 1, o + 64:o + 128] = 1.0
    add("selp", selp)
    lo = np.tril(np.ones((64, 64)), -1)
    up = np.triu(np.ones((64, 64)), 1)
    ms = np.zeros((128, 512)); nms = np.zeros((128, 512)); qi = np.zeros((128, 512)); id4 = np.zeros((128, 256))
    for h in range(4):
        ms[0:64, h * 64:(h + 1) * 64] = lo
        ms[0:64, 256 + h * 64:256 + (h + 1) * 64] = up
        nms[0:64, h * 64:(h + 1) * 64] = -up
        nms[0:64, 256 + h * 64:256 + (h + 1) * 64] = -lo
        qi[0:64, h * 64:(h + 1) * 64] = up + np.eye(64)
        qi[0:64, 256 + h * 64:256 + (h + 1) * 64] = lo + np.eye(64)
        id4[0:64, h * 64:(h + 1) * 64] = np.eye(64)
    add("gdn_ms", ms)
    add("gdn_nms", nms)
    add("gdn_qi", qi)
    add("ident4", id4)
    hm32 = np.zeros((128, 4)); sblk = np.zeros((128, 256)); hm64 = np.zeros((128, 2)); wmask = np.zeros((128, 256))
    for h in range(4):
        hm32[h * 32:(h + 1) * 32, h] = 1.0
        sblk[h * 32:(h + 1) * 32, h * 64:(h + 1) * 64] = 1.0
        wmask[(h % 2) * 64:(h % 2) * 64 + 64, h * 64:(h + 1) * 64] = 1.0
    hm64[0:64, 0] = 1.0
    hm64[64:128, 1] = 1.0
    add("hmask32", hm32)
    add("sblk", sblk)
    add("hm64", hm64)
    add("wmask", wmask)
    return np.concatenate(cols, axis=1), np.concatenate(mcols, axis=1)


CONSTS, MCONSTS = _build_consts()
NMCONST = MCONSTS.shape[1]
CMASK = np.ones((128, T), np.float32)
CMASK[:, ::64] = 0.0
NCONST = CONSTS.shape[1]


def _rope_tables():
    t = np.arange(TL)
    row = (t // 64).astype(np.float32)
    col = (t % 64).astype(np.float32)
    inv = (np.float32(10000.0) ** (-np.arange(16, dtype=np.float32) / np.float32(16))).astype(np.float32)
    ang_r = row[:, None] * inv
    ang_c = col[:, None] * inv
    C = np.zeros((64, TL), np.float32)
    Sg = np.zeros((64, TL), np.float32)
    P = np.zeros((64, 64), np.float32)
    for m in range(64):
        d = m % 64
        ang = ang_r if d < 32 else ang_c
        f = d % 16
        first = (d % 32) < 16
        C[m] = np.cos(ang[:, f])
        Sg[m] = (-1.0 if first else 1.0) * np.sin(ang[:, f])
        src = m + 16 if first else m - 16
        P[src, m] = 1.0
    return np.concatenate([C, Sg, P], axis=1)


ROPE = _rope_tables()


def _rs(r):
    return min(max(r - 4, 0), 24)


def _na_patterns():
    pats = {}
    table = {}
    for t in range(16):
        r0, r1 = 2 * t, 2 * t + 1
        for j in range(_rs(r0) // 2, (_rs(r1) + 7) // 2 + 1):
            key = tuple((2 * j + a - (2 * t + bq), _rs(2 * t + bq) <= 2 * j + a < _rs(2 * t + bq) + 8) for a in (0, 1) for bq in (0, 1))
            if not any(v for _, v in key):
                continue
            table[(t, j)] = pats.setdefault(key, len(pats))
    return pats, table


NA_PATS, NA_TABLE = _na_patterns()
NPAT = len(NA_PATS)


def _na_bias(rpb):
    L = rpb.shape[0]
    out = np.full((L, NPAT, 128, 4, 128), NEG, np.float32)
    qc = np.arange(64)
    kc = np.arange(64)
    cs = np.clip(qc - 8, 0, 48)
    col_ok = (kc[None, :] >= cs[:, None]) & (kc[None, :] < cs[:, None] + 16)
    dc = np.clip(kc[None, :] - qc[:, None] + 15, 0, 30)
    for key, idx in NA_PATS.items():
        n = 0
        for a in (0, 1):
            for bq in (0, 1):
                dr, valid = key[n]
                n += 1
                if not valid:
                    continue
                blk = rpb[:, :, dr + 7, :][:, :, dc]
                blk = np.where(col_ok[None, None], blk, np.float32(NEG))
                out[:, idx, a * 64:(a + 1) * 64, :, bq * 64:(bq + 1) * 64] = np.transpose(blk, (0, 3, 1, 2))
    return out


PV = {}


def _pv_layout():
    off = 0
    for name, n in [("b_ada", 48), ("ln1_g", 8), ("ln1_b", 8), ("ln2_g", 8), ("ln2_b", 8), ("a_qg", 1), ("a_kg", 1), ("c_og", 1), ("d_og", 1), ("d_gb", 2), ("c_conv", 18), ("c_dtb", 1), ("c_alog", 1)]:
        PV[name] = (off, n)
        off += n
    return off


NPV = _pv_layout()


def _fm(v):
    return np.ascontiguousarray(np.asarray(v, np.float32).reshape(-1, 128).T)


class Kern:
    def __init__(self, nc, dbg=None):
        self.nc = nc
        self.S = Sched(nc)
        self.dbg = dbg or {}
        self.es = ExitStack()
        self.nps = 0

    def sb(self, es, name, shape, dt=F32):
        self.nsb = getattr(self, "nsb", 0) + 1
        return es.enter_context(self.nc.sbuf_tensor("%s_%d" % (name, self.nsb), shape, dt))

    def psum(self):
        i = self.nps % 8
        self.nps += 1
        return self.ps[i], self.rps[i]

    def dump(self, name, ap, shape, reads):
        if not self.dbg.get("dump"):
            return
        d = self.nc.dram_tensor("dump_" + name, list(shape), F32, kind="ExternalOutput").ap()
        self.S.barrier()
        self.S.dma(d, ap, reads=reads)
        self.S.barrier()

    def cst(self, name, rows=128):
        w, o, n = CONST_COLS[name]
        return (self.mconsts if w else self.consts)[0:rows, o:o + n]

    def load_mconsts(self, es):
        self.mconsts = self.sb(es, "mconsts", [128, NMCONST])
        self.S.dma(self.mconsts[:], self.A["mconsts"][:, :], writes=[self.rconst])

    def evac(self, out, in_, reads, writes, i):
        if i % 2 == 0:
            self.S.op("act", lambda e: e.activation(out=out, in_=in_, func=AF.Copy), reads, writes)
        else:
            self.S.op("dve", lambda e: e.tensor_copy(out=out, in_=in_), reads, writes)

    def build(self):
        nc, S = self.nc, self.S
        dt = nc.dram_tensor
        A = {}
        A["xT"] = dt("xT_in", [2, D, TL], F32, kind="ExternalInput").ap()
        A["ctxT"] = dt("ctxT_in", [2, D, TC], F32, kind="ExternalInput").ap()
        A["cc"] = dt("cc", [D, 3], F32, kind="ExternalInput").ap()
        A["consts"] = dt("consts", [128, NCONST], F32, kind="ExternalInput").ap()
        A["rope"] = dt("rope", [64, 2 * TL + 64], F32, kind="ExternalInput").ap()
        A["nab"] = dt("nab", [DEPTH, NPAT, 128, 4, 128], F32, kind="ExternalInput").ap()
        A["mconsts"] = dt("mconsts", [128, NMCONST], F32, kind="ExternalInput").ap()
        A["pvec"] = dt("pvec", [DEPTH, 128, NPV], F32, kind="ExternalInput").ap()
        A["w_ada"] = dt("w_ada", [DEPTH, D, 6 * D], F32, kind="ExternalInput").ap()
        A["w_in"] = dt("w_in", [DEPTH if self.dbg.get("do_proj", True) else 1, D, INW], F32, kind="ExternalInput").ap()
        A["w_branch"] = dt("w_branch", [DEPTH, 4, 256, D], F32, kind="ExternalInput").ap()
        A["w_out"] = dt("w_out", [DEPTH, D, D], F32, kind="ExternalInput").ap()
        A["w_rt"] = dt("w_rt", [DEPTH, D, 36], F32, kind="ExternalInput").ap()
        A["b_rt"] = dt("b_rt", [DEPTH, 36], F32, kind="ExternalInput").ap()
        nlw = DEPTH if self.dbg.get("do_moe", True) else 1
        nex = self.dbg.get("nexp", 32) if self.dbg.get("do_moe", True) else 1
        A["w_up"] = dt("w_up", [nlw, nex, D, 512], F32, kind="ExternalInput").ap()
        A["w_gate"] = dt("w_gate", [nlw, nex, D, 512], F32, kind="ExternalInput").ap()
        A["w_down"] = dt("w_down", [nlw, nex, 512, D], F32, kind="ExternalInput").ap()
        A["out"] = dt("outT", [2, D, TL], F32, kind="ExternalOutput").ap()
        A["projT"] = dt("projT", [INW, T], F32, kind=self.dbg.get("proj_kind", "Internal")).ap()
        A["oT"] = dt("oT", [D, T], F32, kind=self.dbg.get("oT_kind", "Internal")).ap()
        self.A = A
        A["xsave"] = dt("xsave", [D, T], F32, kind="Internal").ap()
        self.rxsave = Res()
        A["cmask"] = dt("cmask", [128, T], F32, kind="ExternalInput").ap()
        A["d_gw"] = dt("d_gw", [DEPTH, 2, 16, 128], F32, kind="ExternalInput").ap()
        self.rprojT = Res()
        self.roT = Res()

        es = self.es
        self.ps = [nc.alloc_psum_tensor("ps%d" % i, [128, 512], F32) for i in range(8)]
        self.rps = [Res() for _ in range(8)]
        self.consts = self.sb(es, "consts", [128, NCONST])
        self.rconst = Res()
        S.dma(self.consts[:], A["consts"][:, :], writes=[self.rconst])
        self.pvec = self.sb(es, "pvec", [128, DEPTH, NPV])
        for l in range(DEPTH):
            S.dma(self.pvec[:, l, :], A["pvec"][l], writes=[self.rconst])
        self.modT = self.sb(es, "modT", [128, DEPTH, 48, 3])
        self.rmod = Res()
        self.x_es = ExitStack()
        self.xT = self.sb(self.x_es, "xT", [128, 8, T])
        self.rx = [[Res() for _ in range(5)] for _ in range(8)]

        self.phase_mods()
        S.barrier()
        outs = []
        nitems = self.dbg.get("nitems", 2)
        nlayers = self.dbg.get("nlayers", DEPTH)
        for b in range(nitems):
            self.load_x(b)
            for l in range(nlayers):
                if self.dbg.get("do_proj", True):
                    self.phase_proj(b, l)
                    S.barrier()
                if self.dbg.get("do_mix", True):
                    if not self.dbg.get("nospill"):
                        self.spill_x()
                    self.phase_mixers(b, l)
                    S.barrier()
                    if not self.dbg.get("nospill"):
                        self.restore_x()
                if self.dbg.get("do_merge", True):
                    self.phase_merge(b, l)
                    S.barrier()
                if self.dbg.get("do_moe", True):
                    self.phase_moe(b, l)
                    S.barrier()
            for k in range(8):
                outs.append(S.dma(A["out"][b, k * 128:(k + 1) * 128, :], self.xT[:, k, 0:TL],
                                  reads=[self.rx[k][j] for j in range(4)], eng="sp"))
            S.barrier()
        S.finish(outs)
        self.x_es.close()
        self.es.close()

    def rxs(self, k, t0, n):
        return [self.rx[k][j] for j in range(t0 // 512, (t0 + n - 1) // 512 + 1)]

    def spill_x(self):
        for k in range(8):
            self.S.dma(self.A["xsave"][k * 128:(k + 1) * 128, :], self.xT[:, k, :], reads=self.rx[k], writes=[self.rxsave])
        self.S.barrier()
        self.x_es.close()

    def restore_x(self):
        self.x_es = ExitStack()
        self.xT = self.sb(self.x_es, "xT", [128, 8, T])
        for k in range(8):
            self.S.dma(self.xT[:, k, :], self.A["xsave"][k * 128:(k + 1) * 128, :], reads=[self.rxsave], writes=self.rx[k])

    def load_x(self, b):
        S, A = self.S, self.A
        for k in range(8):
            S.dma(self.xT[:, k, 0:TL], A["xT"][b, k * 128:(k + 1) * 128, :], writes=self.rx[k][0:4])
            S.dma(self.xT[:, k, TL:T], A["ctxT"][b, k * 128:(k + 1) * 128, :], writes=[self.rx[k][4]])

    def phase_mods(self):
        nc, S, A = self.nc, self.S, self.A
        with ExitStack() as es:
            scT = self.sb(es, "scT", [128, 8, 3])
            rsc = Res()
            S.dma(scT[:], A["cc"].rearrange("(k p) j -> p k j", p=128), writes=[rsc])
            S.op("act", lambda e: e.activation(out=scT[:], in_=scT[:], func=AF.Silu), [rsc], [rsc])
            wt = [self.sb(es, "wada%d" % i, [128, 8, 128]) for i in range(3)]
            rw = [Res() for _ in range(3)]
            n = 0
            for l in range(DEPTH):
                bo = PV["b_ada"][0]
                for c in range(48):
                    w, r = wt[n % 3], rw[n % 3]
                    n += 1
                    S.dma(w[:], A["w_ada"][l, :, c * 128:(c + 1) * 128].rearrange("(k p) c -> p k c", p=128), writes=[r])
                    ps, rp = self.psum()
                    for k in range(8):
                        S.op("pe", lambda e, w=w, ps=ps, k=k: e.matmul(ps[:, 0:3], w[:, k, :], scT[:, k, :], start=(k == 0), stop=(k == 7)),
                             [r, rsc], [rp])
                    S.op("dve", lambda e, ps=ps, l=l, c=c: e.tensor_scalar(out=self.modT[:, l, c, :], in0=ps[:, 0:3],
                                                                         scalar1=self.pvec[:, l, bo + c:bo + c + 1], scalar2=None, op0=ALU.add),
                         [rp, self.rconst], [self.rmod])
                for c0 in (8, 32):
                    S.op("dve", lambda e, l=l, c0=c0: e.tensor_scalar(out=self.modT[:, l, c0:c0 + 8, :], in0=self.modT[:, l, c0:c0 + 8, :],
                                                                    scalar1=1.0, scalar2=None, op0=ALU.add), [self.rmod], [self.rmod])

    def mod(self, l, grp, k, col):
        return self.modT[:, l, grp * 8 + k, col:col + 1]

    def phase_proj(self, b, l):
        nc, S, A = self.nc, self.S, self.A
        groups = [(0, 256), (256, 128), (384, 128), (512, 256), (768, 256), (1024, 256), (1280, 768), (2048, 16), (2064, 256),
                  (2320, 128), (2448, 128), (2576, 256), (2832, 32), (2864, 256), (3120, 4096)]
        chunks = []
        for (o, n) in groups:
            for c in range(0, n, 128):
                chunks.append((o + c, min(128, n - c)))
        with ExitStack() as es:
            hT = self.sb(es, "hT", [128, 8, T])
            rh = Res()
            for k in range(8):
                S.op("dve", lambda e, k=k: e.tensor_scalar(out=hT[:, k, 0:TL], in0=self.xT[:, k, 0:TL], scalar1=self.mod(l, 1, k, b),
                                                         scalar2=self.mod(l, 0, k, b), op0=ALU.mult, op1=ALU.add),
                     [self.rmod] + self.rx[k][0:4], [rh])
                S.op("pool", lambda e, k=k: e.tensor_scalar(out=hT[:, k, TL:T], in0=self.xT[:, k, TL:T], scalar1=self.mod(l, 1, k, 2),
                                                          scalar2=self.mod(l, 0, k, 2), op0=ALU.mult, op1=ALU.add),
                     [self.rmod, self.rx[k][4]], [rh])
            NW = 3
            wt = [self.sb(es, "win%d" % i, [128, 8, 128]) for i in range(NW)]
            rw = [Res() for _ in range(NW)]
            NST = 4
            st = [self.sb(es, "pst%d" % i, [128, 512]) for i in range(NST)]
            rst = [Res() for _ in range(NST)]
            ns = 0
            for ci, (c0, cn) in enumerate(chunks):
                w, r = wt[ci % NW], rw[ci % NW]
                S.dma(w[:, :, 0:cn], A["w_in"][l, :, c0:c0 + cn].rearrange("(k p) c -> p k c", p=128), writes=[r])
                for tb in range(5):
                    t0 = tb * 512
                    tn = min(512, T - t0)
                    ps, rp = self.psum()
                    for k in range(8):
                        S.op("pe", lambda e, w=w, ps=ps, k=k, cn=cn, t0=t0, tn=tn: e.matmul(ps[0:cn, 0:tn], w[:, k, 0:cn], hT[:, k, t0:t0 + tn],
                                                                                       start=(k == 0), stop=(k == 7)), [r, rh], [rp])
                    s_, rs_ = st[ns % NST], rst[ns % NST]
                    self.evac(s_[0:cn, 0:tn], ps[0:cn, 0:tn], [rp], [rs_], ns)
                    ns += 1
                    S.dma(A["projT"][c0:c0 + cn, t0:t0 + tn], s_[0:cn, 0:tn], reads=[rs_], writes=[self.rprojT], eng="pool")

    def phase_mixers(self, b, l):
        which = self.dbg.get("mixers", "abcd")
        if "a" in which:
            self.mixer_a(b, l)
            self.S.barrier()
        if "b" in which:
            self.mixer_b(b, l)
            self.S.barrier()
        if "d" in which:
            self.mixer_d(b, l)
            self.S.barrier()
        if "c" in which:
            self.mixer_c(b, l)
            self.S.barrier()

    def load_rows(self, es, name, r0, nch):
        t = self.sb(es, name, [128, nch, T])
        r = Res()
        self.S.dma(t[:], self.A["projT"][r0:r0 + 128 * nch, :].rearrange("(c p) t -> p c t", p=128), reads=[self.rprojT], writes=[r])
        return t, r

    def load_heads(self, es, name, r0, nh):
        t = self.sb(es, name, [64, nh, T])
        r = Res()
        self.S.dma(t[:], self.A["projT"][r0:r0 + 64 * nh, :].rearrange("(h p) t -> p h t", p=64), reads=[self.rprojT], writes=[r])
        return t, r

    def headnorm(self, X, rX, c, tmps, mat, gain, P=128):
        S = self.S
        (s1, r1), (s2, r2) = tmps
        for t0 in range(0, T, 512):
            tn = min(512, T - t0)
            xs = X[0:P, c, t0:t0 + tn]
            S.op("pool", lambda e: e.tensor_tensor(out=s1[0:P, 0:tn], in0=xs, in1=xs, op=ALU.mult), [rX], [r1])
            ps, rp = self.psum()
            S.op("pe", lambda e: e.matmul(ps[0:P, 0:tn], mat[0:P, 0:P], s1[0:P, 0:tn], start=True, stop=True), [r1, self.rconst], [rp])
            S.op("dve", lambda e: e.tensor_scalar(out=s2[0:P, 0:tn], in0=ps[0:P, 0:tn], scalar1=EPS, scalar2=None, op0=ALU.add), [rp], [r2])
            S.op("act", lambda e: e.activation(out=s2[0:P, 0:tn], in_=s2[0:P, 0:tn], func=AF.Sqrt), [r2], [r2])
            S.op("dve", lambda e: e.reciprocal(out=s2[0:P, 0:tn], in_=s2[0:P, 0:tn]), [r2], [r2])
            S.op("dve", lambda e: e.scalar_tensor_tensor(out=xs, in0=xs, scalar=gain, in1=s2[0:P, 0:tn], op0=ALU.mult, op1=ALU.mult),
                 [r2, self.rconst, rX], [rX])

    def rope(self, X, rX, c, tmps, ropet, rrope):
        S = self.S
        (s1, r1), (s2, r2) = tmps
        for t0 in range(0, TL, 512):
            xs = X[0:64, c, t0:t0 + 512]
            ps, rp = self.psum()
            S.op("pe", lambda e: e.matmul(ps[0:64, 0:512], ropet[0:64, 2 * TL:2 * TL + 64], xs, start=True, stop=True), [rX, rrope], [rp])
            S.op("pool", lambda e: e.tensor_tensor(out=s1[0:64, 0:512], in0=xs, in1=ropet[0:64, t0:t0 + 512], op=ALU.mult), [rX, rrope], [r1])
            S.op("dve", lambda e: e.tensor_tensor(out=s2[0:64, 0:512], in0=ps[0:64, 0:512], in1=ropet[0:64, TL + t0:TL + t0 + 512], op=ALU.mult), [rp, rrope], [r2])
            S.op("pool", lambda e: e.tensor_tensor(out=xs, in0=s1[0:64, 0:512], in1=s2[0:64, 0:512], op=ALU.add), [r1, r2, rX], [rX])

    def build_vaug(self, Vaug, vT, rv, H):
        S = self.S
        rV = Res()
        S.op("pool", lambda e: e.memset(Vaug[:, :, :, 64:128], 1.0), [], [rV])
        n = 0
        for h in range(H):
            for j in range(18):
                ps, rp = self.psum()
                S.op("pe", lambda e: e.transpose(ps[:, 0:64], vT[0:64, h, j * 128:(j + 1) * 128], self.cst("ident")[0:64, 0:64]), [rv, self.rconst], [rp])
                self.evac(Vaug[:, j, h, 0:64], ps[:, 0:64], [rp], [rV], n)
                n += 1
        return Vaug, rV

    def attention(self, es, l, qT, rq, kfun, rk, Vaug, rV, hv, keylist, zrow):
        S, A = self.S, self.A
        H = 4
        ident = self.cst("ident")
        OUT = self.sb(es, "attn_out", [64, 4, T])
        rOUT = Res()
        PT = [self.sb(es, "PT%d" % i, [128, 512]) for i in range(3)]
        rPT = [Res() for _ in range(3)]
        BT = [self.sb(es, "BT%d" % i, [128, 512]) for i in range(3)]
        rBT = [Res() for _ in range(3)]
        Rr = [self.sb(es, "Rr%d" % i, [64, 128]) for i in range(2)]
        rRr = [Res() for _ in range(2)]
        n = 0
        nr = 0
        for t in range(18):
            keys = keylist(t)
            for idx, (j, pat) in enumerate(keys):
                sp, rsp = self.ps[4 + n % 4], self.rps[4 + n % 4]
                pt, rpt = PT[n % 3], rPT[n % 3]
                if pat is not None:
                    bt, rbt = BT[n % 3], rBT[n % 3]
                    S.dma(bt[:], A["nab"][l, pat].rearrange("k h q -> k (h q)"), writes=[rbt])
                    S.op("pe", lambda e: e.matmul(sp[:, 0:512], ident, bt[:], start=True, stop=False), [rbt, self.rconst], [rsp])
                for h in range(H):
                    S.op("pe", lambda e: e.matmul(sp[:, h * 128:(h + 1) * 128], kfun(h, j), qT[0:64, h, t * 128:(t + 1) * 128],
                                                  start=(pat is None), stop=True), [rk, rq], [rsp])
                S.op("act", lambda e: e.activation(out=pt[:], in_=sp[:, 0:512], func=AF.Exp), [rsp], [rpt])
                for h in range(H):
                    S.op("pe", lambda e: e.matmul(self.ps[h][:, 0:128], Vaug[:, j, hv(h), :], pt[:, h * 128:(h + 1) * 128],
                                                  start=(idx == 0), stop=(idx == len(keys) - 1)), [rV, rpt], [self.rps[h]])
                n += 1
            for h in range(H):
                rr_, rrr = Rr[nr % 2], rRr[nr % 2]
                nr += 1
                S.op("dve", lambda e: e.reciprocal(out=rr_[0:64, :], in_=self.ps[h][64:128, 0:128]), [self.rps[h]], [rrr])
                S.op("dve", lambda e: e.tensor_tensor(out=OUT[0:64, h, t * 128:(t + 1) * 128], in0=self.ps[h][0:64, 0:128], in1=rr_[0:64, :], op=ALU.mult),
                     [self.rps[h], rrr], [rOUT])
        S.dma(A["oT"][zrow:zrow + 256, :].rearrange("(h p) t -> p h t", p=64), OUT[:], reads=[rOUT], writes=[self.roT], eng="pool")

    def mixer_a(self, b, l):
        S, A = self.S, self.A
        with ExitStack() as es:
            qT, rq = self.load_heads(es, "aq", 0, 4)
            kT, rk = self.load_heads(es, "ak", 256, 2)
            ropet = self.sb(es, "ropet", [64, 2 * TL + 64])
            rrope = Res()
            S.dma(ropet[:], A["rope"][:, :], writes=[rrope])
            Vaug = self.sb(es, "avaug", [128, 18, 2, 128])
            with ExitStack() as es2:
                vT, rv = self.load_heads(es2, "av", 384, 2)
                Vaug, rV = self.build_vaug(Vaug, vT, rv, 2)
                tmps = [(self.sb(es2, "nt%d" % i, [64, 512]), Res()) for i in range(2)]
                bd64 = self.cst("bd64")
                for h in range(4):
                    self.headnorm(qT, rq, h, tmps, bd64, self.pvec[0:64, l, PV["a_qg"][0]:PV["a_qg"][0] + 1], P=64)
                    self.rope(qT, rq, h, tmps, ropet, rrope)
                    S.op("pool", lambda e: e.tensor_scalar(out=qT[:, h, :], in0=qT[:, h, :], scalar1=0.125, scalar2=None, op0=ALU.mult), [rq], [rq])
                for h in range(2):
                    self.headnorm(kT, rk, h, tmps, bd64, self.pvec[0:64, l, PV["a_kg"][0]:PV["a_kg"][0] + 1], P=64)
                    self.rope(kT, rk, h, tmps, ropet, rrope)
                self.S.barrier()
            kfun = lambda h, j: kT[0:64, h // 2, j * 128:(j + 1) * 128]
            keylist = lambda t: [(j, None) for j in (range(18) if t < 16 else (16, 17))]
            self.attention(es, l, qT, rq, kfun, rk, Vaug, rV, lambda h: h // 2, keylist, 0)

    def mixer_b(self, b, l):
        S, A = self.S, self.A
        with ExitStack() as es:
            qT, rq = self.load_heads(es, "bq", 512, 4)
            kT, rk = self.load_heads(es, "bk", 768, 4)
            for h in range(4):
                S.op("pool", lambda e: e.tensor_scalar(out=qT[:, h, :], in0=qT[:, h, :], scalar1=0.125, scalar2=None, op0=ALU.mult), [rq], [rq])
            Vaug = self.sb(es, "bvaug", [128, 18, 4, 128])
            with ExitStack() as es2:
                vT, rv = self.load_heads(es2, "bv", 1024, 4)
                Vaug, rV = self.build_vaug(Vaug, vT, rv, 4)
                self.S.barrier()
            kfun = lambda h, j: kT[0:64, h, j * 128:(j + 1) * 128]

            def keylist(t):
                if t >= 16:
                    return [(16, None), (17, None)]
                ks = [(j, NA_TABLE[(t, j)]) for j in range(16) if (t, j) in NA_TABLE]
                return ks + [(16, None), (17, None)]

            self.attention(es, l, qT, rq, kfun, rk, Vaug, rV, lambda h: h, keylist, 256)

    def mixer_c(self, b, l):
        S, A = self.S, self.A
        ident = self.cst("ident")
        cvec = self.cst("cvec")
        with ExitStack() as eso:
            OT = self.sb(eso, "cOT", [128, 2, T])
            rOT = Res()
            es = ExitStack()
            self.load_mconsts(es)
            sel8 = self.cst("sel8")
            selp = self.cst("selp")
            QKV, rqkv = self.load_rows(es, "cqkv", 1280, 6)
            with ExitStack() as es2:
                Y = self.sb(es2, "cY", [128, T])
                rY = Res()
                tmps = [(self.sb(es2, "cnt%d" % i, [128, 512]), Res()) for i in range(2)]
                co = PV["c_conv"][0]
                for ch in range(6):
                    w = lambda tap: self.pvec[:, l, co + ch * 3 + tap:co + ch * 3 + tap + 1]
                    X = QKV[:, ch, :]
                    S.op("dve", lambda e: e.tensor_scalar(out=Y[:], in0=X, scalar1=w(1), scalar2=None, op0=ALU.mult), [rqkv, self.rconst], [rY])
                    for (a, n) in ((0, TL), (TL, TC)):
                        S.op("dve", lambda e: e.scalar_tensor_tensor(out=Y[:, a + 1:a + n], in0=QKV[:, ch, a:a + n - 1], scalar=w(0), in1=Y[:, a + 1:a + n],
                                                                     op0=ALU.mult, op1=ALU.add), [rqkv, rY, self.rconst], [rY])
                        S.op("dve", lambda e: e.scalar_tensor_tensor(out=Y[:, a:a + n - 1], in0=QKV[:, ch, a + 1:a + n], scalar=w(2), in1=Y[:, a:a + n - 1],
                                                                     op0=ALU.mult, op1=ALU.add), [rqkv, rY, self.rconst], [rY])
                    S.op("act", lambda e: e.activation(out=QKV[:, ch, :], in_=Y[:], func=AF.Silu), [rY, rqkv], [rqkv])
                for ch in range(4):
                    self.headnorm(QKV, rqkv, ch, tmps, self.cst("bd64one"), 0.125 if ch < 2 else 1.0)
                S.barrier()
            B8 = self.sb(es, "cB8", [8, T])
            G8 = self.sb(es, "cG8", [8, T])
            TOT8 = self.sb(es, "cTOT8", [8, 36])
            ETOT8 = self.sb(es, "cETOT8", [8, 36])
            nal = self.sb(es, "cnal", [8, 1])
            TOK = self.sb(es, "cTOK", [64, 36, 48])
            ETOTP = self.sb(es, "cETOTP", [128, 4, 36])
            es3 = ExitStack()
            cm = self.sb(es3, "ccm", [8, T])
            rcm = Res()
            S.dma(cm[:], A["cmask"][0:8, :], writes=[rcm])
            A8 = self.sb(es3, "cA8", [8, T])
            X8 = self.sb(es3, "cX8", [8, T])
            STK = self.sb(es3, "cSTK", [48, T])
            r8 = Res()
            S.dma(B8[:], A["projT"][2048:2056, :], reads=[self.rprojT], writes=[r8])
            S.dma(A8[:], A["projT"][2056:2064, :], reads=[self.rprojT], writes=[r8])
            dtb = self.pvec[0:8, l, PV["c_dtb"][0]:PV["c_dtb"][0] + 1]
            alog = self.pvec[0:8, l, PV["c_alog"][0]:PV["c_alog"][0] + 1]
            o8 = lambda eng, fn: S.op(eng, fn, [r8, self.rconst, rcm], [r8])
            o8("act", lambda e: e.activation(out=B8[:], in_=B8[:], func=AF.Sigmoid))
            o8("act", lambda e: e.activation(out=nal[:], in_=alog, func=AF.Exp))
            o8("dve", lambda e: e.tensor_scalar(out=nal[:], in0=nal[:], scalar1=-1.0, scalar2=None, op0=ALU.mult))
            o8("dve", lambda e: e.tensor_scalar(out=A8[:], in0=A8[:], scalar1=dtb, scalar2=None, op0=ALU.add))
            o8("act", lambda e: e.activation(out=A8[:], in_=A8[:], func=AF.Exp))
            o8("dve", lambda e: e.tensor_scalar(out=A8[:], in0=A8[:], scalar1=1.0, scalar2=None, op0=ALU.add))
            o8("act", lambda e: e.activation(out=A8[:], in_=A8[:], func=AF.Ln))
            o8("dve", lambda e: e.tensor_scalar(out=A8[:], in0=A8[:], scalar1=nal[:, 0:1], scalar2=None, op0=ALU.mult))
            o8("dve", lambda e: e.tensor_tensor_scan(out=G8[:], data0=cm[:], data1=A8[:], initial=0.0, op0=ALU.mult, op1=ALU.add))
            o8("dve", lambda e: e.tensor_reduce(out=TOT8[:], in_=A8[:].rearrange("p (c i) -> p c i", i=64), axis=AX.X, op=ALU.add))
            o8("dve", lambda e: e.tensor_tensor(out=X8[:], in0=A8[:], in1=G8[:], op=ALU.subtract))
            for c in range(36):
                o8("dve", lambda e: e.tensor_scalar(out=X8[:, c * 64:(c + 1) * 64], in0=X8[:, c * 64:(c + 1) * 64], scalar1=TOT8[:, c:c + 1], scalar2=None, op0=ALU.add))
            o8("dve", lambda e: e.tensor_scalar(out=G8[:], in0=G8[:], scalar1=cvec[0:8, 2:3], scalar2=None, op0=ALU.mult))
            o8("dve", lambda e: e.scalar_tensor_tensor(out=G8[:], in0=X8[:], scalar=cvec[0:8, 3:4], in1=G8[:], op0=ALU.mult, op1=ALU.add))
            o8("act", lambda e: e.activation(out=ETOT8[:], in_=TOT8[:], func=AF.Exp))
            o8("act", lambda e: e.activation(out=A8[:], in_=G8[:], func=AF.Exp))
            S.dma(STK[0:8, :], G8[:], reads=[r8], writes=[r8])
            S.dma(STK[24:32, :], B8[:], reads=[r8], writes=[r8])
            S.dma(STK[32:40, :], A8[:], reads=[r8], writes=[r8])
            o8("dve", lambda e: e.tensor_scalar(out=X8[:], in0=B8[:], scalar1=-1.0, scalar2=None, op0=ALU.mult))
            S.dma(STK[8:16, :], X8[:], reads=[r8], writes=[r8])
            o8("dve", lambda e: e.tensor_tensor(out=X8[:], in0=B8[:], in1=A8[:], op=ALU.mult))
            S.dma(STK[16:24, :], X8[:], reads=[r8], writes=[r8])
            for c in range(36):
                o8("dve", lambda e: e.tensor_scalar(out=X8[:, c * 64:(c + 1) * 64], in0=G8[:, c * 64:(c + 1) * 64], scalar1=TOT8[:, c:c + 1], scalar2=None, op0=ALU.subtract))
            o8("act", lambda e: e.activation(out=X8[:], in_=X8[:], func=AF.Exp, scale=-1.0))
            S.dma(STK[40:48, :], X8[:], reads=[r8], writes=[r8])
            rtok = Res()
            for c in range(36):
                ps, rp = self.psum()
                S.op("pe", lambda e: e.transpose(ps[0:64, 0:48], STK[0:48, c * 64:(c + 1) * 64], ident[0:48, 0:48]), [r8, self.rconst], [rp])
                self.evac(TOK[:, c, :], ps[0:64, 0:48], [rp], [rtok], c)
            for i in range(4):
                ps, rp = self.psum()
                S.op("pe", lambda e: e.matmul(ps[:, 0:36], selp[0:8, i * 128:(i + 1) * 128], ETOT8[:], start=True, stop=True), [r8, self.rconst], [rp])
                S.op("act", lambda e: e.activation(out=ETOTP[:, i, :], in_=ps[:, 0:36], func=AF.Copy), [rp], [rtok])
            S.barrier()
            es3.close()
            Otok = self.sb(es, "cOtok", [64, 36, 256])
            rOtok = Res()
            St = self.sb(es, "cS", [128, 2, 128])
            rS = Res()
            mk = lambda nm, shape=(64, 256): (self.sb(es, nm, list(shape)), Res())
            Ktok, rKt = mk("cKtok")
            Vtok, rVt = mk("cVtok")
            Xt, rXt = mk("cXt")
            Xp, rXp = mk("cXp")
            Dt, rDt = mk("cDt")
            DTt, rDTt = mk("cDTt")
            Mt = [mk("cM%d" % i) for i in range(2)]
            Nt = [mk("cN%d" % i) for i in range(2)]
            Pt, rPt = mk("cP")
            qkT, rqk = mk("cqkT")
            rhsm, rrh = mk("crhs", (64, 512))
            wT, rwT = mk("cwT", (128, 256))
            u0, ru0 = mk("cu0")
            ut, rut = mk("cu")
            o2, ro2 = mk("co2")
            otmp, rotmp = mk("cotmp")
            Kd, rKd = mk("cKd")
            KTm, rKTm = mk("cKTm", (128, 256))
            QTm, rQTm = mk("cQTm", (128, 256))
            hm64 = self.cst("hm64")
            wmask = self.cst("wmask")
            HB = lambda h: slice(h * 64, (h + 1) * 64)
            for d in range(2):
                ms = self.cst("gdn_ms")[0:64, d * 256:(d + 1) * 256]
                nms = self.cst("gdn_nms")[0:64, d * 256:(d + 1) * 256]
                qi = self.cst("gdn_qi")[0:64, d * 256:(d + 1) * 256]
                id4 = self.cst("ident4")[0:64, :]
                S.op("pool", lambda e: e.memset(St[:], 0.0), [], [rS])
                order = (list(range(32, 36)) + list(range(32))) if d == 0 else (list(range(35, 31, -1)) + list(range(31, -1, -1)))
                for c in order:
                    cs = slice(c * 64, (c + 1) * 64)
                    tk = lambda grp, h: TOK[:, c, grp * 8 + d * 4 + h:grp * 8 + d * 4 + h + 1]
                    pk, rpk = self.psum()
                    pv, rpv = self.psum()
                    for hc in range(2):
                        S.op("pe", lambda e: e.transpose(pk[0:64, hc * 128:(hc + 1) * 128], QKV[:, 2 + hc, cs], ident), [rqkv, self.rconst], [rpk])
                        S.op("pe", lambda e: e.transpose(pv[0:64, hc * 128:(hc + 1) * 128], QKV[:, 4 + hc, cs], ident), [rqkv, self.rconst], [rpv])
                    S.op("act", lambda e: e.activation(out=Ktok[:], in_=pk[0:64, 0:256], func=AF.Copy), [rpk], [rKt])
                    S.op("dve", lambda e: e.tensor_copy(out=Vtok[:], in_=pv[0:64, 0:256]), [rpv], [rVt])
                    pkk, rpkk = self.psum()
                    pqk, rpqk = self.psum()
                    pg, rpg = self.psum()
                    pb, rpb = self.psum()
                    for h in range(4):
                        S.op("pool", lambda e: e.tensor_scalar(out=KTm[:, HB(h)], in0=QKV[:, 2 + h // 2, cs], scalar1=hm64[:, h % 2:h % 2 + 1], scalar2=None, op0=ALU.mult),
                             [rqkv, self.rconst], [rKTm])
                        S.op("pool", lambda e: e.tensor_scalar(out=QTm[:, HB(h)], in0=QKV[:, h // 2, cs], scalar1=hm64[:, h % 2:h % 2 + 1], scalar2=None, op0=ALU.mult),
                             [rqkv, self.rconst], [rQTm])
                    for h in range(4):
                        S.op("pe", lambda e: e.matmul(pkk[0:64, HB(h)], KTm[:, HB(h)], QKV[:, 2 + h // 2, cs], start=True, stop=True), [rqkv, rKTm], [rpkk])
                        S.op("pe", lambda e: e.matmul(pqk[0:64, HB(h)], KTm[:, HB(h)], QKV[:, h // 2, cs], start=True, stop=True), [rqkv, rKTm], [rpqk])
                        S.op("pe", lambda e: e.matmul(pg[0:64, HB(h)], sel8[0:8, HB(d * 4 + h)], G8[:, cs], start=True, stop=True), [r8, self.rconst], [rpg])
                        S.op("pe", lambda e: e.matmul(pb[0:64, HB(h)], sel8[0:8, HB(d * 4 + h)], B8[:, cs], start=True, stop=True), [r8, self.rconst], [rpb])
                    for h in range(4):
                        S.op("dve", lambda e: e.tensor_scalar(out=Xt[:, HB(h)], in0=pg[0:64, HB(h)], scalar1=tk(0, h), scalar2=None, op0=ALU.subtract), [rpg, rtok], [rXt])
                    S.op("dve", lambda e: e.tensor_scalar(out=Xp[:], in0=Xt[:], scalar1=0.0, scalar2=None, op0=ALU.max), [rXt], [rXp])
                    S.op("pool", lambda e: e.tensor_tensor(out=Xt[:], in0=Xt[:], in1=Xp[:], op=ALU.subtract), [rXt, rXp], [rXt])
                    S.op("act", lambda e: e.activation(out=Dt[:], in_=Xp[:], func=AF.Exp, scale=-1.0), [rXp], [rDt])
                    S.op("act", lambda e: e.activation(out=DTt[:], in_=Xt[:], func=AF.Exp), [rXt], [rDTt])
                    (M, rM), (N, rN) = Mt[0], Nt[0]
                    S.op("dve", lambda e: e.tensor_tensor(out=Dt[:], in0=Dt[:], in1=pkk[0:64, 0:256], op=ALU.mult), [rDt, rpkk], [rDt])
                    for h in range(4):
                        S.op("dve", lambda e: e.scalar_tensor_tensor(out=M[:, HB(h)], in0=Dt[:, HB(h)], scalar=tk(1, h), in1=ms[:, HB(h)], op0=ALU.mult, op1=ALU.mult),
                             [rDt, rtok, self.rconst], [rM])
                    S.op("dve", lambda e: e.tensor_tensor(out=qkT[:], in0=DTt[:], in1=pqk[0:64, 0:256], op=ALU.mult), [rDTt, rpqk], [rqk])
                    S.op("pool", lambda e: e.tensor_tensor(out=qkT[:], in0=qkT[:], in1=qi, op=ALU.mult), [rqk, self.rconst], [rqk])
                    S.op("dve", lambda e: e.tensor_tensor(out=DTt[:], in0=DTt[:], in1=pkk[0:64, 0:256], op=ALU.mult), [rDTt, rpkk], [rDTt])
                    S.op("dve", lambda e: e.tensor_tensor(out=DTt[:], in0=DTt[:], in1=pb[0:64, 0:256], op=ALU.mult), [rDTt, rpb], [rDTt])
                    S.op("pool", lambda e: e.tensor_tensor(out=N[:], in0=DTt[:], in1=nms, op=ALU.mult), [rDTt, self.rconst], [rN])
                    S.op("pool", lambda e: e.tensor_tensor(out=Pt[:], in0=N[:], in1=id4, op=ALU.add), [rN, self.rconst], [rPt])
                    for lev in range(5):
                        (M, rM), (N, rN) = Mt[lev % 2], Nt[lev % 2]
                        (M2, rM2), (N2, rN2) = Mt[(lev + 1) % 2], Nt[(lev + 1) % 2]
                        pm, rpm = self.psum()
                        for h in range(4):
                            S.op("pe", lambda e: e.matmul(pm[0:64, HB(h)], N[:, HB(h)], M[:, HB(h)], start=True, stop=True), [rM, rN], [rpm])
                        if lev < 4:
                            pn, rpn = self.psum()
                            for h in range(4):
                                S.op("pe", lambda e: e.matmul(pn[0:64, HB(h)], M[:, HB(h)], N[:, HB(h)], start=True, stop=True), [rM, rN], [rpn])
                            S.op("dve", lambda e: e.tensor_copy(out=N2[:], in_=pn[0:64, 0:256]), [rpn], [rN2])
                        S.op("act", lambda e: e.activation(out=M2[:], in_=pm[0:64, 0:256], func=AF.Copy), [rpm], [rM2])
                        pp, rpp = self.psum()
                        for h in range(4):
                            S.op("pe", lambda e: e.matmul(pp[0:64, HB(h)], M2[:, HB(h)], Pt[:, HB(h)], start=True, stop=True), [rM2, rPt], [rpp])
                        S.op("dve", lambda e: e.tensor_tensor(out=Pt[:], in0=Pt[:], in1=pp[0:64, 0:256], op=ALU.add), [rPt, rpp], [rPt])
                    for h in range(4):
                        ko, vo = (0, 64) if h % 2 == 0 else (64, 0)
                        S.op("dve", lambda e: e.tensor_scalar(out=rhsm[:, h * 128 + ko:h * 128 + ko + 64], in0=Ktok[:, HB(h)], scalar1=tk(2, h), scalar2=None, op0=ALU.mult),
                             [rKt, rtok], [rrh])
                        S.op("pool", lambda e: e.tensor_scalar(out=rhsm[:, h * 128 + vo:h * 128 + vo + 64], in0=Vtok[:, HB(h)], scalar1=tk(3, h), scalar2=None, op0=ALU.mult),
                             [rVt, rtok], [rrh])
                    pst, rpst = self.psum()
                    pso, rpso = self.psum()
                    for h in range(4):
                        S.op("pe", lambda e: e.matmul(pst[:, HB(h)], rhsm[:, h * 128:(h + 1) * 128], Pt[:, HB(h)], start=True, stop=True), [rrh, rPt], [rpst])
                        S.op("pe", lambda e: e.matmul(pso[0:64, h * 128:(h + 1) * 128], Pt[:, HB(h)], rhsm[:, h * 128:(h + 1) * 128], start=True, stop=True), [rrh, rPt], [rpso])
                    S.op("dve", lambda e: e.tensor_tensor(out=wT[:], in0=pst[:, 0:256], in1=wmask, op=ALU.mult), [rpst, self.rconst], [rwT])
                    for h in range(4):
                        vo = 64 if h % 2 == 0 else 0
                        S.op("dve", lambda e: e.tensor_copy(out=u0[:, HB(h)], in_=pso[0:64, h * 128 + vo:h * 128 + vo + 64]), [rpso], [ru0])
                    pw, rpw = self.psum()
                    po1, rpo1 = self.psum()
                    for h in range(4):
                        p0 = (h % 2) * 64
                        Sh = St[:, h // 2, p0:p0 + 64]
                        S.op("pe", lambda e: e.matmul(pw[0:64, HB(h)], wT[:, HB(h)], Sh, start=True, stop=True), [rwT, rS], [rpw])
                        S.op("pe", lambda e: e.matmul(po1[0:64, HB(h)], QTm[:, HB(h)], Sh, start=True, stop=True), [rQTm, rS], [rpo1])
                    S.op("dve", lambda e: e.tensor_tensor(out=ut[:], in0=u0[:], in1=pw[0:64, 0:256], op=ALU.subtract), [ru0, rpw], [rut])
                    po2, rpo2 = self.psum()
                    for h in range(4):
                        S.op("pe", lambda e: e.matmul(po2[0:64, HB(h)], qkT[:, HB(h)], ut[:, HB(h)], start=True, stop=True), [rqk, rut], [rpo2])
                    S.op("act", lambda e: e.activation(out=o2[:], in_=po2[0:64, 0:256], func=AF.Copy), [rpo2], [ro2])
                    for h in range(4):
                        if d == 0:
                            S.op("dve", lambda e: e.scalar_tensor_tensor(out=Otok[:, c, HB(h)], in0=po1[0:64, HB(h)], scalar=tk(4, h), in1=o2[:, HB(h)], op0=ALU.mult, op1=ALU.add),
                                 [rpo1, ro2, rtok], [rOtok])
                        else:
                            S.op("dve", lambda e: e.scalar_tensor_tensor(out=otmp[:, HB(h)], in0=po1[0:64, HB(h)], scalar=tk(4, h), in1=o2[:, HB(h)], op0=ALU.mult, op1=ALU.add),
                                 [rpo1, ro2, rtok], [rotmp])
                    if d == 1:
                        S.op("pool", lambda e: e.tensor_tensor(out=Otok[:, c, :], in0=Otok[:, c, :], in1=otmp[:], op=ALU.add), [rotmp, rOtok], [rOtok])
                    for h in range(4):
                        S.op("pool", lambda e: e.tensor_scalar(out=Kd[:, HB(h)], in0=Ktok[:, HB(h)], scalar1=tk(5, h), scalar2=None, op0=ALU.mult), [rKt, rtok], [rKd])
                    for hc in range(2):
                        pS, rpS = self.psum()
                        S.op("pe", lambda e: e.matmul(pS[:, 0:128], Kd[:, hc * 128:(hc + 1) * 128], ut[:, hc * 128:(hc + 1) * 128], start=True, stop=True), [rKd, rut], [rpS])
                        S.op("dve", lambda e: e.scalar_tensor_tensor(out=St[:, hc, :], in0=St[:, hc, :], scalar=ETOTP[:, d * 2 + hc, c:c + 1], in1=pS[:, 0:128],
                                                                     op0=ALU.mult, op1=ALU.add), [rS, rpS, rtok], [rS])
            n = 0
            for c in range(36):
                for hc in range(2):
                    ps, rp = self.psum()
                    S.op("pe", lambda e: e.transpose(ps[:, 0:64], Otok[:, c, hc * 128:(hc + 1) * 128], ident[0:64, 0:64]), [rOtok, self.rconst], [rp])
                    self.evac(OT[:, hc, c * 64:(c + 1) * 64], ps[:, 0:64], [rp], [rOT], n)
                    n += 1
            S.barrier()
            es.close()
            self.gated_tail(eso, [OT, None], [rOT, None], l, "c_og", 2064, 512)


    def mixer_d(self, b, l):
        S, A = self.S, self.A
        ident = self.cst("ident")
        with ExitStack() as eso:
            OTs = [self.sb(eso, "dOT%d" % i, [128, 2, T]) for i in range(2)]
            es = ExitStack()
            self.load_mconsts(es)
            gmask = self.cst("gla_mask")
            hm32 = self.cst("hmask32")
            sblk = self.cst("sblk")
            qT, rq = self.load_rows(es, "dq", 2320, 1)
            kT, rk = self.load_rows(es, "dk", 2448, 1)
            Vtok = self.sb(es, "dvtok", [64, 36, 256])
            rVt = Res()
            with ExitStack() as es2:
                vT, rv = self.load_rows(es2, "dv", 2576, 2)
                n = 0
                for c2 in range(2):
                    for c in range(36):
                        ps, rp = self.psum()
                        S.op("pe", lambda e: e.transpose(ps[0:64, 0:128], vT[:, c2, c * 64:(c + 1) * 64], ident), [rv, self.rconst], [rp])
                        self.evac(Vtok[:, c, c2 * 128:(c2 + 1) * 128], ps[0:64, 0:128], [rp], [rVt], n)
                        n += 1
                S.barrier()
            rOTs = [Res() for _ in range(2)]
            cm = self.sb(es, "cm", [128, T])
            rcm = Res()
            S.dma(cm[:], A["cmask"][:, :], writes=[rcm])
            LA = self.sb(es, "dLA", [128, T])
            G = self.sb(es, "dG", [128, T])
            QE = self.sb(es, "dQE", [128, T])
            KE = self.sb(es, "dKE", [128, T])
            KH = self.sb(es, "dKH", [128, T])
            TOT = self.sb(es, "dTOT", [128, 36])
            ETOT = self.sb(es, "dETOT", [128, 36])
            lr = self.sb(es, "dlr", [16, T])
            gw = self.sb(es, "dgw", [16, 128])
            St = self.sb(es, "dS", [128, 256])
            at = [self.sb(es, "dat%d" % i, [64, 256]) for i in range(2)]
            ktok = [self.sb(es, "dktok%d" % i, [64, 128]) for i in range(2)]
            KEm = [self.sb(es, "dKEm%d" % i, [128, 4, 64]) for i in range(2)]
            rKEm = [Res() for _ in range(2)]
            rw = Res()
            rS = Res()
            rat = [Res() for _ in range(2)]
            rkt = [Res() for _ in range(2)]
            gbo = PV["d_gb"][0]
            for d in range(2):
                OT, rOT = OTs[d], rOTs[d]
                S.dma(lr[:], A["projT"][2832 + 16 * d:2848 + 16 * d, :], reads=[self.rprojT], writes=[rw])
                S.dma(gw[:], A["d_gw"][l, d], writes=[rw])
                for t0 in range(0, T, 512):
                    tn = min(512, T - t0)
                    ps, rp = self.psum()
                    S.op("pe", lambda e: e.matmul(ps[:, 0:tn], gw[:], lr[:, t0:t0 + tn], start=True, stop=True), [rw], [rp])
                    S.op("dve", lambda e: e.tensor_scalar(out=LA[:, t0:t0 + tn], in0=ps[:, 0:tn], scalar1=self.pvec[:, l, gbo + d:gbo + d + 1], scalar2=None,
                                                          op0=ALU.add), [rp, self.rconst], [rw])
                S.op("act", lambda e: e.activation(out=LA[:], in_=LA[:], func=AF.Exp, scale=-1.0), [rw], [rw])
                S.op("pool", lambda e: e.tensor_scalar(out=LA[:], in0=LA[:], scalar1=1.0, scalar2=None, op0=ALU.add), [rw], [rw])
                S.op("act", lambda e: e.activation(out=LA[:], in_=LA[:], func=AF.Ln), [rw], [rw])
                S.op("pool", lambda e: e.tensor_scalar(out=LA[:], in0=LA[:], scalar1=-1.0 / 16.0, scalar2=None, op0=ALU.mult), [rw], [rw])
                S.op("dve", lambda e: e.tensor_tensor_scan(out=G[:], data0=cm[:], data1=LA[:], initial=0.0, op0=ALU.mult, op1=ALU.add), [rw, rcm], [rw])
                S.op("dve", lambda e: e.tensor_reduce(out=TOT[:], in_=LA[:].rearrange("p (c i) -> p c i", i=64), axis=AX.X, op=ALU.add), [rw], [rw])
                if d == 1:
                    S.op("pool", lambda e: e.tensor_tensor(out=G[:], in0=LA[:], in1=G[:], op=ALU.subtract), [rw], [rw])
                    for c in range(36):
                        S.op("dve", lambda e: e.tensor_scalar(out=G[:, c * 64:(c + 1) * 64], in0=G[:, c * 64:(c + 1) * 64], scalar1=TOT[:, c:c + 1], scalar2=None,
                                                              op0=ALU.add), [rw], [rw])
                S.op("act", lambda e: e.activation(out=QE[:], in_=G[:], func=AF.Exp), [rw], [rw])
                S.op("dve", lambda e: e.scalar_tensor_tensor(out=QE[:], in0=QE[:], scalar=32.0 ** -0.5, in1=qT[:, 0, :], op0=ALU.mult, op1=ALU.mult), [rw, rq], [rw])
                S.op("act", lambda e: e.activation(out=KE[:], in_=G[:], func=AF.Exp, scale=-1.0), [rw], [rw])
                S.op("pool", lambda e: e.tensor_tensor(out=KE[:], in0=KE[:], in1=kT[:, 0, :], op=ALU.mult), [rw, rk], [rw])
                for c in range(36):
                    S.op("dve", lambda e: e.tensor_scalar(out=KH[:, c * 64:(c + 1) * 64], in0=G[:, c * 64:(c + 1) * 64], scalar1=TOT[:, c:c + 1], scalar2=None,
                                                          op0=ALU.subtract), [rw], [rw])
                S.op("act", lambda e: e.activation(out=KH[:], in_=KH[:], func=AF.Exp, scale=-1.0), [rw], [rw])
                S.op("pool", lambda e: e.tensor_tensor(out=KH[:], in0=KH[:], in1=kT[:, 0, :], op=ALU.mult), [rw, rk], [rw])
                S.op("act", lambda e: e.activation(out=ETOT[:], in_=TOT[:], func=AF.Exp), [rw], [rw])
                S.op("pool", lambda e: e.memset(St[:], 0.0), [], [rS])
                order = (list(range(32, 36)) + list(range(32))) if d == 0 else (list(range(35, 31, -1)) + list(range(31, -1, -1)))
                for n, c in enumerate(order):
                    cs = slice(c * 64, (c + 1) * 64)
                    a_, ra = at[n % 2], rat[n % 2]
                    k_, rk_ = ktok[n % 2], rkt[n % 2]
                    pa, rpa = self.psum()
                    kem, rkem = KEm[n % 2], rKEm[n % 2]
                    for h in range(4):
                        S.op("pool", lambda e: e.tensor_scalar(out=kem[:, h, :], in0=KE[:, cs], scalar1=hm32[:, h:h + 1], scalar2=None, op0=ALU.mult), [rw, self.rconst], [rkem])
                    for h in range(4):
                        S.op("pe", lambda e: e.matmul(pa[0:64, h * 64:(h + 1) * 64], kem[:, h, :], QE[:, cs], start=True, stop=True), [rw, rkem], [rpa])
                    S.op("dve", lambda e: e.tensor_tensor(out=a_[:], in0=pa[0:64, 0:256], in1=gmask[0:64, d * 256:(d + 1) * 256], op=ALU.mult), [rpa, self.rconst], [ra])
                    pk, rpk = self.psum()
                    S.op("pe", lambda e: e.transpose(pk[0:64, 0:128], KH[:, cs], ident), [rw, self.rconst], [rpk])
                    S.op("act", lambda e: e.activation(out=k_[:], in_=pk[0:64, 0:128], func=AF.Copy), [rpk], [rk_])
                    po, rpo = self.psum()
                    for h in range(4):
                        hs = slice(h * 32, (h + 1) * 32)
                        es_ = slice(h * 64, (h + 1) * 64)
                        S.op("pe", lambda e: e.matmul(po[0:64, es_], St[:, es_], QE[:, cs], start=True, stop=False), [rS, rw], [rpo])
                        S.op("pe", lambda e: e.matmul(po[0:64, es_], Vtok[:, c, es_], a_[:, es_], start=False, stop=True), [rVt, ra], [rpo])
                    for h in range(4):
                        p0 = (h % 2) * 64
                        S.op("act", lambda e: e.activation(out=OT[p0:p0 + 64, h // 2, cs], in_=po[0:64, h * 64:(h + 1) * 64], func=AF.Copy), [rpo], [rOT])
                    pS, rpS = self.psum()
                    S.op("pe", lambda e: e.matmul(pS[:, 0:256], k_[:], Vtok[:, c, :], start=True, stop=True), [rk_, rVt], [rpS])
                    S.op("dve", lambda e: e.scalar_tensor_tensor(out=St[:], in0=St[:], scalar=ETOT[:, c:c + 1], in1=pS[:, 0:256], op0=ALU.mult, op1=ALU.add),
                         [rS, rpS, rw], [rS])
                    S.op("dve", lambda e: e.tensor_tensor(out=St[:], in0=St[:], in1=sblk, op=ALU.mult), [rS, self.rconst], [rS])
            S.barrier()
            es.close()
            self.gated_tail(eso, OTs, rOTs, l, "d_og", 2864, 768)

    def gated_tail(self, es, OTs, rOTs, l, gname, gate_row, zrow):
        S, A = self.S, self.A
        OT, rOT = OTs[0], rOTs[0]
        tmps = [(self.sb(es, "gt%d" % i, [128, 512]), Res()) for i in range(2)]
        gate, rg = self.load_rows(es, "gate", gate_row, 2)
        S.op("act", lambda e: e.activation(out=gate[:], in_=gate[:], func=AF.Silu), [rg], [rg])
        go = PV[gname][0]
        for c in range(2):
            if OTs[1] is not None:
                S.op("pool", lambda e: e.tensor_tensor(out=OT[:, c, :], in0=OT[:, c, :], in1=OTs[1][:, c, :], op=ALU.add), [rOT, rOTs[1]], [rOT])
            self.headnorm(OT, rOT, c, tmps, self.cst("bd64"), self.pvec[:, l, go:go + 1])
            S.op("pool", lambda e: e.tensor_tensor(out=OT[:, c, :], in0=OT[:, c, :], in1=gate[:, c, :], op=ALU.mult), [rOT, rg], [rOT])
        S.dma(A["oT"][zrow:zrow + 256, :].rearrange("(c p) t -> p c t", p=128), OT[:], reads=[rOT], writes=[self.roT], eng="pool")


    def layer_norm(self, es_tiles, l, t0, tn, gname, bname):
        S = self.S
        sq, rsq, rstd, rrstd = es_tiles
        onesln = self.cst("onesln")
        mean_ps, rmp = self.psum()
        for k in range(8):
            S.op("pe", lambda e, k=k: e.matmul(mean_ps[:, 0:tn], onesln, self.xT[:, k, t0:t0 + tn], start=(k == 0), stop=(k == 7)),
                 [self.rconst] + self.rxs(k, t0, tn), [rmp])
        for k in range(8):
            S.op("dve", lambda e, k=k: e.tensor_tensor(out=self.xT[:, k, t0:t0 + tn], in0=self.xT[:, k, t0:t0 + tn], in1=mean_ps[:, 0:tn],
                                                     op=ALU.subtract), [rmp] + self.rxs(k, t0, tn), self.rxs(k, t0, tn))
        var_ps, rvp = self.psum()
        for k in range(8):
            s_, r_ = sq[k % 2], rsq[k % 2]
            S.op("pool", lambda e, k=k, s_=s_: e.tensor_tensor(out=s_[:, 0:tn], in0=self.xT[:, k, t0:t0 + tn], in1=self.xT[:, k, t0:t0 + tn], op=ALU.mult),
                 self.rxs(k, t0, tn), [r_])
            S.op("pe", lambda e, k=k, s_=s_: e.matmul(var_ps[:, 0:tn], onesln, s_[:, 0:tn], start=(k == 0), stop=(k == 7)),
                 [r_, self.rconst], [rvp])
        epsc = self.cst("cvec")[:, 0:1]
        S.op("dve", lambda e: e.tensor_scalar(out=rstd[:, 0:tn], in0=var_ps[:, 0:tn], scalar1=EPS, scalar2=None, op0=ALU.add), [rvp], [rrstd])
        S.op("act", lambda e: e.activation(out=rstd[:, 0:tn], in_=rstd[:, 0:tn], func=AF.Sqrt), [rrstd], [rrstd])
        S.op("dve", lambda e: e.reciprocal(out=rstd[:, 0:tn], in_=rstd[:, 0:tn]), [rrstd], [rrstd])
        go, bo = PV[gname][0], PV[bname][0]
        for k in range(8):
            S.op("dve", lambda e, k=k: e.scalar_tensor_tensor(out=self.xT[:, k, t0:t0 + tn], in0=self.xT[:, k, t0:t0 + tn],
                                                            scalar=self.pvec[:, l, go + k:go + k + 1], in1=rstd[:, 0:tn],
                                                            op0=ALU.mult, op1=ALU.mult), [rrstd, self.rconst] + self.rxs(k, t0, tn), self.rxs(k, t0, tn))
            S.op("pool", lambda e, k=k: e.tensor_scalar(out=self.xT[:, k, t0:t0 + tn], in0=self.xT[:, k, t0:t0 + tn],
                                                      scalar1=self.pvec[:, l, bo + k:bo + k + 1], scalar2=None, op0=ALU.add),
                 self.rxs(k, t0, tn) + [self.rconst], self.rxs(k, t0, tn))

    def phase_merge(self, b, l):
        nc, S, A = self.nc, self.S, self.A
        TB = 256
        with ExitStack() as es:
            wbr = self.sb(es, "wbr", [128, 8, D])
            rwbr = Res()
            S.dma(wbr[:], A["w_branch"][l].rearrange("z (c p) d -> p (z c) d", p=128), writes=[rwbr])
            wo = self.sb(es, "wo", [128, 8, D])
            rwo = Res()
            S.dma(wo[:], A["w_out"][l].rearrange("(k p) d -> p k d", p=128), writes=[rwo])
            oTb = [self.sb(es, "oTb%d" % i, [128, 8, TB]) for i in range(2)]
            roTb = [Res() for _ in range(2)]
            G = [self.sb(es, "G%d" % i, [128, 4, TB]) for i in range(2)]
            rG = [Res() for _ in range(2)]
            m = self.sb(es, "m", [128, 8, TB])
            rm = [Res() for _ in range(8)]
            tmp = [self.sb(es, "mtmp%d" % i, [128, TB]) for i in range(2)]
            rtmp = [Res() for _ in range(2)]
            sq = [self.sb(es, "lnsq%d" % i, [128, TB]) for i in range(2)]
            rsq = [Res() for _ in range(2)]
            rstd = self.sb(es, "lnrstd", [128, TB])
            rrstd = Res()
            ng = 0
            nt = 0
            for bi in range(T // TB):
                t0 = bi * TB
                col = b if t0 < TL else 2
                ob, rob = oTb[bi % 2], roTb[bi % 2]
                S.dma(ob[:], A["oT"][:, t0:t0 + TB].rearrange("(c p) t -> p c t", p=128), reads=[self.roT], writes=[rob])
                for dmc in range(8):
                    g, rg = G[ng % 2], rG[ng % 2]
                    ng += 1
                    S.dma(g[:], A["projT"][3120:7216, t0:t0 + TB].rearrange("(z c p) t -> c p z t", z=4, p=128)[dmc],
                          reads=[self.rprojT], writes=[rg])
                    S.op("act", lambda e, g=g: e.activation(out=g[:], in_=g[:], func=AF.Sigmoid), [rg], [rg])
                    for z in range(4):
                        ps, rp = self.psum()
                        for c2 in range(2):
                            S.op("pe", lambda e, ps=ps, z=z, c2=c2, dmc=dmc, ob=ob: e.matmul(ps[:, 0:TB], wbr[:, z * 2 + c2, dmc * 128:(dmc + 1) * 128],
                                                                                       ob[:, z * 2 + c2, :], start=(c2 == 0), stop=(c2 == 1)),
                                 [rwbr, rob], [rp])
                        if z == 0:
                            S.op("dve", lambda e, ps=ps, g=g, dmc=dmc: e.tensor_tensor(out=m[:, dmc, :], in0=ps[:, 0:TB], in1=g[:, 0, :], op=ALU.mult),
                                 [rp, rg], [rm[dmc]])
                        else:
                            tt, rt = tmp[nt % 2], rtmp[nt % 2]
                            nt += 1
                            S.op("dve", lambda e, ps=ps, g=g, z=z, tt=tt: e.tensor_tensor(out=tt[:], in0=ps[:, 0:TB], in1=g[:, z, :], op=ALU.mult),
                                 [rp, rg], [rt])
                            S.op("pool", lambda e, tt=tt, dmc=dmc: e.tensor_tensor(out=m[:, dmc, :], in0=m[:, dmc, :], in1=tt[:], op=ALU.add),
                                 [rt, rm[dmc]], [rm[dmc]])
                for d2 in range(8):
                    ps, rp = self.psum()
                    for k in range(8):
                        S.op("pe", lambda e, ps=ps, k=k, d2=d2: e.matmul(ps[:, 0:TB], wo[:, k, d2 * 128:(d2 + 1) * 128], m[:, k, :],
                                                                       start=(k == 0), stop=(k == 7)), [rwo, rm[k]], [rp])
                    tt, rt = tmp[nt % 2], rtmp[nt % 2]
                    nt += 1
                    S.op("act", lambda e, ps=ps, tt=tt, d2=d2: e.activation(out=tt[:], in_=ps[:, 0:TB], func=AF.Identity, scale=self.mod(l, 2, d2, col)),
                         [rp, self.rmod], [rt])
                    S.op("dve", lambda e, tt=tt, d2=d2: e.scalar_tensor_tensor(out=self.xT[:, d2, t0:t0 + TB], in0=self.xT[:, d2, t0:t0 + TB], scalar=ALPHA,
                                                                            in1=tt[:], op0=ALU.mult, op1=ALU.add),
                         [rt] + self.rxs(d2, t0, TB), self.rxs(d2, t0, TB))
                if self.dbg.get("merge_ln", True):
                    self.layer_norm((sq, rsq, rstd, rrstd), l, t0, TB, "ln1_g", "ln1_b")

    def phase_moe(self, b, l):
        nc, S, A = self.nc, self.S, self.A
        ident = self.cst("ident")
        with ExitStack() as es:
            h2 = self.sb(es, "h2", [128, 8, T], BF16)
            rh2 = Res()
            denseT = self.sb(es, "denseT", [32, T])
            rdT = [Res() for _ in range(5)]
            for k in range(8):
                S.op("dve", lambda e, k=k: e.tensor_scalar(out=h2[:, k, 0:TL], in0=self.xT[:, k, 0:TL], scalar1=self.mod(l, 4, k, b),
                                                         scalar2=self.mod(l, 3, k, b), op0=ALU.mult, op1=ALU.add),
                     [self.rmod] + self.rx[k][0:4], [rh2])
                S.op("pool", lambda e, k=k: e.tensor_scalar(out=h2[:, k, TL:T], in0=self.xT[:, k, TL:T], scalar1=self.mod(l, 4, k, 2),
                                                          scalar2=self.mod(l, 3, k, 2), op0=ALU.mult, op1=ALU.add),
                     [self.rmod, self.rx[k][4]], [rh2])
            with ExitStack() as es2:
                wrt = self.sb(es2, "wrt", [128, 8, 36])
                rwrt = Res()
                S.dma(wrt[:], A["w_rt"][l].rearrange("(k p) c -> p k c", p=128), writes=[rwrt])
                brow = self.sb(es2, "brow", [1, 36])
                S.dma(brow[:], A["b_rt"][l:l + 1, :], writes=[rwrt])
                h2f = [self.sb(es2, "h2f%d" % i, [128, 8, 128]) for i in range(2)]
                rh2f = [Res() for _ in range(2)]
                R = [self.sb(es2, "rt%d" % i, [128, 160]) for i in range(2)]
                rR = [Res() for _ in range(2)]
                ones = self.cst("ones")
                for tt in range(T // 128):
                    t0 = tt * 128
                    col = b if t0 < TL else 2
                    hf, rhf = h2f[tt % 2], rh2f[tt % 2]
                    r_, rr = R[tt % 2], rR[tt % 2]
                    for k in range(8):
                        S.op("dve", lambda e, k=k, hf=hf: e.tensor_scalar(out=hf[:, k, :], in0=self.xT[:, k, t0:t0 + 128], scalar1=self.mod(l, 4, k, col),
                                                                        scalar2=self.mod(l, 3, k, col), op0=ALU.mult, op1=ALU.add),
                             [self.rmod] + self.rxs(k, t0, 128), [rhf])
                    ps, rp = self.psum()
                    for k in range(8):
                        S.op("pe", lambda e, k=k, hf=hf, ps=ps: e.matmul(ps[:, 0:36], hf[:, k, :], wrt[:, k, :], start=(k == 0), stop=False), [rhf, rwrt], [rp])
                    S.op("pe", lambda e, ps=ps: e.matmul(ps[:, 0:36], ones[0:1, :], brow[0:1, :], start=False, stop=True), [rwrt, self.rconst], [rp])
                    L = r_[:, 0:36]
                    sc = lambda i, r_=r_: r_[:, 140 + i:141 + i]
                    MG, NMG, SG, PG, M1, M2, DD, EE, RR, W1, W2 = range(11)
                    S.op("act", lambda e, ps=ps: e.activation(out=L, in_=ps[:, 0:36], func=AF.Copy), [rp], [rr])
                    dv = lambda fn: S.op("dve", fn, [rr], [rr])
                    dv(lambda e: e.tensor_reduce(out=sc(MG), in_=r_[:, 0:4], axis=AX.X, op=ALU.max))
                    dv(lambda e: e.tensor_scalar(out=r_[:, 60:64], in0=r_[:, 0:4], scalar1=sc(MG), scalar2=None, op0=ALU.subtract))
                    S.op("act", lambda e: e.activation(out=r_[:, 60:64], in_=r_[:, 60:64], func=AF.Exp), [rr], [rr])
                    dv(lambda e: e.tensor_reduce(out=sc(SG), in_=r_[:, 60:64], axis=AX.X, op=ALU.add))
                    dv(lambda e: e.reciprocal(out=sc(PG), in_=sc(SG)))
                    dv(lambda e: e.tensor_scalar(out=r_[:, 40:44], in0=r_[:, 0:4], scalar1=sc(MG), scalar2=None, op0=ALU.is_equal))
                    dv(lambda e: e.tensor_scalar(out=r_[:, 44:52], in0=r_[:, 4:12], scalar1=r_[:, 40:41], scalar2=None, op0=ALU.mult))
                    for g in range(1, 4):
                        dv(lambda e, g=g: e.scalar_tensor_tensor(out=r_[:, 44:52], in0=r_[:, 4 + 8 * g:12 + 8 * g], scalar=r_[:, 40 + g:41 + g],
                                                                 in1=r_[:, 44:52], op0=ALU.mult, op1=ALU.add))
                    dv(lambda e: e.tensor_reduce(out=sc(M1), in_=r_[:, 44:52], axis=AX.X, op=ALU.max))
                    dv(lambda e: e.tensor_scalar(out=r_[:, 52:60], in0=r_[:, 44:52], scalar1=sc(M1), scalar2=None, op0=ALU.is_equal))
                    dv(lambda e: e.scalar_tensor_tensor(out=r_[:, 60:68], in0=r_[:, 52:60], scalar=NEG, in1=r_[:, 44:52], op0=ALU.mult, op1=ALU.add))
                    dv(lambda e: e.tensor_reduce(out=sc(M2), in_=r_[:, 60:68], axis=AX.X, op=ALU.max))
                    dv(lambda e: e.tensor_scalar(out=r_[:, 68:76], in0=r_[:, 60:68], scalar1=sc(M2), scalar2=None, op0=ALU.is_equal))
                    dv(lambda e: e.tensor_tensor(out=sc(DD), in0=sc(M2), in1=sc(M1), op=ALU.subtract))
                    S.op("act", lambda e: e.activation(out=sc(EE), in_=sc(DD), func=AF.Exp), [rr], [rr])
                    dv(lambda e: e.tensor_scalar(out=sc(RR), in0=sc(EE), scalar1=1.0, scalar2=None, op0=ALU.add))
                    dv(lambda e: e.reciprocal(out=sc(RR), in_=sc(RR)))
                    dv(lambda e: e.tensor_tensor(out=sc(W1), in0=sc(RR), in1=sc(PG), op=ALU.mult))
                    dv(lambda e: e.tensor_tensor(out=sc(W2), in0=sc(W1), in1=sc(EE), op=ALU.mult))
                    dv(lambda e: e.tensor_scalar(out=r_[:, 76:84], in0=r_[:, 52:60], scalar1=sc(W1), scalar2=None, op0=ALU.mult))
                    dv(lambda e: e.scalar_tensor_tensor(out=r_[:, 76:84], in0=r_[:, 68:76], scalar=sc(W2), in1=r_[:, 76:84], op0=ALU.mult, op1=ALU.add))
                    for g in range(4):
                        dv(lambda e, g=g: e.tensor_scalar(out=r_[:, 84 + 8 * g:92 + 8 * g], in0=r_[:, 76:84], scalar1=r_[:, 40 + g:41 + g], scalar2=None,
                                                          op0=ALU.mult))
                    ps2, rp2 = self.psum()
                    S.op("pe", lambda e, ps2=ps2: e.transpose(ps2[0:32, 0:128], r_[:, 84:116], ident), [rr, self.rconst], [rp2])
                    S.op("act", lambda e, ps2=ps2: e.activation(out=denseT[:, t0:t0 + 128], in_=ps2[0:32, 0:128], func=AF.Copy), [rp2], [rdT[t0 // 512]])
                S.barrier()
            self.dump("denseT", denseT[:], [32, T], rdT)
            for k in range(8):
                for j in range(5):
                    t0 = j * 512
                    tn = min(512, T - t0)
                    S.op("pool", lambda e, k=k, t0=t0, tn=tn: e.tensor_scalar(out=self.xT[:, k, t0:t0 + tn], in0=self.xT[:, k, t0:t0 + tn], scalar1=ALPHA,
                                                                          scalar2=None, op0=ALU.mult), [self.rx[k][j]], [self.rx[k][j]])
            with ExitStack() as es3:
                NSTG = 2
                stg = [self.sb(es3, "stg%d" % i, [128, 2048]) for i in range(NSTG)]
                rstg = [Res() for _ in range(NSTG)]
                wgu = [self.sb(es3, "wgu%d" % i, [128, 8, 512], BF16) for i in range(3)]
                rwgu = [Res() for _ in range(3)]
                wdn = [self.sb(es3, "wdn%d" % i, [128, 4, D], BF16) for i in range(2)]
                rwdn = [Res() for _ in range(2)]
                abf = [self.sb(es3, "abf%d" % i, [128, 4, 512], BF16) for i in range(2)]
                rabf = [Res() for _ in range(2)]
                sgt = [self.sb(es3, "sgt%d" % i, [128, 512]) for i in range(2)]
                rsgt = [Res() for _ in range(2)]
                tt_ = [self.sb(es3, "ttm%d" % i, [128, 512]) for i in range(2)]
                rtt = [Res() for _ in range(2)]
                dsel = [self.sb(es3, "dsel%d" % i, [32, 512]) for i in range(2)]
                rdsel = [Res() for _ in range(2)]
                dB = [self.sb(es3, "dB%d" % i, [128, 512]) for i in range(2)]
                rdB = [Res() for _ in range(2)]
                ones = self.cst("ones")
                nstg = 0
                ngu = 0
                cnt = 0
                for e_ in range(self.dbg.get("e_start", 0), self.dbg.get("nexp", 32)):
                    ws = []
                    for wi, nm in enumerate(("w_gate", "w_up")):
                        w, rw = wgu[ngu % 3], rwgu[ngu % 3]
                        ngu += 1
                        for hh in range(2):
                            s_, rs_ = stg[nstg % NSTG], rstg[nstg % NSTG]
                            nstg += 1
                            S.dma(s_[:].rearrange("p (k h) -> p k h", k=8), A[nm][l, e_, :, hh * 256:(hh + 1) * 256].rearrange("(k p) h -> p k h", p=128),
                                  writes=[rs_])
                            S.op("pool", lambda e, s_=s_, w=w, hh=hh: e.tensor_copy(out=w[:, :, hh * 256:(hh + 1) * 256],
                                                                                 in_=s_[:].rearrange("p (k h) -> p k h", k=8)), [rs_], [rw])
                        ws.append((w, rw))
                    (wg, rwg), (wu, rwu) = ws
                    wd, rwd = wdn[e_ % 2], rwdn[e_ % 2]
                    for hh in range(2):
                        s_, rs_ = stg[nstg % NSTG], rstg[nstg % NSTG]
                        nstg += 1
                        S.dma(s_[:].rearrange("p (c d) -> p c d", c=2), A["w_down"][l, e_, hh * 256:(hh + 1) * 256, :].rearrange("(c p) d -> p c d", p=128),
                              writes=[rs_])
                        S.op("pool", lambda e, s_=s_, wd=wd, hh=hh: e.tensor_copy(out=wd[:, hh * 2:hh * 2 + 2, :],
                                                                               in_=s_[:].rearrange("p (c d) -> p c d", c=2)), [rs_], [rwd])
                    for tb in range(5):
                        t0 = tb * 512
                        tn = min(512, T - t0)
                        col = b if t0 < TL else 2
                        cnt += 1
                        ds_, rds = dsel[cnt % 2], rdsel[cnt % 2]
                        db_, rdb = dB[cnt % 2], rdB[cnt % 2]
                        ab, rab = abf[cnt % 2], rabf[cnt % 2]
                        S.op("dve", lambda e, ds_=ds_, t0=t0, tn=tn, e_=e_: e.tensor_scalar(out=ds_[:, 0:tn], in0=denseT[:, t0:t0 + tn], scalar1=ident[0:32, e_:e_ + 1],
                                                                                       scalar2=None, op0=ALU.mult), [rdT[tb], self.rconst], [rds])
                        psb, rpb = self.psum()
                        S.op("pe", lambda e, psb=psb, ds_=ds_, tn=tn: e.matmul(psb[:, 0:tn], ones[0:32, :], ds_[:, 0:tn], start=True, stop=True),
                             [rds, self.rconst], [rpb])
                        S.op("act", lambda e, psb=psb, db_=db_, tn=tn: e.activation(out=db_[:, 0:tn], in_=psb[:, 0:tn], func=AF.Copy), [rpb], [rdb])
                        for hc in range(4):
                            psg, rpg = self.psum()
                            for k in range(8):
                                S.op("pe", lambda e, psg=psg, k=k, hc=hc, wg=wg, t0=t0, tn=tn: e.matmul(psg[:, 0:tn], wg[:, k, hc * 128:(hc + 1) * 128], h2[:, k, t0:t0 + tn],
                                                                                                start=(k == 0), stop=(k == 7)), [rwg, rh2], [rpg])
                            psu, rpu = self.psum()
                            for k in range(8):
                                S.op("pe", lambda e, psu=psu, k=k, hc=hc, wu=wu, t0=t0, tn=tn: e.matmul(psu[:, 0:tn], wu[:, k, hc * 128:(hc + 1) * 128], h2[:, k, t0:t0 + tn],
                                                                                                start=(k == 0), stop=(k == 7)), [rwu, rh2], [rpu])
                            i2 = (cnt * 4 + hc) % 2
                            sg_, rsg = sgt[i2], rsgt[i2]
                            t_, rt_ = tt_[i2], rtt[i2]
                            S.op("act", lambda e, psg=psg, sg_=sg_, tn=tn: e.activation(out=sg_[:, 0:tn], in_=psg[:, 0:tn], func=AF.Silu), [rpg], [rsg])
                            S.op("dve", lambda e, psu=psu, sg_=sg_, t_=t_, tn=tn: e.tensor_tensor(out=t_[:, 0:tn], in0=psu[:, 0:tn], in1=sg_[:, 0:tn], op=ALU.mult),
                                 [rpu, rsg], [rt_])
                            S.op("pool", lambda e, t_=t_, db_=db_, ab=ab, hc=hc, tn=tn: e.tensor_tensor(out=ab[:, hc, 0:tn], in0=t_[:, 0:tn], in1=db_[:, 0:tn], op=ALU.mult),
                                 [rt_, rdb], [rab])
                        for dmc in range(8):
                            psd, rpd = self.psum()
                            for hc in range(4):
                                S.op("pe", lambda e, psd=psd, hc=hc, dmc=dmc, wd=wd, ab=ab, tn=tn: e.matmul(psd[:, 0:tn], wd[:, hc, dmc * 128:(dmc + 1) * 128], ab[:, hc, 0:tn],
                                                                                                    start=(hc == 0), stop=(hc == 3)), [rwd, rab], [rpd])
                            S.op("dve", lambda e, psd=psd, dmc=dmc, t0=t0, tn=tn, col=col: e.scalar_tensor_tensor(out=self.xT[:, dmc, t0:t0 + tn], in0=psd[:, 0:tn],
                                                                                                              scalar=self.mod(l, 5, dmc, col), in1=self.xT[:, dmc, t0:t0 + tn],
                                                                                                              op0=ALU.mult, op1=ALU.add),
                                 [rpd, self.rmod, self.rx[dmc][tb]], [self.rx[dmc][tb]])
                S.barrier()
            with ExitStack() as es4:
                sq = [self.sb(es4, "lnsq%d" % i, [128, 512]) for i in range(2)]
                rsq = [Res() for _ in range(2)]
                rstd = self.sb(es4, "lnrstd", [128, 512])
                rrstd = Res()
                for tb in range(5):
                    t0 = tb * 512
                    tn = min(512, T - t0)
                    self.layer_norm((sq, rsq, rstd, rrstd), l, t0, tn, "ln2_g", "ln2_b")


def make_in_maps(inputs, ncores=8):
    f = lambda a: np.ascontiguousarray(np.asarray(a, np.float32))
    x, c, ctx, c_ctx = inputs["x"], inputs["c"], inputs["ctx"], inputs["c_ctx"]
    pvec = np.zeros((DEPTH, 128, NPV), np.float32)
    for l in range(DEPTH):
        for name in ("b_ada", "ln1_g", "ln1_b", "ln2_g", "ln2_b"):
            o, n = PV[name]
            pvec[l, :, o:o + n] = _fm(inputs[name][l])
        for name, src in (("a_qg", "a_q_gain"), ("a_kg", "a_k_gain"), ("c_og", "c_out_gain"), ("d_og", "d_out_gain")):
            pvec[l, :, PV[name][0]] = np.tile(np.asarray(inputs[src][l], np.float32), 2)
        cw = np.asarray(inputs["c_conv"][l], np.float32)
        for ch in range(6):
            for tap in range(3):
                pvec[l, :, PV["c_conv"][0] + ch * 3 + tap] = cw[tap, ch * 128:(ch + 1) * 128]
        pvec[l, 0:8, PV["c_dtb"][0]] = np.asarray(inputs["c_dt_bias"][l], np.float32).reshape(8)
        pvec[l, 0:8, PV["c_alog"][0]] = np.asarray(inputs["c_a_log"][l], np.float32).reshape(8)
        pvec[l, :, PV["d_gb"][0]:PV["d_gb"][0] + 2] = np.asarray(inputs["d_gate_b"][l], np.float32).T
    w_rt = np.concatenate([inputs["w_router_g"], np.transpose(inputs["w_router_e"], (0, 2, 1, 3)).reshape(DEPTH, D, 32)], axis=2)
    b_rt = np.concatenate([inputs["b_router_g"], inputs["b_router_e"].reshape(DEPTH, 32)], axis=1)
    shared = {"mconsts": MCONSTS, "cmask": CMASK, "d_gw": f(inputs["d_gate_w"]), "rope": ROPE, "nab": _na_bias(np.asarray(inputs["b_rpb"], np.float32)), "consts": CONSTS, "pvec": pvec, "w_ada": f(inputs["w_ada"]), "w_in": f(inputs["w_in"]), "w_branch": f(inputs["w_branch"]),
              "w_out": f(inputs["w_out"]), "w_rt": f(w_rt), "b_rt": f(b_rt), "w_up": f(inputs["w_up"]), "w_gate": f(inputs["w_gate"]),
              "w_down": f(inputs["w_down"])}
    maps = []
    for i in range(ncores):
        bs = slice(2 * i, 2 * i + 2)
        m = dict(shared)
        m["xT_in"] = f(np.transpose(x[bs], (0, 2, 1)))
        m["ctxT_in"] = f(np.transpose(ctx[bs], (0, 2, 1)))
        m["cc"] = f(np.stack([c[2 * i], c[2 * i + 1], c_ctx], axis=1))
        maps.append(m)
    return maps


def kernel(**inputs):
    nc = bass.Bass("TRN2", target_bir_lowering=False)
    Kern(nc).build()
    maps = make_in_maps(inputs)
    res = run_bass_kernel_spmd(nc, maps, core_ids=list(range(8)))
    out = np.zeros((16, TL, D), np.float32)
    for i in range(8):
        o = res.results[i]["outT"]
        out[2 * i:2 * i + 2] = np.transpose(o, (0, 2, 1))
    return out
```

```python
import numpy as np
import concourse.bass as bass
import concourse.mybir as mybir
from concourse.bass_utils import run_bass_kernel_spmd
from contextlib import ExitStack

F32 = mybir.dt.float32
BF16 = mybir.dt.bfloat16
AF = mybir.ActivationFunctionType
ALU = mybir.AluOpType
AX = mybir.AxisListType

D = 1024
TL = 2048
TC = 256
T = TL + TC
DEPTH = 4
INW = 7216
ALPHA = (2.0 * DEPTH) ** 0.25
EPS = 1e-6
NEG = -30000.0


class Res:
    __slots__ = ("w", "rs")

    def __init__(self):
        self.w = None
        self.rs = []


ENGS = ("pe", "act", "dve", "pool", "sp")


class Sched:
    def __init__(self, nc, ndma_sems=16):
        self.nc = nc
        self.eobj = {"pe": nc.tensor, "act": nc.scalar, "dve": nc.vector, "pool": nc.gpsimd, "sp": nc.sync}
        self.esem = {e: nc.alloc_semaphore("prog_" + e) for e in ENGS}
        self.ecount = {e: 0 for e in ENGS}
        self.dsems = {}
        self.dcount = {}
        self.ndma = ndma_sems
        self.seen = {e: {} for e in ENGS}
        self.nops = 0

    def _wait(self, eng, tok):
        sem, val = tok[0], tok[1]
        k = id(sem)
        if self.seen[eng].get(k, 0) >= val:
            return
        self.seen[eng][k] = val
        self.eobj[eng].wait_ge(sem, val)

    def op(self, eng, fn, reads=(), writes=(), dma=False):
        toks = []
        for r in reads:
            if r.w is not None:
                toks.append(r.w)
        for w in writes:
            if w.w is not None:
                toks.append(w.w)
            toks.extend(w.rs)
        for t in toks:
            if t[2] == eng and not t[3] and eng == "pe":
                continue
            self._wait(eng, t)
        if dma:
            if eng not in self.dsems:
                self.dsems[eng] = [self.nc.alloc_semaphore("dma_%s_%d" % (eng, i)) for i in range(self.ndma)]
                self.dcount[eng] = 0
            j = self.dcount[eng]
            self.dcount[eng] = j + 1
            sem = self.dsems[eng][j % self.ndma]
            if j >= self.ndma:
                self._wait(eng, (sem, 16 * (j // self.ndma)))
            val = 16 * (j // self.ndma + 1)
            ins = fn(self.eobj[eng])
            ins.then_inc(sem, 16)
            tok = (sem, val, eng, True)
        else:
            self.ecount[eng] += 1
            ins = fn(self.eobj[eng])
            ins.then_inc(self.esem[eng], 1)
            tok = (self.esem[eng], self.ecount[eng], eng, False)
        for r in reads:
            r.rs.append(tok)
        for w in writes:
            w.w = tok
            w.rs = []
        self.nops += 1
        return tok

    def dma(self, out, in_, reads=(), writes=(), eng="sp", **kw):
        return self.op(eng, lambda e: e.dma_start(out=out, in_=in_, **kw), reads, writes, dma=True)

    def barrier(self):
        toks = []
        for e in ENGS:
            if self.ecount[e] > 0:
                toks.append((self.esem[e], self.ecount[e], e, False))
        for q, sems in self.dsems.items():
            n = self.dcount[q]
            for i, s in enumerate(sems):
                uses = (n - i + self.ndma - 1) // self.ndma if n > i else 0
                if uses > 0:
                    toks.append((s, 16 * uses, q, True))
        for e in ENGS:
            for t in toks:
                if t[2] == e and not t[3]:
                    continue
                self._wait(e, t)

    def finish(self, toks):
        for t in toks:
            self._wait("sp", t)


CONST_COLS = {}


def _build_consts():
    cols = []

    mcols = []

    def add(name, arr):
        arr = np.asarray(arr, np.float32)
        assert arr.shape[0] == 128
        if name in ("gla_mask", "sel8", "selp", "gdn_ms", "gdn_nms", "gdn_qi", "ident4", "hmask32", "sblk", "hm64", "wmask"):
            CONST_COLS[name] = (1, sum(a.shape[1] for a in mcols), arr.shape[1])
            mcols.append(arr)
        else:
            CONST_COLS[name] = (0, sum(a.shape[1] for a in cols), arr.shape[1])
            cols.append(arr)

    add("ident", np.eye(128))
    add("onesln", np.full((128, 128), 1.0 / 1024))
    bd = np.zeros((128, 128))
    bd[:64, :64] = 1.0 / 64
    bd[64:, 64:] = 1.0 / 64
    add("bd64", bd)
    bdo = np.zeros((128, 128))
    bdo[:64, :64] = 1.0
    bdo[64:, 64:] = 1.0
    add("bd64one", bdo)
    add("ones", np.ones((128, 128)))
    cv = np.zeros((128, 8))
    cv[:, 0] = EPS
    cv[:, 1] = 1.0
    cv[0:4, 2] = 1.0
    cv[4:8, 3] = 1.0
    add("cvec", cv)
    gm = np.zeros((128, 512))
    tri = np.tril(np.ones((64, 64)))
    for h in range(4):
        gm[0:64, h * 64:(h + 1) * 64] = tri.T
        gm[0:64, 256 + h * 64:256 + (h + 1) * 64] = tri
    add("gla_mask", gm)
    sel8 = np.zeros((128, 512))
    for dh in range(8):
        sel8[dh, dh * 64:(dh + 1) * 64] = 1.0
    add("sel8", sel8)
    selp = np.zeros((128, 512))
    for d in range(2):
        for hc in range(2):
            o = (d * 2 + hc) * 128
            selp[d * 4 + 2 * hc, o:o + 64] = 1.0
            selp[d * 4 + 2 * hc + 1, o + 64:o + 128] = 1.0
    add("selp", selp)
    lo = np.tril(np.ones((64, 64)), -1)
    up = np.triu(np.ones((64, 64)), 1)
    ms = np.zeros((128, 512)); nms = np.zeros((128, 512)); qi = np.zeros((128, 512)); id4 = np.zeros((128, 256))
    for h in range(4):
        ms[0:64, h * 64:(h + 1) * 64] = lo
        ms[0:64, 256 + h * 64:256 + (h + 1) * 64] = up
        nms[0:64, h * 64:(h + 1) * 64] = -up
        nms[0:64, 256 + h * 64:256 + (h + 1) * 64] = -lo
        qi[0:64, h * 64:(h + 1) * 64] = up + np.eye(64)
        qi[0:64, 256 + h * 64:256 + (h + 1) * 64] = lo + np.eye(64)
        id4[0:64, h * 64:(h + 1) * 64] = np.eye(64)
    add("gdn_ms", ms)
    add("gdn_nms", nms)
    add("gdn_qi", qi)
    add("ident4", id4)
    hm32 = np.zeros((128, 4)); sblk = np.zeros((128, 256)); hm64 = np.zeros((128, 2)); wmask = np.zeros((128, 256))
    for h in range(4):
        hm32[h * 32:(h + 1) * 32, h] = 1.0
        sblk[h * 32:(h + 1) * 32, h * 64:(h + 1) * 64] = 1.0
        wmask[(h % 2) * 64:(h % 2) * 64 + 64, h * 64:(h + 1) * 64] = 1.0
    hm64[0:64, 0] = 1.0
    hm64[64:128, 1] = 1.0
    add("hmask32", hm32)
    add("sblk", sblk)
    add("hm64", hm64)
    add("wmask", wmask)
    return np.concatenate(cols, axis=1), np.concatenate(mcols, axis=1)


CONSTS, MCONSTS = _build_consts()
NMCONST = MCONSTS.shape[1]
CMASK = np.ones((128, T), np.float32)
CMASK[:, ::64] = 0.0
NCONST = CONSTS.shape[1]


def _rope_tables():
    t = np.arange(TL)
    row = (t // 64).astype(np.float32)
    col = (t % 64).astype(np.float32)
    inv = (np.float32(10000.0) ** (-np.arange(16, dtype=np.float32) / np.float32(16))).astype(np.float32)
    ang_r = row[:, None] * inv
    ang_c = col[:, None] * inv
    C = np.zeros((64, TL), np.float32)
    Sg = np.zeros((64, TL), np.float32)
    P = np.zeros((64, 64), np.float32)
    for m in range(64):
        d = m % 64
        ang = ang_r if d < 32 else ang_c
        f = d % 16
        first = (d % 32) < 16
        C[m] = np.cos(ang[:, f])
        Sg[m] = (-1.0 if first else 1.0) * np.sin(ang[:, f])
        src = m + 16 if first else m - 16
        P[src, m] = 1.0
    return np.concatenate([C, Sg, P], axis=1)


ROPE = _rope_tables()


def _rs(r):
    return min(max(r - 4, 0), 24)


def _na_patterns():
    pats = {}
    table = {}
    for t in range(16):
        r0, r1 = 2 * t, 2 * t + 1
        for j in range(_rs(r0) // 2, (_rs(r1) + 7) // 2 + 1):
            key = tuple((2 * j + a - (2 * t + bq), _rs(2 * t + bq) <= 2 * j + a < _rs(2 * t + bq) + 8) for a in (0, 1) for bq in (0, 1))
            if not any(v for _, v in key):
                continue
            table[(t, j)] = pats.setdefault(key, len(pats))
    return pats, table


NA_PATS, NA_TABLE = _na_patterns()
NPAT = len(NA_PATS)


def _na_bias(rpb):
    L = rpb.shape[0]
    out = np.full((L, NPAT, 128, 4, 128), NEG, np.float32)
    qc = np.arange(64)
    kc = np.arange(64)
    cs = np.clip(qc - 8, 0, 48)
    col_ok = (kc[None, :] >= cs[:, None]) & (kc[None, :] < cs[:, None] + 16)
    dc = np.clip(kc[None, :] - qc[:, None] + 15, 0, 30)
    for key, idx in NA_PATS.items():
        n = 0
        for a in (0, 1):
            for bq in (0, 1):
                dr, valid = key[n]
                n += 1
                if not valid:
                    continue
                blk = rpb[:, :, dr + 7, :][:, :, dc]
                blk = np.where(col_ok[None, None], blk, np.float32(NEG))
                out[:, idx, a * 64:(a + 1) * 64, :, bq * 64:(bq + 1) * 64] = np.transpose(blk, (0, 3, 1, 2))
    return out


PV = {}


def _pv_layout():
    off = 0
    for name, n in [("b_ada", 48), ("ln1_g", 8), ("ln1_b", 8), ("ln2_g", 8), ("ln2_b", 8), ("a_qg", 1), ("a_kg", 1), ("c_og", 1), ("d_og", 1), ("d_gb", 2), ("c_conv", 18), ("c_dtb", 1), ("c_alog", 1)]:
        PV[name] = (off, n)
        off += n
    return off


NPV = _pv_layout()


def _fm(v):
    return np.ascontiguousarray(np.asarray(v, np.float32).reshape(-1, 128).T)


class Kern:
    def __init__(self, nc, dbg=None):
        self.nc = nc
        self.S = Sched(nc)
        self.dbg = dbg or {}
        self.es = ExitStack()
        self.nps = 0

    def sb(self, es, name, shape, dt=F32):
        self.nsb = getattr(self, "nsb", 0) + 1
        return es.enter_context(self.nc.sbuf_tensor("%s_%d" % (name, self.nsb), shape, dt))

    def psum_chain(self, d):
        self.npc = getattr(self, "npc", [0, 0])
        i = d * 4 + self.npc[d] % 4
        self.npc[d] += 1
        return self.ps[i], self.rps[i]

    def psum(self):
        i = self.nps % 8
        self.nps += 1
        return self.ps[i], self.rps[i]

    def dump(self, name, ap, shape, reads):
        if not self.dbg.get("dump"):
            return
        d = self.nc.dram_tensor("dump_" + name, list(shape), F32, kind="ExternalOutput").ap()
        self.S.barrier()
        self.S.dma(d, ap, reads=reads)
        self.S.barrier()

    def cst(self, name, rows=128):
        w, o, n = CONST_COLS[name]
        return (self.mconsts if w else self.consts)[0:rows, o:o + n]

    def load_mconsts(self, es):
        self.mconsts = self.sb(es, "mconsts", [128, NMCONST])
        self.S.dma(self.mconsts[:], self.A["mconsts"][:, :], writes=[self.rconst])

    def evac(self, out, in_, reads, writes, i):
        if i % 2 == 0:
            self.S.op("act", lambda e: e.activation(out=out, in_=in_, func=AF.Copy), reads, writes)
        else:
            self.S.op("dve", lambda e: e.tensor_copy(out=out, in_=in_), reads, writes)

    def build(self):
        nc, S = self.nc, self.S
        dt = nc.dram_tensor
        A = {}
        A["xT"] = dt("xT_in", [2, D, TL], F32, kind="ExternalInput").ap()
        A["ctxT"] = dt("ctxT_in", [2, D, TC], F32, kind="ExternalInput").ap()
        A["cc"] = dt("cc", [D, 3], F32, kind="ExternalInput").ap()
        A["consts"] = dt("consts", [128, NCONST], F32, kind="ExternalInput").ap()
        A["rope"] = dt("rope", [64, 2 * TL + 64], F32, kind="ExternalInput").ap()
        A["nab"] = dt("nab", [DEPTH, NPAT, 128, 4, 128], F32, kind="ExternalInput").ap()
        A["mconsts"] = dt("mconsts", [128, NMCONST], F32, kind="ExternalInput").ap()
        A["pvec"] = dt("pvec", [DEPTH, 128, NPV], F32, kind="ExternalInput").ap()
        A["w_ada"] = dt("w_ada", [DEPTH, D, 6 * D], F32, kind="ExternalInput").ap()
        A["w_in"] = dt("w_in", [DEPTH if self.dbg.get("do_proj", True) else 1, D, INW], F32, kind="ExternalInput").ap()
        A["w_branch"] = dt("w_branch", [DEPTH, 4, 256, D], F32, kind="ExternalInput").ap()
        A["w_out"] = dt("w_out", [DEPTH, D, D], F32, kind="ExternalInput").ap()
        A["w_rt"] = dt("w_rt", [DEPTH, D, 36], F32, kind="ExternalInput").ap()
        A["b_rt"] = dt("b_rt", [DEPTH, 36], F32, kind="ExternalInput").ap()
        nlw = DEPTH if self.dbg.get("do_moe", True) else 1
        nex = self.dbg.get("nexp", 32) if self.dbg.get("do_moe", True) else 1
        A["w_up"] = dt("w_up", [nlw, nex, D, 512], F32, kind="ExternalInput").ap()
        A["w_gate"] = dt("w_gate", [nlw, nex, D, 512], F32, kind="ExternalInput").ap()
        A["w_down"] = dt("w_down", [nlw, nex, 512, D], F32, kind="ExternalInput").ap()
        A["out"] = dt("outT", [2, D, TL], F32, kind="ExternalOutput").ap()
        A["projT"] = dt("projT", [INW, T], F32, kind=self.dbg.get("proj_kind", "Internal")).ap()
        A["oT"] = dt("oT", [D, T], F32, kind=self.dbg.get("oT_kind", "Internal")).ap()
        self.A = A
        A["xsave"] = dt("xsave", [D, T], F32, kind="Internal").ap()
        self.rxsave = Res()
        A["cmask"] = dt("cmask", [128, T], F32, kind="ExternalInput").ap()
        A["d_gw"] = dt("d_gw", [DEPTH, 2, 16, 128], F32, kind="ExternalInput").ap()
        self.rprojT = Res()
        self.roT = Res()

        es = self.es
        self.ps = [nc.alloc_psum_tensor("ps%d" % i, [128, 512], F32) for i in range(8)]
        self.rps = [Res() for _ in range(8)]
        self.consts = self.sb(es, "consts", [128, NCONST])
        self.rconst = Res()
        S.dma(self.consts[:], A["consts"][:, :], writes=[self.rconst])
        self.pvec = self.sb(es, "pvec", [128, DEPTH, NPV])
        for l in range(DEPTH):
            S.dma(self.pvec[:, l, :], A["pvec"][l], writes=[self.rconst])
        self.modT = self.sb(es, "modT", [128, DEPTH, 48, 3])
        self.rmod = Res()
        self.x_es = ExitStack()
        self.xT = self.sb(self.x_es, "xT", [128, 8, T])
        self.rx = [[Res() for _ in range(5)] for _ in range(8)]

        self.phase_mods()
        S.barrier()
        outs = []
        nitems = self.dbg.get("nitems", 2)
        nlayers = self.dbg.get("nlayers", DEPTH)
        for b in range(nitems):
            self.load_x(b)
            for l in range(nlayers):
                if self.dbg.get("do_proj", True):
                    self.phase_proj(b, l)
                    S.barrier()
                if self.dbg.get("do_mix", True):
                    if not self.dbg.get("nospill"):
                        self.spill_x()
                    self.phase_mixers(b, l)
                    S.barrier()
                    if not self.dbg.get("nospill"):
                        self.restore_x()
                if self.dbg.get("do_merge", True):
                    self.phase_merge(b, l)
                    S.barrier()
                if self.dbg.get("do_moe", True):
                    self.phase_moe(b, l)
                    S.barrier()
            for k in range(8):
                outs.append(S.dma(A["out"][b, k * 128:(k + 1) * 128, :], self.xT[:, k, 0:TL],
                                  reads=[self.rx[k][j] for j in range(4)], eng="sp"))
            S.barrier()
        S.finish(outs)
        self.x_es.close()
        self.es.close()

    def rxs(self, k, t0, n):
        return [self.rx[k][j] for j in range(t0 // 512, (t0 + n - 1) // 512 + 1)]

    def spill_x(self):
        for k in range(8):
            self.S.dma(self.A["xsave"][k * 128:(k + 1) * 128, :], self.xT[:, k, :], reads=self.rx[k], writes=[self.rxsave])
        self.S.barrier()
        self.x_es.close()

    def restore_x(self):
        self.x_es = ExitStack()
        self.xT = self.sb(self.x_es, "xT", [128, 8, T])
        for k in range(8):
            self.S.dma(self.xT[:, k, :], self.A["xsave"][k * 128:(k + 1) * 128, :], reads=[self.rxsave], writes=self.rx[k])

    def load_x(self, b):
        S, A = self.S, self.A
        for k in range(8):
            S.dma(self.xT[:, k, 0:TL], A["xT"][b, k * 128:(k + 1) * 128, :], writes=self.rx[k][0:4])
            S.dma(self.xT[:, k, TL:T], A["ctxT"][b, k * 128:(k + 1) * 128, :], writes=[self.rx[k][4]])

    def phase_mods(self):
        nc, S, A = self.nc, self.S, self.A
        with ExitStack() as es:
            scT = self.sb(es, "scT", [128, 8, 3])
            rsc = Res()
            S.dma(scT[:], A["cc"].rearrange("(k p) j -> p k j", p=128), writes=[rsc])
            S.op("act", lambda e: e.activation(out=scT[:], in_=scT[:], func=AF.Silu), [rsc], [rsc])
            wt = [self.sb(es, "wada%d" % i, [128, 8, 128]) for i in range(3)]
            rw = [Res() for _ in range(3)]
            n = 0
            for l in range(DEPTH):
                bo = PV["b_ada"][0]
                for c in range(48):
                    w, r = wt[n % 3], rw[n % 3]
                    n += 1
                    S.dma(w[:], A["w_ada"][l, :, c * 128:(c + 1) * 128].rearrange("(k p) c -> p k c", p=128), writes=[r])
                    ps, rp = self.psum()
                    for k in range(8):
                        S.op("pe", lambda e, w=w, ps=ps, k=k: e.matmul(ps[:, 0:3], w[:, k, :], scT[:, k, :], start=(k == 0), stop=(k == 7)),
                             [r, rsc], [rp])
                    S.op("dve", lambda e, ps=ps, l=l, c=c: e.tensor_scalar(out=self.modT[:, l, c, :], in0=ps[:, 0:3],
                                                                         scalar1=self.pvec[:, l, bo + c:bo + c + 1], scalar2=None, op0=ALU.add),
                         [rp, self.rconst], [self.rmod])
                for c0 in (8, 32):
                    S.op("dve", lambda e, l=l, c0=c0: e.tensor_scalar(out=self.modT[:, l, c0:c0 + 8, :], in0=self.modT[:, l, c0:c0 + 8, :],
                                                                    scalar1=1.0, scalar2=None, op0=ALU.add), [self.rmod], [self.rmod])

    def mod(self, l, grp, k, col):
        return self.modT[:, l, grp * 8 + k, col:col + 1]

    def phase_proj(self, b, l):
        nc, S, A = self.nc, self.S, self.A
        groups = [(0, 256), (256, 128), (384, 128), (512, 256), (768, 256), (1024, 256), (1280, 768), (2048, 16), (2064, 256),
                  (2320, 128), (2448, 128), (2576, 256), (2832, 32), (2864, 256), (3120, 4096)]
        chunks = []
        for (o, n) in groups:
            for c in range(0, n, 128):
                chunks.append((o + c, min(128, n - c)))
        with ExitStack() as es:
            hT = self.sb(es, "hT", [128, 8, T], BF16)
            rh = Res()
            for k in range(8):
                S.op("dve", lambda e, k=k: e.tensor_scalar(out=hT[:, k, 0:TL], in0=self.xT[:, k, 0:TL], scalar1=self.mod(l, 1, k, b),
                                                         scalar2=self.mod(l, 0, k, b), op0=ALU.mult, op1=ALU.add),
                     [self.rmod] + self.rx[k][0:4], [rh])
                S.op("pool", lambda e, k=k: e.tensor_scalar(out=hT[:, k, TL:T], in0=self.xT[:, k, TL:T], scalar1=self.mod(l, 1, k, 2),
                                                          scalar2=self.mod(l, 0, k, 2), op0=ALU.mult, op1=ALU.add),
                     [self.rmod, self.rx[k][4]], [rh])
            NW = 3
            wst = [self.sb(es, "wins%d" % i, [128, 8, 128]) for i in range(NW)]
            rwst = [Res() for _ in range(NW)]
            wt = [self.sb(es, "win%d" % i, [128, 8, 128], BF16) for i in range(NW)]
            rw = [Res() for _ in range(NW)]
            NST = 4
            st = [self.sb(es, "pst%d" % i, [128, 512]) for i in range(NST)]
            rst = [Res() for _ in range(NST)]
            ns = 0
            for ci, (c0, cn) in enumerate(chunks):
                w, r = wt[ci % NW], rw[ci % NW]
                ws_, rws = wst[ci % NW], rwst[ci % NW]
                S.dma(ws_[:, :, 0:cn], A["w_in"][l, :, c0:c0 + cn].rearrange("(k p) c -> p k c", p=128), writes=[rws])
                S.op("pool", lambda e: e.tensor_copy(out=w[:, :, 0:cn], in_=ws_[:, :, 0:cn]), [rws], [r])
                for tb in range(5):
                    t0 = tb * 512
                    tn = min(512, T - t0)
                    ps, rp = self.psum()
                    for k in range(8):
                        S.op("pe", lambda e, w=w, ps=ps, k=k, cn=cn, t0=t0, tn=tn: e.matmul(ps[0:cn, 0:tn], w[:, k, 0:cn], hT[:, k, t0:t0 + tn],
                                                                                       start=(k == 0), stop=(k == 7)), [r, rh], [rp])
                    s_, rs_ = st[ns % NST], rst[ns % NST]
                    self.evac(s_[0:cn, 0:tn], ps[0:cn, 0:tn], [rp], [rs_], ns)
                    ns += 1
                    S.dma(A["projT"][c0:c0 + cn, t0:t0 + tn], s_[0:cn, 0:tn], reads=[rs_], writes=[self.rprojT], eng="pool")

    def phase_mixers(self, b, l):
        which = self.dbg.get("mixers", "abcd")
        if "a" in which:
            self.mixer_a(b, l)
            self.S.barrier()
        if "b" in which:
            self.mixer_b(b, l)
            self.S.barrier()
        if "d" in which:
            self.mixer_d(b, l)
            self.S.barrier()
        if "c" in which:
            self.mixer_c(b, l)
            self.S.barrier()

    def load_rows(self, es, name, r0, nch):
        t = self.sb(es, name, [128, nch, T])
        r = Res()
        self.S.dma(t[:], self.A["projT"][r0:r0 + 128 * nch, :].rearrange("(c p) t -> p c t", p=128), reads=[self.rprojT], writes=[r])
        return t, r

    def load_heads(self, es, name, r0, nh):
        t = self.sb(es, name, [64, nh, T])
        r = Res()
        self.S.dma(t[:], self.A["projT"][r0:r0 + 64 * nh, :].rearrange("(h p) t -> p h t", p=64), reads=[self.rprojT], writes=[r])
        return t, r

    def headnorm(self, X, rX, c, tmps, mat, gain, P=128):
        S = self.S
        (s1, r1), (s2, r2) = tmps
        for t0 in range(0, T, 512):
            tn = min(512, T - t0)
            xs = X[0:P, c, t0:t0 + tn]
            S.op("pool", lambda e: e.tensor_tensor(out=s1[0:P, 0:tn], in0=xs, in1=xs, op=ALU.mult), [rX], [r1])
            ps, rp = self.psum()
            S.op("pe", lambda e: e.matmul(ps[0:P, 0:tn], mat[0:P, 0:P], s1[0:P, 0:tn], start=True, stop=True), [r1, self.rconst], [rp])
            S.op("dve", lambda e: e.tensor_scalar(out=s2[0:P, 0:tn], in0=ps[0:P, 0:tn], scalar1=EPS, scalar2=None, op0=ALU.add), [rp], [r2])
            S.op("act", lambda e: e.activation(out=s2[0:P, 0:tn], in_=s2[0:P, 0:tn], func=AF.Sqrt), [r2], [r2])
            S.op("dve", lambda e: e.reciprocal(out=s2[0:P, 0:tn], in_=s2[0:P, 0:tn]), [r2], [r2])
            S.op("dve", lambda e: e.scalar_tensor_tensor(out=xs, in0=xs, scalar=gain, in1=s2[0:P, 0:tn], op0=ALU.mult, op1=ALU.mult),
                 [r2, self.rconst, rX], [rX])

    def rope(self, X, rX, c, tmps, ropet, rrope):
        S = self.S
        (s1, r1), (s2, r2) = tmps
        for t0 in range(0, TL, 512):
            xs = X[0:64, c, t0:t0 + 512]
            ps, rp = self.psum()
            S.op("pe", lambda e: e.matmul(ps[0:64, 0:512], ropet[0:64, 2 * TL:2 * TL + 64], xs, start=True, stop=True), [rX, rrope], [rp])
            S.op("pool", lambda e: e.tensor_tensor(out=s1[0:64, 0:512], in0=xs, in1=ropet[0:64, t0:t0 + 512], op=ALU.mult), [rX, rrope], [r1])
            S.op("dve", lambda e: e.tensor_tensor(out=s2[0:64, 0:512], in0=ps[0:64, 0:512], in1=ropet[0:64, TL + t0:TL + t0 + 512], op=ALU.mult), [rp, rrope], [r2])
            S.op("pool", lambda e: e.tensor_tensor(out=xs, in0=s1[0:64, 0:512], in1=s2[0:64, 0:512], op=ALU.add), [r1, r2, rX], [rX])

    def build_vaug(self, Vaug, vT, rv, H):
        S = self.S
        rV = Res()
        S.op("pool", lambda e: e.memset(Vaug[:, :, :, 64:128], 1.0), [], [rV])
        n = 0
        for h in range(H):
            for j in range(18):
                ps, rp = self.psum()
                S.op("pe", lambda e: e.transpose(ps[:, 0:64], vT[0:64, h, j * 128:(j + 1) * 128], self.cst("ident")[0:64, 0:64]), [rv, self.rconst], [rp])
                self.evac(Vaug[:, j, h, 0:64], ps[:, 0:64], [rp], [rV], n)
                n += 1
        return Vaug, rV

    def attention(self, es, l, qT, rq, kfun, rk, Vaug, rV, hv, keylist, zrow):
        S, A = self.S, self.A
        H = 4
        ident = self.cst("ident")
        OUT = self.sb(es, "attn_out", [64, 4, T])
        rOUT = Res()
        PT = [self.sb(es, "PT%d" % i, [128, 512]) for i in range(3)]
        rPT = [Res() for _ in range(3)]
        BT = [self.sb(es, "BT%d" % i, [128, 512]) for i in range(3)]
        rBT = [Res() for _ in range(3)]
        Rr = [self.sb(es, "Rr%d" % i, [64, 128]) for i in range(2)]
        rRr = [Res() for _ in range(2)]
        n = 0
        nr = 0
        for t in range(18):
            keys = keylist(t)
            for idx, (j, pat) in enumerate(keys):
                sp, rsp = self.ps[4 + n % 4], self.rps[4 + n % 4]
                pt, rpt = PT[n % 3], rPT[n % 3]
                if pat is not None:
                    bt, rbt = BT[n % 3], rBT[n % 3]
                    S.dma(bt[:], A["nab"][l, pat].rearrange("k h q -> k (h q)"), writes=[rbt])
                    S.op("pe", lambda e: e.matmul(sp[:, 0:512], ident, bt[:], start=True, stop=False), [rbt, self.rconst], [rsp])
                for h in range(H):
                    S.op("pe", lambda e: e.matmul(sp[:, h * 128:(h + 1) * 128], kfun(h, j), qT[0:64, h, t * 128:(t + 1) * 128],
                                                  start=(pat is None), stop=True), [rk, rq], [rsp])
                S.op("act", lambda e: e.activation(out=pt[:], in_=sp[:, 0:512], func=AF.Exp), [rsp], [rpt])
                for h in range(H):
                    S.op("pe", lambda e: e.matmul(self.ps[h][:, 0:128], Vaug[:, j, hv(h), :], pt[:, h * 128:(h + 1) * 128],
                                                  start=(idx == 0), stop=(idx == len(keys) - 1)), [rV, rpt], [self.rps[h]])
                n += 1
            for h in range(H):
                rr_, rrr = Rr[nr % 2], rRr[nr % 2]
                nr += 1
                S.op("dve", lambda e: e.reciprocal(out=rr_[0:64, :], in_=self.ps[h][64:128, 0:128]), [self.rps[h]], [rrr])
                S.op("dve", lambda e: e.tensor_tensor(out=OUT[0:64, h, t * 128:(t + 1) * 128], in0=self.ps[h][0:64, 0:128], in1=rr_[0:64, :], op=ALU.mult),
                     [self.rps[h], rrr], [rOUT])
        S.dma(A["oT"][zrow:zrow + 256, :].rearrange("(h p) t -> p h t", p=64), OUT[:], reads=[rOUT], writes=[self.roT], eng="pool")

    def mixer_a(self, b, l):
        S, A = self.S, self.A
        with ExitStack() as es:
            qT, rq = self.load_heads(es, "aq", 0, 4)
            kT, rk = self.load_heads(es, "ak", 256, 2)
            ropet = self.sb(es, "ropet", [64, 2 * TL + 64])
            rrope = Res()
            S.dma(ropet[:], A["rope"][:, :], writes=[rrope])
            Vaug = self.sb(es, "avaug", [128, 18, 2, 128])
            with ExitStack() as es2:
                vT, rv = self.load_heads(es2, "av", 384, 2)
                Vaug, rV = self.build_vaug(Vaug, vT, rv, 2)
                tmps = [(self.sb(es2, "nt%d" % i, [64, 512]), Res()) for i in range(2)]
                bd64 = self.cst("bd64")
                for h in range(4):
                    self.headnorm(qT, rq, h, tmps, bd64, self.pvec[0:64, l, PV["a_qg"][0]:PV["a_qg"][0] + 1], P=64)
                    self.rope(qT, rq, h, tmps, ropet, rrope)
                    S.op("pool", lambda e: e.tensor_scalar(out=qT[:, h, :], in0=qT[:, h, :], scalar1=0.125, scalar2=None, op0=ALU.mult), [rq], [rq])
                for h in range(2):
                    self.headnorm(kT, rk, h, tmps, bd64, self.pvec[0:64, l, PV["a_kg"][0]:PV["a_kg"][0] + 1], P=64)
                    self.rope(kT, rk, h, tmps, ropet, rrope)
                self.S.barrier()
            kfun = lambda h, j: kT[0:64, h // 2, j * 128:(j + 1) * 128]
            keylist = lambda t: [(j, None) for j in (range(18) if t < 16 else (16, 17))]
            self.attention(es, l, qT, rq, kfun, rk, Vaug, rV, lambda h: h // 2, keylist, 0)

    def mixer_b(self, b, l):
        S, A = self.S, self.A
        with ExitStack() as es:
            qT, rq = self.load_heads(es, "bq", 512, 4)
            kT, rk = self.load_heads(es, "bk", 768, 4)
            for h in range(4):
                S.op("pool", lambda e: e.tensor_scalar(out=qT[:, h, :], in0=qT[:, h, :], scalar1=0.125, scalar2=None, op0=ALU.mult), [rq], [rq])
            Vaug = self.sb(es, "bvaug", [128, 18, 4, 128])
            with ExitStack() as es2:
                vT, rv = self.load_heads(es2, "bv", 1024, 4)
                Vaug, rV = self.build_vaug(Vaug, vT, rv, 4)
                self.S.barrier()
            kfun = lambda h, j: kT[0:64, h, j * 128:(j + 1) * 128]

            def keylist(t):
                if t >= 16:
                    return [(16, None), (17, None)]
                ks = [(j, NA_TABLE[(t, j)]) for j in range(16) if (t, j) in NA_TABLE]
                return ks + [(16, None), (17, None)]

            self.attention(es, l, qT, rq, kfun, rk, Vaug, rV, lambda h: h, keylist, 256)

    def mixer_c(self, b, l):
        S, A = self.S, self.A
        ident = self.cst("ident")
        cvec = self.cst("cvec")
        with ExitStack() as eso:
            OT = self.sb(eso, "cOT", [128, 2, T])
            rOT = Res()
            es = ExitStack()
            self.load_mconsts(es)
            sel8 = self.cst("sel8")
            selp = self.cst("selp")
            QKV, rqkv = self.load_rows(es, "cqkv", 1280, 6)
            with ExitStack() as es2:
                Y = self.sb(es2, "cY", [128, T])
                rY = Res()
                tmps = [(self.sb(es2, "cnt%d" % i, [128, 512]), Res()) for i in range(2)]
                co = PV["c_conv"][0]
                for ch in range(6):
                    w = lambda tap: self.pvec[:, l, co + ch * 3 + tap:co + ch * 3 + tap + 1]
                    X = QKV[:, ch, :]
                    S.op("dve", lambda e: e.tensor_scalar(out=Y[:], in0=X, scalar1=w(1), scalar2=None, op0=ALU.mult), [rqkv, self.rconst], [rY])
                    for (a, n) in ((0, TL), (TL, TC)):
                        S.op("dve", lambda e: e.scalar_tensor_tensor(out=Y[:, a + 1:a + n], in0=QKV[:, ch, a:a + n - 1], scalar=w(0), in1=Y[:, a + 1:a + n],
                                                                     op0=ALU.mult, op1=ALU.add), [rqkv, rY, self.rconst], [rY])
                        S.op("dve", lambda e: e.scalar_tensor_tensor(out=Y[:, a:a + n - 1], in0=QKV[:, ch, a + 1:a + n], scalar=w(2), in1=Y[:, a:a + n - 1],
                                                                     op0=ALU.mult, op1=ALU.add), [rqkv, rY, self.rconst], [rY])
                    S.op("act", lambda e: e.activation(out=QKV[:, ch, :], in_=Y[:], func=AF.Silu), [rY, rqkv], [rqkv])
                for ch in range(4):
                    self.headnorm(QKV, rqkv, ch, tmps, self.cst("bd64one"), 0.125 if ch < 2 else 1.0)
                S.barrier()
            B8 = self.sb(es, "cB8", [8, T])
            G8 = self.sb(es, "cG8", [8, T])
            TOT8 = self.sb(es, "cTOT8", [8, 36])
            ETOT8 = self.sb(es, "cETOT8", [8, 36])
            nal = self.sb(es, "cnal", [8, 1])
            TOK = self.sb(es, "cTOK", [64, 36, 48])
            ETOTP = self.sb(es, "cETOTP", [128, 4, 36])
            es3 = ExitStack()
            cm = self.sb(es3, "ccm", [8, T])
            rcm = Res()
            S.dma(cm[:], A["cmask"][0:8, :], writes=[rcm])
            A8 = self.sb(es3, "cA8", [8, T])
            X8 = self.sb(es3, "cX8", [8, T])
            STK = self.sb(es3, "cSTK", [48, T])
            r8 = Res()
            S.dma(B8[:], A["projT"][2048:2056, :], reads=[self.rprojT], writes=[r8])
            S.dma(A8[:], A["projT"][2056:2064, :], reads=[self.rprojT], writes=[r8])
            dtb = self.pvec[0:8, l, PV["c_dtb"][0]:PV["c_dtb"][0] + 1]
            alog = self.pvec[0:8, l, PV["c_alog"][0]:PV["c_alog"][0] + 1]
            o8 = lambda eng, fn: S.op(eng, fn, [r8, self.rconst, rcm], [r8])
            o8("act", lambda e: e.activation(out=B8[:], in_=B8[:], func=AF.Sigmoid))
            o8("act", lambda e: e.activation(out=nal[:], in_=alog, func=AF.Exp))
            o8("dve", lambda e: e.tensor_scalar(out=nal[:], in0=nal[:], scalar1=-1.0, scalar2=None, op0=ALU.mult))
            o8("dve", lambda e: e.tensor_scalar(out=A8[:], in0=A8[:], scalar1=dtb, scalar2=None, op0=ALU.add))
            o8("act", lambda e: e.activation(out=A8[:], in_=A8[:], func=AF.Exp))
            o8("dve", lambda e: e.tensor_scalar(out=A8[:], in0=A8[:], scalar1=1.0, scalar2=None, op0=ALU.add))
            o8("act", lambda e: e.activation(out=A8[:], in_=A8[:], func=AF.Ln))
            o8("dve", lambda e: e.tensor_scalar(out=A8[:], in0=A8[:], scalar1=nal[:, 0:1], scalar2=None, op0=ALU.mult))
            o8("dve", lambda e: e.tensor_tensor_scan(out=G8[:], data0=cm[:], data1=A8[:], initial=0.0, op0=ALU.mult, op1=ALU.add))
            o8("dve", lambda e: e.tensor_reduce(out=TOT8[:], in_=A8[:].rearrange("p (c i) -> p c i", i=64), axis=AX.X, op=ALU.add))
            o8("dve", lambda e: e.tensor_tensor(out=X8[:], in0=A8[:], in1=G8[:], op=ALU.subtract))
            for c in range(36):
                o8("dve", lambda e: e.tensor_scalar(out=X8[:, c * 64:(c + 1) * 64], in0=X8[:, c * 64:(c + 1) * 64], scalar1=TOT8[:, c:c + 1], scalar2=None, op0=ALU.add))
            o8("dve", lambda e: e.tensor_scalar(out=G8[:], in0=G8[:], scalar1=cvec[0:8, 2:3], scalar2=None, op0=ALU.mult))
            o8("dve", lambda e: e.scalar_tensor_tensor(out=G8[:], in0=X8[:], scalar=cvec[0:8, 3:4], in1=G8[:], op0=ALU.mult, op1=ALU.add))
            o8("act", lambda e: e.activation(out=ETOT8[:], in_=TOT8[:], func=AF.Exp))
            o8("act", lambda e: e.activation(out=A8[:], in_=G8[:], func=AF.Exp))
            S.dma(STK[0:8, :], G8[:], reads=[r8], writes=[r8])
            S.dma(STK[24:32, :], B8[:], reads=[r8], writes=[r8])
            S.dma(STK[32:40, :], A8[:], reads=[r8], writes=[r8])
            o8("dve", lambda e: e.tensor_scalar(out=X8[:], in0=B8[:], scalar1=-1.0, scalar2=None, op0=ALU.mult))
            S.dma(STK[8:16, :], X8[:], reads=[r8], writes=[r8])
            o8("dve", lambda e: e.tensor_tensor(out=X8[:], in0=B8[:], in1=A8[:], op=ALU.mult))
            S.dma(STK[16:24, :], X8[:], reads=[r8], writes=[r8])
            for c in range(36):
                o8("dve", lambda e: e.tensor_scalar(out=X8[:, c * 64:(c + 1) * 64], in0=G8[:, c * 64:(c + 1) * 64], scalar1=TOT8[:, c:c + 1], scalar2=None, op0=ALU.subtract))
            o8("act", lambda e: e.activation(out=X8[:], in_=X8[:], func=AF.Exp, scale=-1.0))
            S.dma(STK[40:48, :], X8[:], reads=[r8], writes=[r8])
            rtok = Res()
            for c in range(36):
                ps, rp = self.psum()
                S.op("pe", lambda e: e.transpose(ps[0:64, 0:48], STK[0:48, c * 64:(c + 1) * 64], ident[0:48, 0:48]), [r8, self.rconst], [rp])
                self.evac(TOK[:, c, :], ps[0:64, 0:48], [rp], [rtok], c)
            for i in range(4):
                ps, rp = self.psum()
                S.op("pe", lambda e: e.matmul(ps[:, 0:36], selp[0:8, i * 128:(i + 1) * 128], ETOT8[:], start=True, stop=True), [r8, self.rconst], [rp])
                S.op("act", lambda e: e.activation(out=ETOTP[:, i, :], in_=ps[:, 0:36], func=AF.Copy), [rp], [rtok])
            S.barrier()
            es3.close()
            Otok = self.sb(es, "cOtok", [64, 36, 256])
            rOtok = [Res() for _ in range(36)]
            mk = lambda nm, shape=(64, 256): (self.sb(es, nm, list(shape)), Res())
            def mktiles():
                t = {}
                t['Ktok, rKt'] = mk("cKtok")
                t['Vtok, rVt'] = mk("cVtok")
                t['Xt, rXt'] = mk("cXt")
                t['Xp, rXp'] = mk("cXp")
                t['Dt, rDt'] = mk("cDt")
                t['DTt, rDTt'] = mk("cDTt")
                t['Mt'] = [mk("cM%d" % i) for i in range(2)]
                t['Nt'] = [mk("cN%d" % i) for i in range(2)]
                t['Pt, rPt'] = mk("cP")
                t['qkT, rqk'] = mk("cqkT")
                t['rhsm, rrh'] = mk("crhs", (64, 512))
                t['wT, rwT'] = mk("cwT", (128, 256))
                t['u0, ru0'] = mk("cu0")
                t['ut, rut'] = mk("cu")
                t['o2, ro2'] = mk("co2")
                t['otmp, rotmp'] = mk("cotmp")
                t['Kd, rKd'] = mk("cKd")
                t['KTm, rKTm'] = mk("cKTm", (128, 256))
                t['QTm, rQTm'] = mk("cQTm", (128, 256))
                return t
            TD = [mktiles() for _ in range(2)]
            Sts = [self.sb(es, "cS%d" % i, [128, 2, 128]) for i in range(2)]
            rSs = [Res() for _ in range(2)]
            hm64 = self.cst("hm64")
            wmask = self.cst("wmask")
            HB = lambda h: slice(h * 64, (h + 1) * 64)
            id4 = self.cst("ident4")[0:64, :]
            S.op("pool", lambda e: e.memset(Otok[:], 0.0), [], rOtok)
            for d in range(2):
                S.op("pool", lambda e: e.memset(Sts[d][:], 0.0), [], [rSs[d]])

            def step(d, c):
                t = TD[d]
                Ktok, rKt = t['Ktok, rKt']
                Vtok, rVt = t['Vtok, rVt']
                Xt, rXt = t['Xt, rXt']
                Xp, rXp = t['Xp, rXp']
                Dt, rDt = t['Dt, rDt']
                DTt, rDTt = t['DTt, rDTt']
                Mt = t['Mt']
                Nt = t['Nt']
                Pt, rPt = t['Pt, rPt']
                qkT, rqk = t['qkT, rqk']
                rhsm, rrh = t['rhsm, rrh']
                wT, rwT = t['wT, rwT']
                u0, ru0 = t['u0, ru0']
                ut, rut = t['ut, rut']
                o2, ro2 = t['o2, ro2']
                otmp, rotmp = t['otmp, rotmp']
                Kd, rKd = t['Kd, rKd']
                KTm, rKTm = t['KTm, rKTm']
                QTm, rQTm = t['QTm, rQTm']
                St, rS = Sts[d], rSs[d]
                ms = self.cst("gdn_ms")[0:64, d * 256:(d + 1) * 256]
                nms = self.cst("gdn_nms")[0:64, d * 256:(d + 1) * 256]
                qi = self.cst("gdn_qi")[0:64, d * 256:(d + 1) * 256]
                cs = slice(c * 64, (c + 1) * 64)
                tk = lambda grp, h: TOK[:, c, grp * 8 + d * 4 + h:grp * 8 + d * 4 + h + 1]
                pk, rpk = self.psum_chain(d)
                pv, rpv = self.psum_chain(d)
                for hc in range(2):
                    S.op("pe", lambda e: e.transpose(pk[0:64, hc * 128:(hc + 1) * 128], QKV[:, 2 + hc, cs], ident), [rqkv, self.rconst], [rpk])
                    S.op("pe", lambda e: e.transpose(pv[0:64, hc * 128:(hc + 1) * 128], QKV[:, 4 + hc, cs], ident), [rqkv, self.rconst], [rpv])
                S.op("act", lambda e: e.activation(out=Ktok[:], in_=pk[0:64, 0:256], func=AF.Copy), [rpk], [rKt])
                S.op("dve", lambda e: e.tensor_copy(out=Vtok[:], in_=pv[0:64, 0:256]), [rpv], [rVt])
                yield
                pkk, rpkk = self.psum_chain(d)
                pqk, rpqk = self.psum_chain(d)
                pg, rpg = self.psum_chain(d)
                pb, rpb = self.psum_chain(d)
                for h in range(4):
                    S.op("pool", lambda e: e.tensor_scalar(out=KTm[:, HB(h)], in0=QKV[:, 2 + h // 2, cs], scalar1=hm64[:, h % 2:h % 2 + 1], scalar2=None, op0=ALU.mult),
                         [rqkv, self.rconst], [rKTm])
                    S.op("pool", lambda e: e.tensor_scalar(out=QTm[:, HB(h)], in0=QKV[:, h // 2, cs], scalar1=hm64[:, h % 2:h % 2 + 1], scalar2=None, op0=ALU.mult),
                         [rqkv, self.rconst], [rQTm])
                for h in range(4):
                    S.op("pe", lambda e: e.matmul(pkk[0:64, HB(h)], KTm[:, HB(h)], QKV[:, 2 + h // 2, cs], start=True, stop=True), [rqkv, rKTm], [rpkk])
                    S.op("pe", lambda e: e.matmul(pqk[0:64, HB(h)], KTm[:, HB(h)], QKV[:, h // 2, cs], start=True, stop=True), [rqkv, rKTm], [rpqk])
                    S.op("pe", lambda e: e.matmul(pg[0:64, HB(h)], sel8[0:8, HB(d * 4 + h)], G8[:, cs], start=True, stop=True), [r8, self.rconst], [rpg])
                    S.op("pe", lambda e: e.matmul(pb[0:64, HB(h)], sel8[0:8, HB(d * 4 + h)], B8[:, cs], start=True, stop=True), [r8, self.rconst], [rpb])
                yield
                for h in range(4):
                    S.op("dve", lambda e: e.tensor_scalar(out=Xt[:, HB(h)], in0=pg[0:64, HB(h)], scalar1=tk(0, h), scalar2=None, op0=ALU.subtract), [rpg, rtok], [rXt])
                S.op("dve", lambda e: e.tensor_scalar(out=Xp[:], in0=Xt[:], scalar1=0.0, scalar2=None, op0=ALU.max), [rXt], [rXp])
                S.op("pool", lambda e: e.tensor_tensor(out=Xt[:], in0=Xt[:], in1=Xp[:], op=ALU.subtract), [rXt, rXp], [rXt])
                yield
                S.op("act", lambda e: e.activation(out=Dt[:], in_=Xp[:], func=AF.Exp, scale=-1.0), [rXp], [rDt])
                S.op("act", lambda e: e.activation(out=DTt[:], in_=Xt[:], func=AF.Exp), [rXt], [rDTt])
                yield
                (M, rM), (N, rN) = Mt[0], Nt[0]
                S.op("dve", lambda e: e.tensor_tensor(out=Dt[:], in0=Dt[:], in1=pkk[0:64, 0:256], op=ALU.mult), [rDt, rpkk], [rDt])
                for h in range(4):
                    S.op("dve", lambda e: e.scalar_tensor_tensor(out=M[:, HB(h)], in0=Dt[:, HB(h)], scalar=tk(1, h), in1=ms[:, HB(h)], op0=ALU.mult, op1=ALU.mult),
                         [rDt, rtok, self.rconst], [rM])
                S.op("dve", lambda e: e.tensor_tensor(out=qkT[:], in0=DTt[:], in1=pqk[0:64, 0:256], op=ALU.mult), [rDTt, rpqk], [rqk])
                S.op("pool", lambda e: e.tensor_tensor(out=qkT[:], in0=qkT[:], in1=qi, op=ALU.mult), [rqk, self.rconst], [rqk])
                S.op("dve", lambda e: e.tensor_tensor(out=DTt[:], in0=DTt[:], in1=pkk[0:64, 0:256], op=ALU.mult), [rDTt, rpkk], [rDTt])
                S.op("dve", lambda e: e.tensor_tensor(out=DTt[:], in0=DTt[:], in1=pb[0:64, 0:256], op=ALU.mult), [rDTt, rpb], [rDTt])
                S.op("pool", lambda e: e.tensor_tensor(out=N[:], in0=DTt[:], in1=nms, op=ALU.mult), [rDTt, self.rconst], [rN])
                S.op("pool", lambda e: e.tensor_tensor(out=Pt[:], in0=N[:], in1=id4, op=ALU.add), [rN, self.rconst], [rPt])
                yield
                for lev in range(5):
                    (M, rM), (N, rN) = Mt[lev % 2], Nt[lev % 2]
                    (M2, rM2), (N2, rN2) = Mt[(lev + 1) % 2], Nt[(lev + 1) % 2]
                    pm, rpm = self.psum_chain(d)
                    for h in range(4):
                        S.op("pe", lambda e: e.matmul(pm[0:64, HB(h)], N[:, HB(h)], M[:, HB(h)], start=True, stop=True), [rM, rN], [rpm])
                    if lev < 4:
                        pn, rpn = self.psum_chain(d)
                        for h in range(4):
                            S.op("pe", lambda e: e.matmul(pn[0:64, HB(h)], M[:, HB(h)], N[:, HB(h)], start=True, stop=True), [rM, rN], [rpn])
                        S.op("dve", lambda e: e.tensor_copy(out=N2[:], in_=pn[0:64, 0:256]), [rpn], [rN2])
                    S.op("act", lambda e: e.activation(out=M2[:], in_=pm[0:64, 0:256], func=AF.Copy), [rpm], [rM2])
                    yield
                    pp, rpp = self.psum_chain(d)
                    for h in range(4):
                        S.op("pe", lambda e: e.matmul(pp[0:64, HB(h)], M2[:, HB(h)], Pt[:, HB(h)], start=True, stop=True), [rM2, rPt], [rpp])
                    S.op("dve", lambda e: e.tensor_tensor(out=Pt[:], in0=Pt[:], in1=pp[0:64, 0:256], op=ALU.add), [rPt, rpp], [rPt])
                    yield
                for h in range(4):
                    ko, vo = (0, 64) if h % 2 == 0 else (64, 0)
                    S.op("dve", lambda e: e.tensor_scalar(out=rhsm[:, h * 128 + ko:h * 128 + ko + 64], in0=Ktok[:, HB(h)], scalar1=tk(2, h), scalar2=None, op0=ALU.mult),
                         [rKt, rtok], [rrh])
                    S.op("pool", lambda e: e.tensor_scalar(out=rhsm[:, h * 128 + vo:h * 128 + vo + 64], in0=Vtok[:, HB(h)], scalar1=tk(3, h), scalar2=None, op0=ALU.mult),
                         [rVt, rtok], [rrh])
                yield
                pst, rpst = self.psum_chain(d)
                pso, rpso = self.psum_chain(d)
                for h in range(4):
                    S.op("pe", lambda e: e.matmul(pst[:, HB(h)], rhsm[:, h * 128:(h + 1) * 128], Pt[:, HB(h)], start=True, stop=True), [rrh, rPt], [rpst])
                    S.op("pe", lambda e: e.matmul(pso[0:64, h * 128:(h + 1) * 128], Pt[:, HB(h)], rhsm[:, h * 128:(h + 1) * 128], start=True, stop=True), [rrh, rPt], [rpso])
                S.op("dve", lambda e: e.tensor_tensor(out=wT[:], in0=pst[:, 0:256], in1=wmask, op=ALU.mult), [rpst, self.rconst], [rwT])
                for h in range(4):
                    vo = 64 if h % 2 == 0 else 0
                    S.op("dve", lambda e: e.tensor_copy(out=u0[:, HB(h)], in_=pso[0:64, h * 128 + vo:h * 128 + vo + 64]), [rpso], [ru0])
                yield
                pw, rpw = self.psum_chain(d)
                po1, rpo1 = self.psum_chain(d)
                for h in range(4):
                    p0 = (h % 2) * 64
                    Sh = St[:, h // 2, p0:p0 + 64]
                    S.op("pe", lambda e: e.matmul(pw[0:64, HB(h)], wT[:, HB(h)], Sh, start=True, stop=True), [rwT, rS], [rpw])
                    S.op("pe", lambda e: e.matmul(po1[0:64, HB(h)], QTm[:, HB(h)], Sh, start=True, stop=True), [rQTm, rS], [rpo1])
                S.op("dve", lambda e: e.tensor_tensor(out=ut[:], in0=u0[:], in1=pw[0:64, 0:256], op=ALU.subtract), [ru0, rpw], [rut])
                yield
                po2, rpo2 = self.psum_chain(d)
                for h in range(4):
                    S.op("pe", lambda e: e.matmul(po2[0:64, HB(h)], qkT[:, HB(h)], ut[:, HB(h)], start=True, stop=True), [rqk, rut], [rpo2])
                S.op("act", lambda e: e.activation(out=o2[:], in_=po2[0:64, 0:256], func=AF.Copy), [rpo2], [ro2])
                yield
                for h in range(4):
                    S.op("dve", lambda e: e.scalar_tensor_tensor(out=otmp[:, HB(h)], in0=po1[0:64, HB(h)], scalar=tk(4, h), in1=o2[:, HB(h)], op0=ALU.mult, op1=ALU.add),
                         [rpo1, ro2, rtok], [rotmp])
                S.op("pool", lambda e: e.tensor_tensor(out=Otok[:, c, :], in0=Otok[:, c, :], in1=otmp[:], op=ALU.add), [rotmp, rOtok[c]], [rOtok[c]])
                yield
                for h in range(4):
                    S.op("pool", lambda e: e.tensor_scalar(out=Kd[:, HB(h)], in0=Ktok[:, HB(h)], scalar1=tk(5, h), scalar2=None, op0=ALU.mult), [rKt, rtok], [rKd])
                for hc in range(2):
                    pS, rpS = self.psum_chain(d)
                    S.op("pe", lambda e: e.matmul(pS[:, 0:128], Kd[:, hc * 128:(hc + 1) * 128], ut[:, hc * 128:(hc + 1) * 128], start=True, stop=True), [rKd, rut], [rpS])
                    S.op("dve", lambda e: e.scalar_tensor_tensor(out=St[:, hc, :], in0=St[:, hc, :], scalar=ETOTP[:, d * 2 + hc, c:c + 1], in1=pS[:, 0:128],
                                                                 op0=ALU.mult, op1=ALU.add), [rS, rpS, rtok], [rS])

            orders = [list(range(32, 36)) + list(range(32)), list(range(35, 31, -1)) + list(range(31, -1, -1))]
            for n in range(36):
                gens = [step(d, orders[d][n]) for d in range(2)]
                while gens:
                    for g in list(gens):
                        try:
                            next(g)
                        except StopIteration:
                            gens.remove(g)
            n = 0
            for c in range(36):
                for hc in range(2):
                    ps, rp = self.psum()
                    S.op("pe", lambda e: e.transpose(ps[:, 0:64], Otok[:, c, hc * 128:(hc + 1) * 128], ident[0:64, 0:64]), [rOtok[c], self.rconst], [rp])
                    self.evac(OT[:, hc, c * 64:(c + 1) * 64], ps[:, 0:64], [rp], [rOT], n)
                    n += 1
            S.barrier()
            es.close()
            self.gated_tail(eso, [OT, None], [rOT, None], l, "c_og", 2064, 512)


    def mixer_d(self, b, l):
        S, A = self.S, self.A
        ident = self.cst("ident")
        with ExitStack() as eso:
            OTs = [self.sb(eso, "dOT%d" % i, [128, 2, T]) for i in range(2)]
            es = ExitStack()
            self.load_mconsts(es)
            gmask = self.cst("gla_mask")
            hm32 = self.cst("hmask32")
            sblk = self.cst("sblk")
            qT, rq = self.load_rows(es, "dq", 2320, 1)
            kT, rk = self.load_rows(es, "dk", 2448, 1)
            Vtok = self.sb(es, "dvtok", [64, 36, 256])
            rVt = Res()
            with ExitStack() as es2:
                vT, rv = self.load_rows(es2, "dv", 2576, 2)
                n = 0
                for c2 in range(2):
                    for c in range(36):
                        ps, rp = self.psum()
                        S.op("pe", lambda e: e.transpose(ps[0:64, 0:128], vT[:, c2, c * 64:(c + 1) * 64], ident), [rv, self.rconst], [rp])
                        self.evac(Vtok[:, c, c2 * 128:(c2 + 1) * 128], ps[0:64, 0:128], [rp], [rVt], n)
                        n += 1
                S.barrier()
            rOTs = [Res() for _ in range(2)]
            cm = self.sb(es, "cm", [128, T])
            rcm = Res()
            S.dma(cm[:], A["cmask"][:, :], writes=[rcm])
            LA = self.sb(es, "dLA", [128, T])
            G = self.sb(es, "dG", [128, T])
            QE = self.sb(es, "dQE", [128, T])
            KE = self.sb(es, "dKE", [128, T])
            KH = self.sb(es, "dKH", [128, T])
            TOT = self.sb(es, "dTOT", [128, 36])
            ETOT = self.sb(es, "dETOT", [128, 36])
            lr = self.sb(es, "dlr", [16, T])
            gw = self.sb(es, "dgw", [16, 128])
            St = self.sb(es, "dS", [128, 256])
            at = [self.sb(es, "dat%d" % i, [64, 256]) for i in range(2)]
            ktok = [self.sb(es, "dktok%d" % i, [64, 128]) for i in range(2)]
            KEm = [self.sb(es, "dKEm%d" % i, [128, 4, 64]) for i in range(2)]
            rKEm = [Res() for _ in range(2)]
            rw = Res()
            rS = Res()
            rat = [Res() for _ in range(2)]
            rkt = [Res() for _ in range(2)]
            gbo = PV["d_gb"][0]
            for d in range(2):
                OT, rOT = OTs[d], rOTs[d]
                S.dma(lr[:], A["projT"][2832 + 16 * d:2848 + 16 * d, :], reads=[self.rprojT], writes=[rw])
                S.dma(gw[:], A["d_gw"][l, d], writes=[rw])
                for t0 in range(0, T, 512):
                    tn = min(512, T - t0)
                    ps, rp = self.psum()
                    S.op("pe", lambda e: e.matmul(ps[:, 0:tn], gw[:], lr[:, t0:t0 + tn], start=True, stop=True), [rw], [rp])
                    S.op("dve", lambda e: e.tensor_scalar(out=LA[:, t0:t0 + tn], in0=ps[:, 0:tn], scalar1=self.pvec[:, l, gbo + d:gbo + d + 1], scalar2=None,
                                                          op0=ALU.add), [rp, self.rconst], [rw])
                S.op("act", lambda e: e.activation(out=LA[:], in_=LA[:], func=AF.Exp, scale=-1.0), [rw], [rw])
                S.op("pool", lambda e: e.tensor_scalar(out=LA[:], in0=LA[:], scalar1=1.0, scalar2=None, op0=ALU.add), [rw], [rw])
                S.op("act", lambda e: e.activation(out=LA[:], in_=LA[:], func=AF.Ln), [rw], [rw])
                S.op("pool", lambda e: e.tensor_scalar(out=LA[:], in0=LA[:], scalar1=-1.0 / 16.0, scalar2=None, op0=ALU.mult), [rw], [rw])
                S.op("dve", lambda e: e.tensor_tensor_scan(out=G[:], data0=cm[:], data1=LA[:], initial=0.0, op0=ALU.mult, op1=ALU.add), [rw, rcm], [rw])
                S.op("dve", lambda e: e.tensor_reduce(out=TOT[:], in_=LA[:].rearrange("p (c i) -> p c i", i=64), axis=AX.X, op=ALU.add), [rw], [rw])
                if d == 1:
                    S.op("pool", lambda e: e.tensor_tensor(out=G[:], in0=LA[:], in1=G[:], op=ALU.subtract), [rw], [rw])
                    for c in range(36):
                        S.op("dve", lambda e: e.tensor_scalar(out=G[:, c * 64:(c + 1) * 64], in0=G[:, c * 64:(c + 1) * 64], scalar1=TOT[:, c:c + 1], scalar2=None,
                                                              op0=ALU.add), [rw], [rw])
                S.op("act", lambda e: e.activation(out=QE[:], in_=G[:], func=AF.Exp), [rw], [rw])
                S.op("dve", lambda e: e.scalar_tensor_tensor(out=QE[:], in0=QE[:], scalar=32.0 ** -0.5, in1=qT[:, 0, :], op0=ALU.mult, op1=ALU.mult), [rw, rq], [rw])
                S.op("act", lambda e: e.activation(out=KE[:], in_=G[:], func=AF.Exp, scale=-1.0), [rw], [rw])
                S.op("pool", lambda e: e.tensor_tensor(out=KE[:], in0=KE[:], in1=kT[:, 0, :], op=ALU.mult), [rw, rk], [rw])
                for c in range(36):
                    S.op("dve", lambda e: e.tensor_scalar(out=KH[:, c * 64:(c + 1) * 64], in0=G[:, c * 64:(c + 1) * 64], scalar1=TOT[:, c:c + 1], scalar2=None,
                                                          op0=ALU.subtract), [rw], [rw])
                S.op("act", lambda e: e.activation(out=KH[:], in_=KH[:], func=AF.Exp, scale=-1.0), [rw], [rw])
                S.op("pool", lambda e: e.tensor_tensor(out=KH[:], in0=KH[:], in1=kT[:, 0, :], op=ALU.mult), [rw, rk], [rw])
                S.op("act", lambda e: e.activation(out=ETOT[:], in_=TOT[:], func=AF.Exp), [rw], [rw])
                S.op("pool", lambda e: e.memset(St[:], 0.0), [], [rS])
                order = (list(range(32, 36)) + list(range(32))) if d == 0 else (list(range(35, 31, -1)) + list(range(31, -1, -1)))
                for n, c in enumerate(order):
                    cs = slice(c * 64, (c + 1) * 64)
                    a_, ra = at[n % 2], rat[n % 2]
                    k_, rk_ = ktok[n % 2], rkt[n % 2]
                    pa, rpa = self.psum()
                    kem, rkem = KEm[n % 2], rKEm[n % 2]
                    for h in range(4):
                        S.op("pool", lambda e: e.tensor_scalar(out=kem[:, h, :], in0=KE[:, cs], scalar1=hm32[:, h:h + 1], scalar2=None, op0=ALU.mult), [rw, self.rconst], [rkem])
                    for h in range(4):
                        S.op("pe", lambda e: e.matmul(pa[0:64, h * 64:(h + 1) * 64], kem[:, h, :], QE[:, cs], start=True, stop=True), [rw, rkem], [rpa])
                    S.op("dve", lambda e: e.tensor_tensor(out=a_[:], in0=pa[0:64, 0:256], in1=gmask[0:64, d * 256:(d + 1) * 256], op=ALU.mult), [rpa, self.rconst], [ra])
                    pk, rpk = self.psum()
                    S.op("pe", lambda e: e.transpose(pk[0:64, 0:128], KH[:, cs], ident), [rw, self.rconst], [rpk])
                    S.op("act", lambda e: e.activation(out=k_[:], in_=pk[0:64, 0:128], func=AF.Copy), [rpk], [rk_])
                    po, rpo = self.psum()
                    for h in range(4):
                        hs = slice(h * 32, (h + 1) * 32)
                        es_ = slice(h * 64, (h + 1) * 64)
                        S.op("pe", lambda e: e.matmul(po[0:64, es_], St[:, es_], QE[:, cs], start=True, stop=False), [rS, rw], [rpo])
                        S.op("pe", lambda e: e.matmul(po[0:64, es_], Vtok[:, c, es_], a_[:, es_], start=False, stop=True), [rVt, ra], [rpo])
                    for h in range(4):
                        p0 = (h % 2) * 64
                        S.op("act", lambda e: e.activation(out=OT[p0:p0 + 64, h // 2, cs], in_=po[0:64, h * 64:(h + 1) * 64], func=AF.Copy), [rpo], [rOT])
                    pS, rpS = self.psum()
                    S.op("pe", lambda e: e.matmul(pS[:, 0:256], k_[:], Vtok[:, c, :], start=True, stop=True), [rk_, rVt], [rpS])
                    S.op("dve", lambda e: e.scalar_tensor_tensor(out=St[:], in0=St[:], scalar=ETOT[:, c:c + 1], in1=pS[:, 0:256], op0=ALU.mult, op1=ALU.add),
                         [rS, rpS, rw], [rS])
                    S.op("dve", lambda e: e.tensor_tensor(out=St[:], in0=St[:], in1=sblk, op=ALU.mult), [rS, self.rconst], [rS])
            S.barrier()
            es.close()
            self.gated_tail(eso, OTs, rOTs, l, "d_og", 2864, 768)

    def gated_tail(self, es, OTs, rOTs, l, gname, gate_row, zrow):
        S, A = self.S, self.A
        OT, rOT = OTs[0], rOTs[0]
        tmps = [(self.sb(es, "gt%d" % i, [128, 512]), Res()) for i in range(2)]
        gate, rg = self.load_rows(es, "gate", gate_row, 2)
        S.op("act", lambda e: e.activation(out=gate[:], in_=gate[:], func=AF.Silu), [rg], [rg])
        go = PV[gname][0]
        for c in range(2):
            if OTs[1] is not None:
                S.op("pool", lambda e: e.tensor_tensor(out=OT[:, c, :], in0=OT[:, c, :], in1=OTs[1][:, c, :], op=ALU.add), [rOT, rOTs[1]], [rOT])
            self.headnorm(OT, rOT, c, tmps, self.cst("bd64"), self.pvec[:, l, go:go + 1])
            S.op("pool", lambda e: e.tensor_tensor(out=OT[:, c, :], in0=OT[:, c, :], in1=gate[:, c, :], op=ALU.mult), [rOT, rg], [rOT])
        S.dma(A["oT"][zrow:zrow + 256, :].rearrange("(c p) t -> p c t", p=128), OT[:], reads=[rOT], writes=[self.roT], eng="pool")


    def layer_norm(self, es_tiles, l, t0, tn, gname, bname):
        S = self.S
        sq, rsq, rstd, rrstd = es_tiles
        onesln = self.cst("onesln")
        mean_ps, rmp = self.psum()
        for k in range(8):
            S.op("pe", lambda e, k=k: e.matmul(mean_ps[:, 0:tn], onesln, self.xT[:, k, t0:t0 + tn], start=(k == 0), stop=(k == 7)),
                 [self.rconst] + self.rxs(k, t0, tn), [rmp])
        for k in range(8):
            S.op("dve", lambda e, k=k: e.tensor_tensor(out=self.xT[:, k, t0:t0 + tn], in0=self.xT[:, k, t0:t0 + tn], in1=mean_ps[:, 0:tn],
                                                     op=ALU.subtract), [rmp] + self.rxs(k, t0, tn), self.rxs(k, t0, tn))
        var_ps, rvp = self.psum()
        for k in range(8):
            s_, r_ = sq[k % 2], rsq[k % 2]
            S.op("pool", lambda e, k=k, s_=s_: e.tensor_tensor(out=s_[:, 0:tn], in0=self.xT[:, k, t0:t0 + tn], in1=self.xT[:, k, t0:t0 + tn], op=ALU.mult),
                 self.rxs(k, t0, tn), [r_])
            S.op("pe", lambda e, k=k, s_=s_: e.matmul(var_ps[:, 0:tn], onesln, s_[:, 0:tn], start=(k == 0), stop=(k == 7)),
                 [r_, self.rconst], [rvp])
        epsc = self.cst("cvec")[:, 0:1]
        S.op("dve", lambda e: e.tensor_scalar(out=rstd[:, 0:tn], in0=var_ps[:, 0:tn], scalar1=EPS, scalar2=None, op0=ALU.add), [rvp], [rrstd])
        S.op("act", lambda e: e.activation(out=rstd[:, 0:tn], in_=rstd[:, 0:tn], func=AF.Sqrt), [rrstd], [rrstd])
        S.op("dve", lambda e: e.reciprocal(out=rstd[:, 0:tn], in_=rstd[:, 0:tn]), [rrstd], [rrstd])
        go, bo = PV[gname][0], PV[bname][0]
        for k in range(8):
            S.op("dve", lambda e, k=k: e.scalar_tensor_tensor(out=self.xT[:, k, t0:t0 + tn], in0=self.xT[:, k, t0:t0 + tn],
                                                            scalar=self.pvec[:, l, go + k:go + k + 1], in1=rstd[:, 0:tn],
                                                            op0=ALU.mult, op1=ALU.mult), [rrstd, self.rconst] + self.rxs(k, t0, tn), self.rxs(k, t0, tn))
            S.op("pool", lambda e, k=k: e.tensor_scalar(out=self.xT[:, k, t0:t0 + tn], in0=self.xT[:, k, t0:t0 + tn],
                                                      scalar1=self.pvec[:, l, bo + k:bo + k + 1], scalar2=None, op0=ALU.add),
                 self.rxs(k, t0, tn) + [self.rconst], self.rxs(k, t0, tn))

    def phase_merge(self, b, l):
        nc, S, A = self.nc, self.S, self.A
        TB = 256
        with ExitStack() as es:
            wbr = self.sb(es, "wbr", [128, 8, D])
            rwbr = Res()
            S.dma(wbr[:], A["w_branch"][l].rearrange("z (c p) d -> p (z c) d", p=128), writes=[rwbr])
            wo = self.sb(es, "wo", [128, 8, D])
            rwo = Res()
            S.dma(wo[:], A["w_out"][l].rearrange("(k p) d -> p k d", p=128), writes=[rwo])
            oTb = [self.sb(es, "oTb%d" % i, [128, 8, TB]) for i in range(2)]
            roTb = [Res() for _ in range(2)]
            G = [self.sb(es, "G%d" % i, [128, 4, TB]) for i in range(2)]
            rG = [Res() for _ in range(2)]
            m = self.sb(es, "m", [128, 8, TB])
            rm = [Res() for _ in range(8)]
            tmp = [self.sb(es, "mtmp%d" % i, [128, TB]) for i in range(2)]
            rtmp = [Res() for _ in range(2)]
            sq = [self.sb(es, "lnsq%d" % i, [128, TB]) for i in range(2)]
            rsq = [Res() for _ in range(2)]
            rstd = self.sb(es, "lnrstd", [128, TB])
            rrstd = Res()
            ng = 0
            nt = 0
            for bi in range(T // TB):
                t0 = bi * TB
                col = b if t0 < TL else 2
                ob, rob = oTb[bi % 2], roTb[bi % 2]
                S.dma(ob[:], A["oT"][:, t0:t0 + TB].rearrange("(c p) t -> p c t", p=128), reads=[self.roT], writes=[rob])
                for dmc in range(8):
                    g, rg = G[ng % 2], rG[ng % 2]
                    ng += 1
                    S.dma(g[:], A["projT"][3120:7216, t0:t0 + TB].rearrange("(z c p) t -> c p z t", z=4, p=128)[dmc],
                          reads=[self.rprojT], writes=[rg])
                    S.op("act", lambda e, g=g: e.activation(out=g[:], in_=g[:], func=AF.Sigmoid), [rg], [rg])
                    for z in range(4):
                        ps, rp = self.psum()
                        for c2 in range(2):
                            S.op("pe", lambda e, ps=ps, z=z, c2=c2, dmc=dmc, ob=ob: e.matmul(ps[:, 0:TB], wbr[:, z * 2 + c2, dmc * 128:(dmc + 1) * 128],
                                                                                       ob[:, z * 2 + c2, :], start=(c2 == 0), stop=(c2 == 1)),
                                 [rwbr, rob], [rp])
                        if z == 0:
                            S.op("dve", lambda e, ps=ps, g=g, dmc=dmc: e.tensor_tensor(out=m[:, dmc, :], in0=ps[:, 0:TB], in1=g[:, 0, :], op=ALU.mult),
                                 [rp, rg], [rm[dmc]])
                        else:
                            tt, rt = tmp[nt % 2], rtmp[nt % 2]
                            nt += 1
                            S.op("dve", lambda e, ps=ps, g=g, z=z, tt=tt: e.tensor_tensor(out=tt[:], in0=ps[:, 0:TB], in1=g[:, z, :], op=ALU.mult),
                                 [rp, rg], [rt])
                            S.op("pool", lambda e, tt=tt, dmc=dmc: e.tensor_tensor(out=m[:, dmc, :], in0=m[:, dmc, :], in1=tt[:], op=ALU.add),
                                 [rt, rm[dmc]], [rm[dmc]])
                for d2 in range(8):
                    ps, rp = self.psum()
                    for k in range(8):
                        S.op("pe", lambda e, ps=ps, k=k, d2=d2: e.matmul(ps[:, 0:TB], wo[:, k, d2 * 128:(d2 + 1) * 128], m[:, k, :],
                                                                       start=(k == 0), stop=(k == 7)), [rwo, rm[k]], [rp])
                    tt, rt = tmp[nt % 2], rtmp[nt % 2]
                    nt += 1
                    S.op("act", lambda e, ps=ps, tt=tt, d2=d2: e.activation(out=tt[:], in_=ps[:, 0:TB], func=AF.Identity, scale=self.mod(l, 2, d2, col)),
                         [rp, self.rmod], [rt])
                    S.op("dve", lambda e, tt=tt, d2=d2: e.scalar_tensor_tensor(out=self.xT[:, d2, t0:t0 + TB], in0=self.xT[:, d2, t0:t0 + TB], scalar=ALPHA,
                                                                            in1=tt[:], op0=ALU.mult, op1=ALU.add),
                         [rt] + self.rxs(d2, t0, TB), self.rxs(d2, t0, TB))
                if self.dbg.get("merge_ln", True):
                    self.layer_norm((sq, rsq, rstd, rrstd), l, t0, TB, "ln1_g", "ln1_b")

    def phase_moe(self, b, l):
        nc, S, A = self.nc, self.S, self.A
        ident = self.cst("ident")
        with ExitStack() as es:
            h2 = self.sb(es, "h2", [128, 8, T], BF16)
            rh2 = Res()
            denseT = self.sb(es, "denseT", [32, T])
            rdT = [Res() for _ in range(5)]
            for k in range(8):
                S.op("dve", lambda e, k=k: e.tensor_scalar(out=h2[:, k, 0:TL], in0=self.xT[:, k, 0:TL], scalar1=self.mod(l, 4, k, b),
                                                         scalar2=self.mod(l, 3, k, b), op0=ALU.mult, op1=ALU.add),
                     [self.rmod] + self.rx[k][0:4], [rh2])
                S.op("pool", lambda e, k=k: e.tensor_scalar(out=h2[:, k, TL:T], in0=self.xT[:, k, TL:T], scalar1=self.mod(l, 4, k, 2),
                                                          scalar2=self.mod(l, 3, k, 2), op0=ALU.mult, op1=ALU.add),
                     [self.rmod, self.rx[k][4]], [rh2])
            with ExitStack() as es2:
                wrt = self.sb(es2, "wrt", [128, 8, 36])
                rwrt = Res()
                S.dma(wrt[:], A["w_rt"][l].rearrange("(k p) c -> p k c", p=128), writes=[rwrt])
                brow = self.sb(es2, "brow", [1, 36])
                S.dma(brow[:], A["b_rt"][l:l + 1, :], writes=[rwrt])
                h2f = [self.sb(es2, "h2f%d" % i, [128, 8, 128]) for i in range(2)]
                rh2f = [Res() for _ in range(2)]
                R = [self.sb(es2, "rt%d" % i, [128, 160]) for i in range(2)]
                rR = [Res() for _ in range(2)]
                ones = self.cst("ones")
                for tt in range(T // 128):
                    t0 = tt * 128
                    col = b if t0 < TL else 2
                    hf, rhf = h2f[tt % 2], rh2f[tt % 2]
                    r_, rr = R[tt % 2], rR[tt % 2]
                    for k in range(8):
                        S.op("dve", lambda e, k=k, hf=hf: e.tensor_scalar(out=hf[:, k, :], in0=self.xT[:, k, t0:t0 + 128], scalar1=self.mod(l, 4, k, col),
                                                                        scalar2=self.mod(l, 3, k, col), op0=ALU.mult, op1=ALU.add),
                             [self.rmod] + self.rxs(k, t0, 128), [rhf])
                    ps, rp = self.psum()
                    for k in range(8):
                        S.op("pe", lambda e, k=k, hf=hf, ps=ps: e.matmul(ps[:, 0:36], hf[:, k, :], wrt[:, k, :], start=(k == 0), stop=False), [rhf, rwrt], [rp])
                    S.op("pe", lambda e, ps=ps: e.matmul(ps[:, 0:36], ones[0:1, :], brow[0:1, :], start=False, stop=True), [rwrt, self.rconst], [rp])
                    L = r_[:, 0:36]
                    sc = lambda i, r_=r_: r_[:, 140 + i:141 + i]
                    MG, NMG, SG, PG, M1, M2, DD, EE, RR, W1, W2 = range(11)
                    S.op("act", lambda e, ps=ps: e.activation(out=L, in_=ps[:, 0:36], func=AF.Copy), [rp], [rr])
                    dv = lambda fn: S.op("dve", fn, [rr], [rr])
                    dv(lambda e: e.tensor_reduce(out=sc(MG), in_=r_[:, 0:4], axis=AX.X, op=ALU.max))
                    dv(lambda e: e.tensor_scalar(out=r_[:, 60:64], in0=r_[:, 0:4], scalar1=sc(MG), scalar2=None, op0=ALU.subtract))
                    S.op("act", lambda e: e.activation(out=r_[:, 60:64], in_=r_[:, 60:64], func=AF.Exp), [rr], [rr])
                    dv(lambda e: e.tensor_reduce(out=sc(SG), in_=r_[:, 60:64], axis=AX.X, op=ALU.add))
                    dv(lambda e: e.reciprocal(out=sc(PG), in_=sc(SG)))
                    dv(lambda e: e.tensor_scalar(out=r_[:, 40:44], in0=r_[:, 0:4], scalar1=sc(MG), scalar2=None, op0=ALU.is_equal))
                    dv(lambda e: e.tensor_scalar(out=r_[:, 44:52], in0=r_[:, 4:12], scalar1=r_[:, 40:41], scalar2=None, op0=ALU.mult))
                    for g in range(1, 4):
                        dv(lambda e, g=g: e.scalar_tensor_tensor(out=r_[:, 44:52], in0=r_[:, 4 + 8 * g:12 + 8 * g], scalar=r_[:, 40 + g:41 + g],
                                                                 in1=r_[:, 44:52], op0=ALU.mult, op1=ALU.add))
                    dv(lambda e: e.tensor_reduce(out=sc(M1), in_=r_[:, 44:52], axis=AX.X, op=ALU.max))
                    dv(lambda e: e.tensor_scalar(out=r_[:, 52:60], in0=r_[:, 44:52], scalar1=sc(M1), scalar2=None, op0=ALU.is_equal))
                    dv(lambda e: e.scalar_tensor_tensor(out=r_[:, 60:68], in0=r_[:, 52:60], scalar=NEG, in1=r_[:, 44:52], op0=ALU.mult, op1=ALU.add))
                    dv(lambda e: e.tensor_reduce(out=sc(M2), in_=r_[:, 60:68], axis=AX.X, op=ALU.max))
                    dv(lambda e: e.tensor_scalar(out=r_[:, 68:76], in0=r_[:, 60:68], scalar1=sc(M2), scalar2=None, op0=ALU.is_equal))
                    dv(lambda e: e.tensor_tensor(out=sc(DD), in0=sc(M2), in1=sc(M1), op=ALU.subtract))
                    S.op("act", lambda e: e.activation(out=sc(EE), in_=sc(DD), func=AF.Exp), [rr], [rr])
                    dv(lambda e: e.tensor_scalar(out=sc(RR), in0=sc(EE), scalar1=1.0, scalar2=None, op0=ALU.add))
                    dv(lambda e: e.reciprocal(out=sc(RR), in_=sc(RR)))
                    dv(lambda e: e.tensor_tensor(out=sc(W1), in0=sc(RR), in1=sc(PG), op=ALU.mult))
                    dv(lambda e: e.tensor_tensor(out=sc(W2), in0=sc(W1), in1=sc(EE), op=ALU.mult))
                    dv(lambda e: e.tensor_scalar(out=r_[:, 76:84], in0=r_[:, 52:60], scalar1=sc(W1), scalar2=None, op0=ALU.mult))
                    dv(lambda e: e.scalar_tensor_tensor(out=r_[:, 76:84], in0=r_[:, 68:76], scalar=sc(W2), in1=r_[:, 76:84], op0=ALU.mult, op1=ALU.add))
                    for g in range(4):
                        dv(lambda e, g=g: e.tensor_scalar(out=r_[:, 84 + 8 * g:92 + 8 * g], in0=r_[:, 76:84], scalar1=r_[:, 40 + g:41 + g], scalar2=None,
                                                          op0=ALU.mult))
                    ps2, rp2 = self.psum()
                    S.op("pe", lambda e, ps2=ps2: e.transpose(ps2[0:32, 0:128], r_[:, 84:116], ident), [rr, self.rconst], [rp2])
                    S.op("act", lambda e, ps2=ps2: e.activation(out=denseT[:, t0:t0 + 128], in_=ps2[0:32, 0:128], func=AF.Copy), [rp2], [rdT[t0 // 512]])
                S.barrier()
            self.dump("denseT", denseT[:], [32, T], rdT)
            for k in range(8):
                for j in range(5):
                    t0 = j * 512
                    tn = min(512, T - t0)
                    S.op("pool", lambda e, k=k, t0=t0, tn=tn: e.tensor_scalar(out=self.xT[:, k, t0:t0 + tn], in0=self.xT[:, k, t0:t0 + tn], scalar1=ALPHA,
                                                                          scalar2=None, op0=ALU.mult), [self.rx[k][j]], [self.rx[k][j]])
            with ExitStack() as es3:
                NSTG = 2
                stg = [self.sb(es3, "stg%d" % i, [128, 2048]) for i in range(NSTG)]
                rstg = [Res() for _ in range(NSTG)]
                wgu = [self.sb(es3, "wgu%d" % i, [128, 8, 512], BF16) for i in range(3)]
                rwgu = [Res() for _ in range(3)]
                wdn = [self.sb(es3, "wdn%d" % i, [128, 4, D], BF16) for i in range(2)]
                rwdn = [Res() for _ in range(2)]
                abf = [self.sb(es3, "abf%d" % i, [128, 4, 512], BF16) for i in range(2)]
                rabf = [Res() for _ in range(2)]
                sgt = [self.sb(es3, "sgt%d" % i, [128, 512]) for i in range(2)]
                rsgt = [Res() for _ in range(2)]
                tt_ = [self.sb(es3, "ttm%d" % i, [128, 512]) for i in range(2)]
                rtt = [Res() for _ in range(2)]
                dsel = [self.sb(es3, "dsel%d" % i, [32, 512]) for i in range(2)]
                rdsel = [Res() for _ in range(2)]
                dB = [self.sb(es3, "dB%d" % i, [128, 512]) for i in range(2)]
                rdB = [Res() for _ in range(2)]
                ones = self.cst("ones")
                nstg = 0
                ngu = 0
                cnt = 0
                for e_ in range(self.dbg.get("e_start", 0), self.dbg.get("nexp", 32)):
                    ws = []
                    for wi, nm in enumerate(("w_gate", "w_up")):
                        w, rw = wgu[ngu % 3], rwgu[ngu % 3]
                        ngu += 1
                        for hh in range(2):
                            s_, rs_ = stg[nstg % NSTG], rstg[nstg % NSTG]
                            nstg += 1
                            S.dma(s_[:].rearrange("p (k h) -> p k h", k=8), A[nm][l, e_, :, hh * 256:(hh + 1) * 256].rearrange("(k p) h -> p k h", p=128),
                                  writes=[rs_])
                            S.op("pool", lambda e, s_=s_, w=w, hh=hh: e.tensor_copy(out=w[:, :, hh * 256:(hh + 1) * 256],
                                                                                 in_=s_[:].rearrange("p (k h) -> p k h", k=8)), [rs_], [rw])
                        ws.append((w, rw))
                    (wg, rwg), (wu, rwu) = ws
                    wd, rwd = wdn[e_ % 2], rwdn[e_ % 2]
                    for hh in range(2):
                        s_, rs_ = stg[nstg % NSTG], rstg[nstg % NSTG]
                        nstg += 1
                        S.dma(s_[:].rearrange("p (c d) -> p c d", c=2), A["w_down"][l, e_, hh * 256:(hh + 1) * 256, :].rearrange("(c p) d -> p c d", p=128),
                              writes=[rs_])
                        S.op("pool", lambda e, s_=s_, wd=wd, hh=hh: e.tensor_copy(out=wd[:, hh * 2:hh * 2 + 2, :],
                                                                               in_=s_[:].rearrange("p (c d) -> p c d", c=2)), [rs_], [rwd])
                    for tb in range(5):
                        t0 = tb * 512
                        tn = min(512, T - t0)
                        col = b if t0 < TL else 2
                        cnt += 1
                        ds_, rds = dsel[cnt % 2], rdsel[cnt % 2]
                        db_, rdb = dB[cnt % 2], rdB[cnt % 2]
                        ab, rab = abf[cnt % 2], rabf[cnt % 2]
                        S.op("dve", lambda e, ds_=ds_, t0=t0, tn=tn, e_=e_: e.tensor_scalar(out=ds_[:, 0:tn], in0=denseT[:, t0:t0 + tn], scalar1=ident[0:32, e_:e_ + 1],
                                                                                       scalar2=None, op0=ALU.mult), [rdT[tb], self.rconst], [rds])
                        psb, rpb = self.psum()
                        S.op("pe", lambda e, psb=psb, ds_=ds_, tn=tn: e.matmul(psb[:, 0:tn], ones[0:32, :], ds_[:, 0:tn], start=True, stop=True),
                             [rds, self.rconst], [rpb])
                        S.op("act", lambda e, psb=psb, db_=db_, tn=tn: e.activation(out=db_[:, 0:tn], in_=psb[:, 0:tn], func=AF.Copy), [rpb], [rdb])
                        for hc in range(4):
                            psg, rpg = self.psum()
                            for k in range(8):
                                S.op("pe", lambda e, psg=psg, k=k, hc=hc, wg=wg, t0=t0, tn=tn: e.matmul(psg[:, 0:tn], wg[:, k, hc * 128:(hc + 1) * 128], h2[:, k, t0:t0 + tn],
                                                                                                start=(k == 0), stop=(k == 7)), [rwg, rh2], [rpg])
                            psu, rpu = self.psum()
                            for k in range(8):
                                S.op("pe", lambda e, psu=psu, k=k, hc=hc, wu=wu, t0=t0, tn=tn: e.matmul(psu[:, 0:tn], wu[:, k, hc * 128:(hc + 1) * 128], h2[:, k, t0:t0 + tn],
                                                                                                start=(k == 0), stop=(k == 7)), [rwu, rh2], [rpu])
                            i2 = (cnt * 4 + hc) % 2
                            sg_, rsg = sgt[i2], rsgt[i2]
                            t_, rt_ = tt_[i2], rtt[i2]
                            S.op("act", lambda e, psg=psg, sg_=sg_, tn=tn: e.activation(out=sg_[:, 0:tn], in_=psg[:, 0:tn], func=AF.Silu), [rpg], [rsg])
                            S.op("dve", lambda e, psu=psu, sg_=sg_, t_=t_, tn=tn: e.tensor_tensor(out=t_[:, 0:tn], in0=psu[:, 0:tn], in1=sg_[:, 0:tn], op=ALU.mult),
                                 [rpu, rsg], [rt_])
                            S.op("pool", lambda e, t_=t_, db_=db_, ab=ab, hc=hc, tn=tn: e.tensor_tensor(out=ab[:, hc, 0:tn], in0=t_[:, 0:tn], in1=db_[:, 0:tn], op=ALU.mult),
                                 [rt_, rdb], [rab])
                        for dmc in range(8):
                            psd, rpd = self.psum()
                            for hc in range(4):
                                S.op("pe", lambda e, psd=psd, hc=hc, dmc=dmc, wd=wd, ab=ab, tn=tn: e.matmul(psd[:, 0:tn], wd[:, hc, dmc * 128:(dmc + 1) * 128], ab[:, hc, 0:tn],
                                                                                                    start=(hc == 0), stop=(hc == 3)), [rwd, rab], [rpd])
                            S.op("dve", lambda e, psd=psd, dmc=dmc, t0=t0, tn=tn, col=col: e.scalar_tensor_tensor(out=self.xT[:, dmc, t0:t0 + tn], in0=psd[:, 0:tn],
                                                                                                              scalar=self.mod(l, 5, dmc, col), in1=self.xT[:, dmc, t0:t0 + tn],
                                                                                                              op0=ALU.mult, op1=ALU.add),
                                 [rpd, self.rmod, self.rx[dmc][tb]], [self.rx[dmc][tb]])
                S.barrier()
            with ExitStack() as es4:
                sq = [self.sb(es4, "lnsq%d" % i, [128, 512]) for i in range(2)]
                rsq = [Res() for _ in range(2)]
                rstd = self.sb(es4, "lnrstd", [128, 512])
                rrstd = Res()
                for tb in range(5):
                    t0 = tb * 512
                    tn = min(512, T - t0)
                    self.layer_norm((sq, rsq, rstd, rrstd), l, t0, tn, "ln2_g", "ln2_b")


def make_in_maps(inputs, ncores=8):
    f = lambda a: np.ascontiguousarray(np.asarray(a, np.float32))
    x, c, ctx, c_ctx = inputs["x"], inputs["c"], inputs["ctx"], inputs["c_ctx"]
    pvec = np.zeros((DEPTH, 128, NPV), np.float32)
    for l in range(DEPTH):
        for name in ("b_ada", "ln1_g", "ln1_b", "ln2_g", "ln2_b"):
            o, n = PV[name]
            pvec[l, :, o:o + n] = _fm(inputs[name][l])
        for name, src in (("a_qg", "a_q_gain"), ("a_kg", "a_k_gain"), ("c_og", "c_out_gain"), ("d_og", "d_out_gain")):
            pvec[l, :, PV[name][0]] = np.tile(np.asarray(inputs[src][l], np.float32), 2)
        cw = np.asarray(inputs["c_conv"][l], np.float32)
        for ch in range(6):
            for tap in range(3):
                pvec[l, :, PV["c_conv"][0] + ch * 3 + tap] = cw[tap, ch * 128:(ch + 1) * 128]
        pvec[l, 0:8, PV["c_dtb"][0]] = np.asarray(inputs["c_dt_bias"][l], np.float32).reshape(8)
        pvec[l, 0:8, PV["c_alog"][0]] = np.asarray(inputs["c_a_log"][l], np.float32).reshape(8)
        pvec[l, :, PV["d_gb"][0]:PV["d_gb"][0] + 2] = np.asarray(inputs["d_gate_b"][l], np.float32).T
    w_rt = np.concatenate([inputs["w_router_g"], np.transpose(inputs["w_router_e"], (0, 2, 1, 3)).reshape(DEPTH, D, 32)], axis=2)
    b_rt = np.concatenate([inputs["b_router_g"], inputs["b_router_e"].reshape(DEPTH, 32)], axis=1)
    shared = {"mconsts": MCONSTS, "cmask": CMASK, "d_gw": f(inputs["d_gate_w"]), "rope": ROPE, "nab": _na_bias(np.asarray(inputs["b_rpb"], np.float32)), "consts": CONSTS, "pvec": pvec, "w_ada": f(inputs["w_ada"]), "w_in": f(inputs["w_in"]), "w_branch": f(inputs["w_branch"]),
              "w_out": f(inputs["w_out"]), "w_rt": f(w_rt), "b_rt": f(b_rt), "w_up": f(inputs["w_up"]), "w_gate": f(inputs["w_gate"]),
              "w_down": f(inputs["w_down"])}
    maps = []
    for i in range(ncores):
        bs = slice(2 * i, 2 * i + 2)
        m = dict(shared)
        m["xT_in"] = f(np.transpose(x[bs], (0, 2, 1)))
        m["ctxT_in"] = f(np.transpose(ctx[bs], (0, 2, 1)))
        m["cc"] = f(np.stack([c[2 * i], c[2 * i + 1], c_ctx], axis=1))
        maps.append(m)
    return maps


def kernel(**inputs):
    nc = bass.Bass("TRN2", target_bir_lowering=False)
    Kern(nc).build()
    maps = make_in_maps(inputs)
    res = run_bass_kernel_spmd(nc, maps, core_ids=list(range(8)))
    out = np.zeros((16, TL, D), np.float32)
    for i in range(8):
        o = res.results[i]["outT"]
        out[2 * i:2 * i + 2] = np.transpose(o, (0, 2, 1))
    return out
```

```python
import numpy as np
import concourse.bass as bass
import concourse.mybir as mybir
from concourse.bass_utils import run_bass_kernel_spmd
from contextlib import ExitStack

F32 = mybir.dt.float32
BF16 = mybir.dt.bfloat16
AF = mybir.ActivationFunctionType
ALU = mybir.AluOpType
AX = mybir.AxisListType

D = 1024
TL = 2048
TC = 256
T = TL + TC
DEPTH = 4
INW = 7216
ALPHA = (2.0 * DEPTH) ** 0.25
EPS = 1e-6
NEG = -30000.0


class Res:
    __slots__ = ("w", "rs")

    def __init__(self):
        self.w = None
        self.rs = []


ENGS = ("pe", "act", "dve", "pool", "sp")


class Sched:
    def __init__(self, nc, ndma_sems=16):
        self.nc = nc
        self.eobj = {"pe": nc.tensor, "act": nc.scalar, "dve": nc.vector, "pool": nc.gpsimd, "sp": nc.sync}
        self.esem = {e: nc.alloc_semaphore("prog_" + e) for e in ENGS}
        self.ecount = {e: 0 for e in ENGS}
        self.dsems = {}
        self.dcount = {}
        self.ndma = ndma_sems
        self.seen = {e: {} for e in ENGS}
        self.nops = 0

    def _wait(self, eng, tok):
        sem, val = tok[0], tok[1]
        k = id(sem)
        if self.seen[eng].get(k, 0) >= val:
            return
        self.seen[eng][k] = val
        self.eobj[eng].wait_ge(sem, val)

    def op(self, eng, fn, reads=(), writes=(), dma=False):
        toks = []
        for r in reads:
            if r.w is not None:
                toks.append(r.w)
        for w in writes:
            if w.w is not None:
                toks.append(w.w)
            toks.extend(w.rs)
        for t in toks:
            if t[2] == eng and not t[3] and eng == "pe":
                continue
            self._wait(eng, t)
        if dma:
            if eng not in self.dsems:
                self.dsems[eng] = [self.nc.alloc_semaphore("dma_%s_%d" % (eng, i)) for i in range(self.ndma)]
                self.dcount[eng] = 0
            j = self.dcount[eng]
            self.dcount[eng] = j + 1
            sem = self.dsems[eng][j % self.ndma]
            if j >= self.ndma:
                self._wait(eng, (sem, 16 * (j // self.ndma)))
            val = 16 * (j // self.ndma + 1)
            ins = fn(self.eobj[eng])
            ins.then_inc(sem, 16)
            tok = (sem, val, eng, True)
        else:
            self.ecount[eng] += 1
            ins = fn(self.eobj[eng])
            ins.then_inc(self.esem[eng], 1)
            tok = (self.esem[eng], self.ecount[eng], eng, False)
        for r in reads:
            r.rs.append(tok)
        for w in writes:
            w.w = tok
            w.rs = []
        self.nops += 1
        return tok

    def dma(self, out, in_, reads=(), writes=(), eng="sp", **kw):
        return self.op(eng, lambda e: e.dma_start(out=out, in_=in_, **kw), reads, writes, dma=True)

    def barrier(self):
        toks = []
        for e in ENGS:
            if self.ecount[e] > 0:
                toks.append((self.esem[e], self.ecount[e], e, False))
        for q, sems in self.dsems.items():
            n = self.dcount[q]
            for i, s in enumerate(sems):
                uses = (n - i + self.ndma - 1) // self.ndma if n > i else 0
                if uses > 0:
                    toks.append((s, 16 * uses, q, True))
        for e in ENGS:
            for t in toks:
                if t[2] == e and not t[3]:
                    continue
                self._wait(e, t)

    def finish(self, toks):
        for t in toks:
            self._wait("sp", t)


CONST_COLS = {}


def _build_consts():
    cols = []

    mcols = []

    def add(name, arr):
        arr = np.asarray(arr, np.float32)
        assert arr.shape[0] == 128
        if name in ("gla_mask", "sel8", "selp", "gdn_ms", "gdn_nms", "gdn_qi", "ident4", "hmask32", "sblk", "hm64", "wmask"):
            CONST_COLS[name] = (1, sum(a.shape[1] for a in mcols), arr.shape[1])
            mcols.append(arr)
        else:
            CONST_COLS[name] = (0, sum(a.shape[1] for a in cols), arr.shape[1])
            cols.append(arr)

    add("ident", np.eye(128))
    add("onesln", np.full((128, 128), 1.0 / 1024))
    bd = np.zeros((128, 128))
    bd[:64, :64] = 1.0 / 64
    bd[64:, 64:] = 1.0 / 64
    add("bd64", bd)
    bdo = np.zeros((128, 128))
    bdo[:64, :64] = 1.0
    bdo[64:, 64:] = 1.0
    add("bd64one", bdo)
    add("ones", np.ones((128, 128)))
    cv = np.zeros((128, 8))
    cv[:, 0] = EPS
    cv[:, 1] = 1.0
    cv[0:4, 2] = 1.0
    cv[4:8, 3] = 1.0
    add("cvec", cv)
    gm = np.zeros((128, 512))
    tri = np.tril(np.ones((64, 64)))
    for h in range(4):
        gm[0:64, h * 64:(h + 1) * 64] = tri.T
        gm[0:64, 256 + h * 64:256 + (h + 1) * 64] = tri
    add("gla_mask", gm)
    sel8 = np.zeros((128, 512))
    for dh in range(8):
        sel8[dh, dh * 64:(dh + 1) * 64] = 1.0
    add("sel8", sel8)
    selp = np.zeros((128, 512))
    for d in range(2):
        for hc in range(2):
            o = (d * 2 + hc) * 128
            selp[d * 4 + 2 * hc, o:o + 64] = 1.0
            selp[d * 4 + 2 * hc + 1, o + 64:o + 128] = 1.0
    add("selp", selp)
    lo = np.tril(np.ones((64, 64)), -1)
    up = np.triu(np.ones((64, 64)), 1)
    ms = np.zeros((128, 512)); nms = np.zeros((128, 512)); qi = np.zeros((128, 512)); id4 = np.zeros((128, 256))
    for h in range(4):
        ms[0:64, h * 64:(h + 1) * 64] = lo
        ms[0:64, 256 + h * 64:256 + (h + 1) * 64] = up
        nms[0:64, h * 64:(h + 1) * 64] = -up
        nms[0:64, 256 + h * 64:256 + (h + 1) * 64] = -lo
        qi[0:64, h * 64:(h + 1) * 64] = up + np.eye(64)
        qi[0:64, 256 + h * 64:256 + (h + 1) * 64] = lo + np.eye(64)
        id4[0:64, h * 64:(h + 1) * 64] = np.eye(64)
    add("gdn_ms", ms)
    add("gdn_nms", nms)
    add("gdn_qi", qi)
    add("ident4", id4)
    hm32 = np.zeros((128, 4)); sblk = np.zeros((128, 256)); hm64 = np.zeros((128, 2)); wmask = np.zeros((128, 256))
    for h in range(4):
        hm32[h * 32:(h + 1) * 32, h] = 1.0
        sblk[h * 32:(h + 1) * 32, h * 64:(h + 1) * 64] = 1.0
        wmask[(h % 2) * 64:(h % 2) * 64 + 64, h * 64:(h + 1) * 64] = 1.0
    hm64[0:64, 0] = 1.0
    hm64[64:128, 1] = 1.0
    add("hmask32", hm32)
    add("sblk", sblk)
    add("hm64", hm64)
    add("wmask", wmask)
    return np.concatenate(cols, axis=1), np.concatenate(mcols, axis=1)


CONSTS, MCONSTS = _build_consts()
NMCONST = MCONSTS.shape[1]
CMASK = np.ones((128, T), np.float32)
CMASK[:, ::64] = 0.0
NCONST = CONSTS.shape[1]


def _rope_tables():
    t = np.arange(TL)
    row = (t // 64).astype(np.float32)
    col = (t % 64).astype(np.float32)
    inv = (np.float32(10000.0) ** (-np.arange(16, dtype=np.float32) / np.float32(16))).astype(np.float32)
    ang_r = row[:, None] * inv
    ang_c = col[:, None] * inv
    C = np.zeros((64, TL), np.float32)
    Sg = np.zeros((64, TL), np.float32)
    P = np.zeros((64, 64), np.float32)
    for m in range(64):
        d = m % 64
        ang = ang_r if d < 32 else ang_c
        f = d % 16
        first = (d % 32) < 16
        C[m] = np.cos(ang[:, f])
        Sg[m] = (-1.0 if first else 1.0) * np.sin(ang[:, f])
        src = m + 16 if first else m - 16
        P[src, m] = 1.0
    return np.concatenate([C, Sg, P], axis=1)


ROPE = _rope_tables()


def _rs(r):
    return min(max(r - 4, 0), 24)


def _na_patterns():
    pats = {}
    table = {}
    for t in range(16):
        r0, r1 = 2 * t, 2 * t + 1
        for j in range(_rs(r0) // 2, (_rs(r1) + 7) // 2 + 1):
            key = tuple((2 * j + a - (2 * t + bq), _rs(2 * t + bq) <= 2 * j + a < _rs(2 * t + bq) + 8) for a in (0, 1) for bq in (0, 1))
            if not any(v for _, v in key):
                continue
            table[(t, j)] = pats.setdefault(key, len(pats))
    return pats, table


NA_PATS, NA_TABLE = _na_patterns()
NPAT = len(NA_PATS)


def _na_bias(rpb):
    L = rpb.shape[0]
    out = np.full((L, NPAT, 128, 4, 128), NEG, np.float32)
    qc = np.arange(64)
    kc = np.arange(64)
    cs = np.clip(qc - 8, 0, 48)
    col_ok = (kc[None, :] >= cs[:, None]) & (kc[None, :] < cs[:, None] + 16)
    dc = np.clip(kc[None, :] - qc[:, None] + 15, 0, 30)
    for key, idx in NA_PATS.items():
        n = 0
        for a in (0, 1):
            for bq in (0, 1):
                dr, valid = key[n]
                n += 1
                if not valid:
                    continue
                blk = rpb[:, :, dr + 7, :][:, :, dc]
                blk = np.where(col_ok[None, None], blk, np.float32(NEG))
                out[:, idx, a * 64:(a + 1) * 64, :, bq * 64:(bq + 1) * 64] = np.transpose(blk, (0, 3, 1, 2))
    return out


PV = {}


def _pv_layout():
    off = 0
    for name, n in [("b_ada", 48), ("ln1_g", 8), ("ln1_b", 8), ("ln2_g", 8), ("ln2_b", 8), ("a_qg", 1), ("a_kg", 1), ("c_og", 1), ("d_og", 1), ("d_gb", 2), ("c_conv", 18), ("c_dtb", 1), ("c_alog", 1)]:
        PV[name] = (off, n)
        off += n
    return off


NPV = _pv_layout()


def _fm(v):
    return np.ascontiguousarray(np.asarray(v, np.float32).reshape(-1, 128).T)


class Kern:
    def __init__(self, nc, dbg=None):
        self.nc = nc
        self.S = Sched(nc)
        self.dbg = dbg or {}
        self.es = ExitStack()
        self.nps = 0

    def sb(self, es, name, shape, dt=F32):
        self.nsb = getattr(self, "nsb", 0) + 1
        return es.enter_context(self.nc.sbuf_tensor("%s_%d" % (name, self.nsb), shape, dt))

    def psum_chain(self, d):
        self.npc = getattr(self, "npc", [0, 0])
        i = d * 4 + self.npc[d] % 4
        self.npc[d] += 1
        return self.ps[i], self.rps[i]

    def psum(self):
        i = self.nps % 8
        self.nps += 1
        return self.ps[i], self.rps[i]

    def dump(self, name, ap, shape, reads):
        if not self.dbg.get("dump"):
            return
        d = self.nc.dram_tensor("dump_" + name, list(shape), F32, kind="ExternalOutput").ap()
        self.S.barrier()
        self.S.dma(d, ap, reads=reads)
        self.S.barrier()

    def cst(self, name, rows=128):
        w, o, n = CONST_COLS[name]
        return (self.mconsts if w else self.consts)[0:rows, o:o + n]

    def load_mconsts(self, es):
        self.mconsts = self.sb(es, "mconsts", [128, NMCONST])
        self.S.dma(self.mconsts[:], self.A["mconsts"][:, :], writes=[self.rconst])

    def evac(self, out, in_, reads, writes, i):
        if i % 2 == 0:
            self.S.op("act", lambda e: e.activation(out=out, in_=in_, func=AF.Copy), reads, writes)
        else:
            self.S.op("dve", lambda e: e.tensor_copy(out=out, in_=in_), reads, writes)

    def build(self):
        nc, S = self.nc, self.S
        dt = nc.dram_tensor
        A = {}
        A["xT"] = dt("xT_in", [2, D, TL], F32, kind="ExternalInput").ap()
        A["ctxT"] = dt("ctxT_in", [2, D, TC], F32, kind="ExternalInput").ap()
        A["cc"] = dt("cc", [D, 3], F32, kind="ExternalInput").ap()
        A["consts"] = dt("consts", [128, NCONST], F32, kind="ExternalInput").ap()
        A["rope"] = dt("rope", [64, 2 * TL + 64], F32, kind="ExternalInput").ap()
        A["nab"] = dt("nab", [DEPTH, NPAT, 128, 4, 128], F32, kind="ExternalInput").ap()
        A["mconsts"] = dt("mconsts", [128, NMCONST], F32, kind="ExternalInput").ap()
        A["pvec"] = dt("pvec", [DEPTH, 128, NPV], F32, kind="ExternalInput").ap()
        A["w_ada"] = dt("w_ada", [DEPTH, D, 6 * D], F32, kind="ExternalInput").ap()
        A["w_in"] = dt("w_in", [DEPTH if self.dbg.get("do_proj", True) else 1, D, INW], F32, kind="ExternalInput").ap()
        A["w_branch"] = dt("w_branch", [DEPTH, 4, 256, D], F32, kind="ExternalInput").ap()
        A["w_out"] = dt("w_out", [DEPTH, D, D], F32, kind="ExternalInput").ap()
        A["w_rt"] = dt("w_rt", [DEPTH, D, 36], F32, kind="ExternalInput").ap()
        A["b_rt"] = dt("b_rt", [DEPTH, 36], F32, kind="ExternalInput").ap()
        nlw = DEPTH if self.dbg.get("do_moe", True) else 1
        nex = self.dbg.get("nexp", 32) if self.dbg.get("do_moe", True) else 1
        A["w_up"] = dt("w_up", [nlw, nex, D, 512], F32, kind="ExternalInput").ap()
        A["w_gate"] = dt("w_gate", [nlw, nex, D, 512], F32, kind="ExternalInput").ap()
        A["w_down"] = dt("w_down", [nlw, nex, 512, D], F32, kind="ExternalInput").ap()
        A["out"] = dt("outT", [2, D, TL], F32, kind="ExternalOutput").ap()
        A["projT"] = dt("projT", [INW, T], F32, kind=self.dbg.get("proj_kind", "Internal")).ap()
        A["oT"] = dt("oT", [D, T], F32, kind=self.dbg.get("oT_kind", "Internal")).ap()
        self.A = A
        A["xsave"] = dt("xsave", [D, T], F32, kind="Internal").ap()
        self.rxsave = Res()
        A["cmask"] = dt("cmask", [128, T], F32, kind="ExternalInput").ap()
        A["d_gw"] = dt("d_gw", [DEPTH, 2, 16, 128], F32, kind="ExternalInput").ap()
        self.rprojT = Res()
        self.roT = Res()

        es = self.es
        self.ps = [nc.alloc_psum_tensor("ps%d" % i, [128, 512], F32) for i in range(8)]
        self.rps = [Res() for _ in range(8)]
        self.consts = self.sb(es, "consts", [128, NCONST])
        self.rconst = Res()
        S.dma(self.consts[:], A["consts"][:, :], writes=[self.rconst])
        self.pvec = self.sb(es, "pvec", [128, DEPTH, NPV])
        for l in range(DEPTH):
            S.dma(self.pvec[:, l, :], A["pvec"][l], writes=[self.rconst])
        self.modT = self.sb(es, "modT", [128, DEPTH, 48, 3])
        self.rmod = Res()
        self.x_es = ExitStack()
        self.xT = self.sb(self.x_es, "xT", [128, 8, T])
        self.rx = [[Res() for _ in range(5)] for _ in range(8)]

        self.phase_mods()
        S.barrier()
        outs = []
        nitems = self.dbg.get("nitems", 2)
        nlayers = self.dbg.get("nlayers", DEPTH)
        for b in range(nitems):
            self.load_x(b)
            for l in range(nlayers):
                if self.dbg.get("do_proj", True):
                    self.phase_proj(b, l)
                    S.barrier()
                if self.dbg.get("do_mix", True):
                    if not self.dbg.get("nospill"):
                        self.spill_x()
                    self.phase_mixers(b, l)
                    S.barrier()
                    if not self.dbg.get("nospill"):
                        self.restore_x()
                if self.dbg.get("do_merge", True):
                    self.phase_merge(b, l)
                    S.barrier()
                if self.dbg.get("do_moe", True):
                    self.phase_moe(b, l)
                    S.barrier()
            for k in range(8):
                outs.append(S.dma(A["out"][b, k * 128:(k + 1) * 128, :], self.xT[:, k, 0:TL],
                                  reads=[self.rx[k][j] for j in range(4)], eng="sp"))
            S.barrier()
        S.finish(outs)
        self.x_es.close()
        self.es.close()

    def rxs(self, k, t0, n):
        return [self.rx[k][j] for j in range(t0 // 512, (t0 + n - 1) // 512 + 1)]

    def spill_x(self):
        for k in range(8):
            self.S.dma(self.A["xsave"][k * 128:(k + 1) * 128, :], self.xT[:, k, :], reads=self.rx[k], writes=[self.rxsave])
        self.S.barrier()
        self.x_es.close()

    def restore_x(self):
        self.x_es = ExitStack()
        self.xT = self.sb(self.x_es, "xT", [128, 8, T])
        for k in range(8):
            self.S.dma(self.xT[:, k, :], self.A["xsave"][k * 128:(k + 1) * 128, :], reads=[self.rxsave], writes=self.rx[k])

    def load_x(self, b):
        S, A = self.S, self.A
        for k in range(8):
            S.dma(self.xT[:, k, 0:TL], A["xT"][b, k * 128:(k + 1) * 128, :], writes=self.rx[k][0:4])
            S.dma(self.xT[:, k, TL:T], A["ctxT"][b, k * 128:(k + 1) * 128, :], writes=[self.rx[k][4]])

    def phase_mods(self):
        nc, S, A = self.nc, self.S, self.A
        with ExitStack() as es:
            scT = self.sb(es, "scT", [128, 8, 3])
            rsc = Res()
            S.dma(scT[:], A["cc"].rearrange("(k p) j -> p k j", p=128), writes=[rsc])
            S.op("act", lambda e: e.activation(out=scT[:], in_=scT[:], func=AF.Silu), [rsc], [rsc])
            wt = [self.sb(es, "wada%d" % i, [128, 8, 128]) for i in range(3)]
            rw = [Res() for _ in range(3)]
            n = 0
            for l in range(DEPTH):
                bo = PV["b_ada"][0]
                for c in range(48):
                    w, r = wt[n % 3], rw[n % 3]
                    n += 1
                    S.dma(w[:], A["w_ada"][l, :, c * 128:(c + 1) * 128].rearrange("(k p) c -> p k c", p=128), writes=[r])
                    ps, rp = self.psum()
                    for k in range(8):
                        S.op("pe", lambda e, w=w, ps=ps, k=k: e.matmul(ps[:, 0:3], w[:, k, :], scT[:, k, :], start=(k == 0), stop=(k == 7)),
                             [r, rsc], [rp])
                    S.op("dve", lambda e, ps=ps, l=l, c=c: e.tensor_scalar(out=self.modT[:, l, c, :], in0=ps[:, 0:3],
                                                                         scalar1=self.pvec[:, l, bo + c:bo + c + 1], scalar2=None, op0=ALU.add),
                         [rp, self.rconst], [self.rmod])
                for c0 in (8, 32):
                    S.op("dve", lambda e, l=l, c0=c0: e.tensor_scalar(out=self.modT[:, l, c0:c0 + 8, :], in0=self.modT[:, l, c0:c0 + 8, :],
                                                                    scalar1=1.0, scalar2=None, op0=ALU.add), [self.rmod], [self.rmod])

    def mod(self, l, grp, k, col):
        return self.modT[:, l, grp * 8 + k, col:col + 1]

    def phase_proj(self, b, l):
        nc, S, A = self.nc, self.S, self.A
        groups = [(0, 256), (256, 128), (384, 128), (512, 256), (768, 256), (1024, 256), (1280, 768), (2048, 16), (2064, 256),
                  (2320, 128), (2448, 128), (2576, 256), (2832, 32), (2864, 256), (3120, 4096)]
        chunks = []
        for (o, n) in groups:
            for c in range(0, n, 128):
                chunks.append((o + c, min(128, n - c)))
        with ExitStack() as es:
            hT = self.sb(es, "hT", [128, 8, T], BF16)
            rh = Res()
            for k in range(8):
                S.op("dve", lambda e, k=k: e.tensor_scalar(out=hT[:, k, 0:TL], in0=self.xT[:, k, 0:TL], scalar1=self.mod(l, 1, k, b),
                                                         scalar2=self.mod(l, 0, k, b), op0=ALU.mult, op1=ALU.add),
                     [self.rmod] + self.rx[k][0:4], [rh])
                S.op("pool", lambda e, k=k: e.tensor_scalar(out=hT[:, k, TL:T], in0=self.xT[:, k, TL:T], scalar1=self.mod(l, 1, k, 2),
                                                          scalar2=self.mod(l, 0, k, 2), op0=ALU.mult, op1=ALU.add),
                     [self.rmod, self.rx[k][4]], [rh])
            NW = 3
            wst = [self.sb(es, "wins%d" % i, [128, 8, 128]) for i in range(NW)]
            rwst = [Res() for _ in range(NW)]
            wt = [self.sb(es, "win%d" % i, [128, 8, 128], BF16) for i in range(NW)]
            rw = [Res() for _ in range(NW)]
            NST = 4
            st = [self.sb(es, "pst%d" % i, [128, 512]) for i in range(NST)]
            rst = [Res() for _ in range(NST)]
            ns = 0
            for ci, (c0, cn) in enumerate(chunks):
                w, r = wt[ci % NW], rw[ci % NW]
                ws_, rws = wst[ci % NW], rwst[ci % NW]
                S.dma(ws_[:, :, 0:cn], A["w_in"][l, :, c0:c0 + cn].rearrange("(k p) c -> p k c", p=128), writes=[rws])
                S.op("pool", lambda e: e.tensor_copy(out=w[:, :, 0:cn], in_=ws_[:, :, 0:cn]), [rws], [r])
                for tb in range(5):
                    t0 = tb * 512
                    tn = min(512, T - t0)
                    ps, rp = self.psum()
                    for k in range(8):
                        S.op("pe", lambda e, w=w, ps=ps, k=k, cn=cn, t0=t0, tn=tn: e.matmul(ps[0:cn, 0:tn], w[:, k, 0:cn], hT[:, k, t0:t0 + tn],
                                                                                       start=(k == 0), stop=(k == 7)), [r, rh], [rp])
                    s_, rs_ = st[ns % NST], rst[ns % NST]
                    self.evac(s_[0:cn, 0:tn], ps[0:cn, 0:tn], [rp], [rs_], ns)
                    ns += 1
                    S.dma(A["projT"][c0:c0 + cn, t0:t0 + tn], s_[0:cn, 0:tn], reads=[rs_], writes=[self.rprojT], eng="pool")

    def phase_mixers(self, b, l):
        which = self.dbg.get("mixers", "abcd")
        if "a" in which:
            self.mixer_a(b, l)
            self.S.barrier()
        if "b" in which:
            self.mixer_b(b, l)
            self.S.barrier()
        if "d" in which:
            self.mixer_d(b, l)
            self.S.barrier()
        if "c" in which:
            self.mixer_c(b, l)
            self.S.barrier()

    def load_rows(self, es, name, r0, nch):
        t = self.sb(es, name, [128, nch, T])
        r = Res()
        self.S.dma(t[:], self.A["projT"][r0:r0 + 128 * nch, :].rearrange("(c p) t -> p c t", p=128), reads=[self.rprojT], writes=[r])
        return t, r

    def load_heads(self, es, name, r0, nh):
        t = self.sb(es, name, [64, nh, T])
        r = Res()
        self.S.dma(t[:], self.A["projT"][r0:r0 + 64 * nh, :].rearrange("(h p) t -> p h t", p=64), reads=[self.rprojT], writes=[r])
        return t, r

    def headnorm(self, X, rX, c, tmps, mat, gain, P=128):
        S = self.S
        (s1, r1), (s2, r2) = tmps
        for t0 in range(0, T, 512):
            tn = min(512, T - t0)
            xs = X[0:P, c, t0:t0 + tn]
            S.op("pool", lambda e: e.tensor_tensor(out=s1[0:P, 0:tn], in0=xs, in1=xs, op=ALU.mult), [rX], [r1])
            ps, rp = self.psum()
            S.op("pe", lambda e: e.matmul(ps[0:P, 0:tn], mat[0:P, 0:P], s1[0:P, 0:tn], start=True, stop=True), [r1, self.rconst], [rp])
            S.op("dve", lambda e: e.tensor_scalar(out=s2[0:P, 0:tn], in0=ps[0:P, 0:tn], scalar1=EPS, scalar2=None, op0=ALU.add), [rp], [r2])
            S.op("act", lambda e: e.activation(out=s2[0:P, 0:tn], in_=s2[0:P, 0:tn], func=AF.Sqrt), [r2], [r2])
            S.op("dve", lambda e: e.reciprocal(out=s2[0:P, 0:tn], in_=s2[0:P, 0:tn]), [r2], [r2])
            S.op("dve", lambda e: e.scalar_tensor_tensor(out=xs, in0=xs, scalar=gain, in1=s2[0:P, 0:tn], op0=ALU.mult, op1=ALU.mult),
                 [r2, self.rconst, rX], [rX])

    def rope(self, X, rX, c, tmps, ropet, rrope):
        S = self.S
        (s1, r1), (s2, r2) = tmps
        for t0 in range(0, TL, 512):
            xs = X[0:64, c, t0:t0 + 512]
            ps, rp = self.psum()
            S.op("pe", lambda e: e.matmul(ps[0:64, 0:512], ropet[0:64, 2 * TL:2 * TL + 64], xs, start=True, stop=True), [rX, rrope], [rp])
            S.op("pool", lambda e: e.tensor_tensor(out=s1[0:64, 0:512], in0=xs, in1=ropet[0:64, t0:t0 + 512], op=ALU.mult), [rX, rrope], [r1])
            S.op("dve", lambda e: e.tensor_tensor(out=s2[0:64, 0:512], in0=ps[0:64, 0:512], in1=ropet[0:64, TL + t0:TL + t0 + 512], op=ALU.mult), [rp, rrope], [r2])
            S.op("pool", lambda e: e.tensor_tensor(out=xs, in0=s1[0:64, 0:512], in1=s2[0:64, 0:512], op=ALU.add), [r1, r2, rX], [rX])

    def build_vaug(self, Vaug, vT, rv, H):
        S = self.S
        rV = Res()
        S.op("pool", lambda e: e.memset(Vaug[:, :, :, 64:128], 1.0), [], [rV])
        n = 0
        for h in range(H):
            for j in range(18):
                ps, rp = self.psum()
                S.op("pe", lambda e: e.transpose(ps[:, 0:64], vT[0:64, h, j * 128:(j + 1) * 128], self.cst("ident")[0:64, 0:64]), [rv, self.rconst], [rp])
                self.evac(Vaug[:, j, h, 0:64], ps[:, 0:64], [rp], [rV], n)
                n += 1
        return Vaug, rV

    def attention(self, es, l, qT, rq, kfun, rk, Vaug, rV, hv, keylist, zrow):
        S, A = self.S, self.A
        H = 4
        ident = self.cst("ident")
        OUT = self.sb(es, "attn_out", [64, 4, T])
        rOUT = Res()
        PT = [self.sb(es, "PT%d" % i, [128, 512]) for i in range(3)]
        rPT = [Res() for _ in range(3)]
        BT = [self.sb(es, "BT%d" % i, [128, 512]) for i in range(3)]
        rBT = [Res() for _ in range(3)]
        Rr = [self.sb(es, "Rr%d" % i, [64, 128]) for i in range(2)]
        rRr = [Res() for _ in range(2)]
        seq = []
        for t in range(18):
            keys = keylist(t)
            for idx, (j, pat) in enumerate(keys):
                seq.append((t, idx, j, pat, len(keys)))
        nr = [0]

        def scores(n):
            t, idx, j, pat, nk = seq[n]
            sp, rsp = self.ps[4 + n % 4], self.rps[4 + n % 4]
            pt, rpt = PT[n % 3], rPT[n % 3]
            if pat is not None:
                bt, rbt = BT[n % 3], rBT[n % 3]
                S.dma(bt[:], A["nab"][l, pat].rearrange("k h q -> k (h q)"), writes=[rbt])
                S.op("pe", lambda e: e.matmul(sp[:, 0:512], ident, bt[:], start=True, stop=False), [rbt, self.rconst], [rsp])
            for h in range(H):
                S.op("pe", lambda e: e.matmul(sp[:, h * 128:(h + 1) * 128], kfun(h, j), qT[0:64, h, t * 128:(t + 1) * 128],
                                              start=(pat is None), stop=True), [rk, rq], [rsp])
            S.op("act", lambda e: e.activation(out=pt[:], in_=sp[:, 0:512], func=AF.Exp), [rsp], [rpt])

        def pv(n):
            t, idx, j, pat, nk = seq[n]
            pt, rpt = PT[n % 3], rPT[n % 3]
            for h in range(H):
                S.op("pe", lambda e: e.matmul(self.ps[h][:, 0:128], Vaug[:, j, hv(h), :], pt[:, h * 128:(h + 1) * 128],
                                              start=(idx == 0), stop=(idx == nk - 1)), [rV, rpt], [self.rps[h]])
            if idx == nk - 1:
                for h in range(H):
                    rr_, rrr = Rr[nr[0] % 2], rRr[nr[0] % 2]
                    nr[0] += 1
                    S.op("dve", lambda e: e.reciprocal(out=rr_[0:64, :], in_=self.ps[h][64:128, 0:128]), [self.rps[h]], [rrr])
                    S.op("dve", lambda e: e.tensor_tensor(out=OUT[0:64, h, t * 128:(t + 1) * 128], in0=self.ps[h][0:64, 0:128], in1=rr_[0:64, :], op=ALU.mult),
                         [self.rps[h], rrr], [rOUT])

        scores(0)
        for n in range(len(seq)):
            if n + 1 < len(seq):
                scores(n + 1)
            pv(n)
        S.dma(A["oT"][zrow:zrow + 256, :].rearrange("(h p) t -> p h t", p=64), OUT[:], reads=[rOUT], writes=[self.roT], eng="pool")

    def mixer_a(self, b, l):
        S, A = self.S, self.A
        with ExitStack() as es:
            qT, rq = self.load_heads(es, "aq", 0, 4)
            kT, rk = self.load_heads(es, "ak", 256, 2)
            ropet = self.sb(es, "ropet", [64, 2 * TL + 64])
            rrope = Res()
            S.dma(ropet[:], A["rope"][:, :], writes=[rrope])
            Vaug = self.sb(es, "avaug", [128, 18, 2, 128])
            with ExitStack() as es2:
                vT, rv = self.load_heads(es2, "av", 384, 2)
                Vaug, rV = self.build_vaug(Vaug, vT, rv, 2)
                tmps = [(self.sb(es2, "nt%d" % i, [64, 512]), Res()) for i in range(2)]
                bd64 = self.cst("bd64")
                for h in range(4):
                    self.headnorm(qT, rq, h, tmps, bd64, self.pvec[0:64, l, PV["a_qg"][0]:PV["a_qg"][0] + 1], P=64)
                    self.rope(qT, rq, h, tmps, ropet, rrope)
                    S.op("pool", lambda e: e.tensor_scalar(out=qT[:, h, :], in0=qT[:, h, :], scalar1=0.125, scalar2=None, op0=ALU.mult), [rq], [rq])
                for h in range(2):
                    self.headnorm(kT, rk, h, tmps, bd64, self.pvec[0:64, l, PV["a_kg"][0]:PV["a_kg"][0] + 1], P=64)
                    self.rope(kT, rk, h, tmps, ropet, rrope)
                self.S.barrier()
            kfun = lambda h, j: kT[0:64, h // 2, j * 128:(j + 1) * 128]
            keylist = lambda t: [(j, None) for j in (range(18) if t < 16 else (16, 17))]
            self.attention(es, l, qT, rq, kfun, rk, Vaug, rV, lambda h: h // 2, keylist, 0)

    def mixer_b(self, b, l):
        S, A = self.S, self.A
        with ExitStack() as es:
            qT, rq = self.load_heads(es, "bq", 512, 4)
            kT, rk = self.load_heads(es, "bk", 768, 4)
            for h in range(4):
                S.op("pool", lambda e: e.tensor_scalar(out=qT[:, h, :], in0=qT[:, h, :], scalar1=0.125, scalar2=None, op0=ALU.mult), [rq], [rq])
            Vaug = self.sb(es, "bvaug", [128, 18, 4, 128])
            with ExitStack() as es2:
                vT, rv = self.load_heads(es2, "bv", 1024, 4)
                Vaug, rV = self.build_vaug(Vaug, vT, rv, 4)
                self.S.barrier()
            kfun = lambda h, j: kT[0:64, h, j * 128:(j + 1) * 128]

            def keylist(t):
                if t >= 16:
                    return [(16, None), (17, None)]
                ks = [(j, NA_TABLE[(t, j)]) for j in range(16) if (t, j) in NA_TABLE]
                return ks + [(16, None), (17, None)]

            self.attention(es, l, qT, rq, kfun, rk, Vaug, rV, lambda h: h, keylist, 256)

    def mixer_c(self, b, l):
        S, A = self.S, self.A
        ident = self.cst("ident")
        cvec = self.cst("cvec")
        with ExitStack() as eso:
            OT = self.sb(eso, "cOT", [128, 2, T])
            rOT = Res()
            es = ExitStack()
            self.load_mconsts(es)
            sel8 = self.cst("sel8")
            selp = self.cst("selp")
            QKV, rqkv = self.load_rows(es, "cqkv", 1280, 6)
            with ExitStack() as es2:
                Y = self.sb(es2, "cY", [128, T])
                rY = Res()
                tmps = [(self.sb(es2, "cnt%d" % i, [128, 512]), Res()) for i in range(2)]
                co = PV["c_conv"][0]
                for ch in range(6):
                    w = lambda tap: self.pvec[:, l, co + ch * 3 + tap:co + ch * 3 + tap + 1]
                    X = QKV[:, ch, :]
                    S.op("dve", lambda e: e.tensor_scalar(out=Y[:], in0=X, scalar1=w(1), scalar2=None, op0=ALU.mult), [rqkv, self.rconst], [rY])
                    for (a, n) in ((0, TL), (TL, TC)):
                        S.op("dve", lambda e: e.scalar_tensor_tensor(out=Y[:, a + 1:a + n], in0=QKV[:, ch, a:a + n - 1], scalar=w(0), in1=Y[:, a + 1:a + n],
                                                                     op0=ALU.mult, op1=ALU.add), [rqkv, rY, self.rconst], [rY])
                        S.op("dve", lambda e: e.scalar_tensor_tensor(out=Y[:, a:a + n - 1], in0=QKV[:, ch, a + 1:a + n], scalar=w(2), in1=Y[:, a:a + n - 1],
                                                                     op0=ALU.mult, op1=ALU.add), [rqkv, rY, self.rconst], [rY])
                    S.op("act", lambda e: e.activation(out=QKV[:, ch, :], in_=Y[:], func=AF.Silu), [rY, rqkv], [rqkv])
                for ch in range(4):
                    self.headnorm(QKV, rqkv, ch, tmps, self.cst("bd64one"), 0.125 if ch < 2 else 1.0)
                S.barrier()
            B8 = self.sb(es, "cB8", [8, T])
            G8 = self.sb(es, "cG8", [8, T])
            TOT8 = self.sb(es, "cTOT8", [8, 36])
            ETOT8 = self.sb(es, "cETOT8", [8, 36])
            nal = self.sb(es, "cnal", [8, 1])
            TOK = self.sb(es, "cTOK", [64, 36, 48])
            ETOTP = self.sb(es, "cETOTP", [128, 4, 36])
            es3 = ExitStack()
            cm = self.sb(es3, "ccm", [8, T])
            rcm = Res()
            S.dma(cm[:], A["cmask"][0:8, :], writes=[rcm])
            A8 = self.sb(es3, "cA8", [8, T])
            X8 = self.sb(es3, "cX8", [8, T])
            STK = self.sb(es3, "cSTK", [48, T])
            r8 = Res()
            S.dma(B8[:], A["projT"][2048:2056, :], reads=[self.rprojT], writes=[r8])
            S.dma(A8[:], A["projT"][2056:2064, :], reads=[self.rprojT], writes=[r8])
            dtb = self.pvec[0:8, l, PV["c_dtb"][0]:PV["c_dtb"][0] + 1]
            alog = self.pvec[0:8, l, PV["c_alog"][0]:PV["c_alog"][0] + 1]
            o8 = lambda eng, fn: S.op(eng, fn, [r8, self.rconst, rcm], [r8])
            o8("act", lambda e: e.activation(out=B8[:], in_=B8[:], func=AF.Sigmoid))
            o8("act", lambda e: e.activation(out=nal[:], in_=alog, func=AF.Exp))
            o8("dve", lambda e: e.tensor_scalar(out=nal[:], in0=nal[:], scalar1=-1.0, scalar2=None, op0=ALU.mult))
            o8("dve", lambda e: e.tensor_scalar(out=A8[:], in0=A8[:], scalar1=dtb, scalar2=None, op0=ALU.add))
            o8("act", lambda e: e.activation(out=A8[:], in_=A8[:], func=AF.Exp))
            o8("dve", lambda e: e.tensor_scalar(out=A8[:], in0=A8[:], scalar1=1.0, scalar2=None, op0=ALU.add))
            o8("act", lambda e: e.activation(out=A8[:], in_=A8[:], func=AF.Ln))
            o8("dve", lambda e: e.tensor_scalar(out=A8[:], in0=A8[:], scalar1=nal[:, 0:1], scalar2=None, op0=ALU.mult))
            o8("dve", lambda e: e.tensor_tensor_scan(out=G8[:], data0=cm[:], data1=A8[:], initial=0.0, op0=ALU.mult, op1=ALU.add))
            o8("dve", lambda e: e.tensor_reduce(out=TOT8[:], in_=A8[:].rearrange("p (c i) -> p c i", i=64), axis=AX.X, op=ALU.add))
            o8("dve", lambda e: e.tensor_tensor(out=X8[:], in0=A8[:], in1=G8[:], op=ALU.subtract))
            for c in range(36):
                o8("dve", lambda e: e.tensor_scalar(out=X8[:, c * 64:(c + 1) * 64], in0=X8[:, c * 64:(c + 1) * 64], scalar1=TOT8[:, c:c + 1], scalar2=None, op0=ALU.add))
            o8("dve", lambda e: e.tensor_scalar(out=G8[:], in0=G8[:], scalar1=cvec[0:8, 2:3], scalar2=None, op0=ALU.mult))
            o8("dve", lambda e: e.scalar_tensor_tensor(out=G8[:], in0=X8[:], scalar=cvec[0:8, 3:4], in1=G8[:], op0=ALU.mult, op1=ALU.add))
            o8("act", lambda e: e.activation(out=ETOT8[:], in_=TOT8[:], func=AF.Exp))
            o8("act", lambda e: e.activation(out=A8[:], in_=G8[:], func=AF.Exp))
            S.dma(STK[0:8, :], G8[:], reads=[r8], writes=[r8])
            S.dma(STK[24:32, :], B8[:], reads=[r8], writes=[r8])
            S.dma(STK[32:40, :], A8[:], reads=[r8], writes=[r8])
            o8("dve", lambda e: e.tensor_scalar(out=X8[:], in0=B8[:], scalar1=-1.0, scalar2=None, op0=ALU.mult))
            S.dma(STK[8:16, :], X8[:], reads=[r8], writes=[r8])
            o8("dve", lambda e: e.tensor_tensor(out=X8[:], in0=B8[:], in1=A8[:], op=ALU.mult))
            S.dma(STK[16:24, :], X8[:], reads=[r8], writes=[r8])
            for c in range(36):
                o8("dve", lambda e: e.tensor_scalar(out=X8[:, c * 64:(c + 1) * 64], in0=G8[:, c * 64:(c + 1) * 64], scalar1=TOT8[:, c:c + 1], scalar2=None, op0=ALU.subtract))
            o8("act", lambda e: e.activation(out=X8[:], in_=X8[:], func=AF.Exp, scale=-1.0))
            S.dma(STK[40:48, :], X8[:], reads=[r8], writes=[r8])
            rtok = Res()
            for c in range(36):
                ps, rp = self.psum()
                S.op("pe", lambda e: e.transpose(ps[0:64, 0:48], STK[0:48, c * 64:(c + 1) * 64], ident[0:48, 0:48]), [r8, self.rconst], [rp])
                self.evac(TOK[:, c, :], ps[0:64, 0:48], [rp], [rtok], c)
            for i in range(4):
                ps, rp = self.psum()
                S.op("pe", lambda e: e.matmul(ps[:, 0:36], selp[0:8, i * 128:(i + 1) * 128], ETOT8[:], start=True, stop=True), [r8, self.rconst], [rp])
                S.op("act", lambda e: e.activation(out=ETOTP[:, i, :], in_=ps[:, 0:36], func=AF.Copy), [rp], [rtok])
            S.barrier()
            es3.close()
            Otok = self.sb(es, "cOtok", [64, 36, 256])
            rOtok = [Res() for _ in range(36)]
            mk = lambda nm, shape=(64, 256): (self.sb(es, nm, list(shape)), Res())
            def mktiles():
                t = {}
                t['Ktok, rKt'] = mk("cKtok")
                t['Vtok, rVt'] = mk("cVtok")
                t['Xt, rXt'] = mk("cXt")
                t['Xp, rXp'] = mk("cXp")
                t['Dt, rDt'] = mk("cDt")
                t['DTt, rDTt'] = mk("cDTt")
                t['Mt'] = [mk("cM%d" % i) for i in range(2)]
                t['Nt'] = [mk("cN%d" % i) for i in range(2)]
                t['Pt, rPt'] = mk("cP")
                t['qkT, rqk'] = mk("cqkT")
                t['rhsm, rrh'] = mk("crhs", (64, 512))
                t['wT, rwT'] = mk("cwT", (128, 256))
                t['u0, ru0'] = mk("cu0")
                t['ut, rut'] = mk("cu")
                t['o2, ro2'] = mk("co2")
                t['otmp, rotmp'] = mk("cotmp")
                t['Kd, rKd'] = mk("cKd")
                t['KTm, rKTm'] = mk("cKTm", (128, 256))
                t['QTm, rQTm'] = mk("cQTm", (128, 256))
                return t
            TD = [mktiles() for _ in range(2)]
            Sts = [self.sb(es, "cS%d" % i, [128, 2, 128]) for i in range(2)]
            rSs = [Res() for _ in range(2)]
            hm64 = self.cst("hm64")
            wmask = self.cst("wmask")
            HB = lambda h: slice(h * 64, (h + 1) * 64)
            id4 = self.cst("ident4")[0:64, :]
            S.op("pool", lambda e: e.memset(Otok[:], 0.0), [], rOtok)
            for d in range(2):
                S.op("pool", lambda e: e.memset(Sts[d][:], 0.0), [], [rSs[d]])

            def step(d, c):
                t = TD[d]
                Ktok, rKt = t['Ktok, rKt']
                Vtok, rVt = t['Vtok, rVt']
                Xt, rXt = t['Xt, rXt']
                Xp, rXp = t['Xp, rXp']
                Dt, rDt = t['Dt, rDt']
                DTt, rDTt = t['DTt, rDTt']
                Mt = t['Mt']
                Nt = t['Nt']
                Pt, rPt = t['Pt, rPt']
                qkT, rqk = t['qkT, rqk']
                rhsm, rrh = t['rhsm, rrh']
                wT, rwT = t['wT, rwT']
                u0, ru0 = t['u0, ru0']
                ut, rut = t['ut, rut']
                o2, ro2 = t['o2, ro2']
                otmp, rotmp = t['otmp, rotmp']
                Kd, rKd = t['Kd, rKd']
                KTm, rKTm = t['KTm, rKTm']
                QTm, rQTm = t['QTm, rQTm']
                St, rS = Sts[d], rSs[d]
                ms = self.cst("gdn_ms")[0:64, d * 256:(d + 1) * 256]
                nms = self.cst("gdn_nms")[0:64, d * 256:(d + 1) * 256]
                qi = self.cst("gdn_qi")[0:64, d * 256:(d + 1) * 256]
                cs = slice(c * 64, (c + 1) * 64)
                tk = lambda grp, h: TOK[:, c, grp * 8 + d * 4 + h:grp * 8 + d * 4 + h + 1]
                pk, rpk = self.psum_chain(d)
                pv, rpv = self.psum_chain(d)
                for hc in range(2):
                    S.op("pe", lambda e: e.transpose(pk[0:64, hc * 128:(hc + 1) * 128], QKV[:, 2 + hc, cs], ident), [rqkv, self.rconst], [rpk])
                    S.op("pe", lambda e: e.transpose(pv[0:64, hc * 128:(hc + 1) * 128], QKV[:, 4 + hc, cs], ident), [rqkv, self.rconst], [rpv])
                S.op("act", lambda e: e.activation(out=Ktok[:], in_=pk[0:64, 0:256], func=AF.Copy), [rpk], [rKt])
                S.op("dve", lambda e: e.tensor_copy(out=Vtok[:], in_=pv[0:64, 0:256]), [rpv], [rVt])
                yield
                pkk, rpkk = self.psum_chain(d)
                pqk, rpqk = self.psum_chain(d)
                pg, rpg = self.psum_chain(d)
                pb, rpb = self.psum_chain(d)
                for h in range(4):
                    S.op("pool", lambda e: e.tensor_scalar(out=KTm[:, HB(h)], in0=QKV[:, 2 + h // 2, cs], scalar1=hm64[:, h % 2:h % 2 + 1], scalar2=None, op0=ALU.mult),
                         [rqkv, self.rconst], [rKTm])
                    S.op("pool", lambda e: e.tensor_scalar(out=QTm[:, HB(h)], in0=QKV[:, h // 2, cs], scalar1=hm64[:, h % 2:h % 2 + 1], scalar2=None, op0=ALU.mult),
                         [rqkv, self.rconst], [rQTm])
                for h in range(4):
                    S.op("pe", lambda e: e.matmul(pkk[0:64, HB(h)], KTm[:, HB(h)], QKV[:, 2 + h // 2, cs], start=True, stop=True), [rqkv, rKTm], [rpkk])
                    S.op("pe", lambda e: e.matmul(pqk[0:64, HB(h)], KTm[:, HB(h)], QKV[:, h // 2, cs], start=True, stop=True), [rqkv, rKTm], [rpqk])
                    S.op("pe", lambda e: e.matmul(pg[0:64, HB(h)], sel8[0:8, HB(d * 4 + h)], G8[:, cs], start=True, stop=True), [r8, self.rconst], [rpg])
                    S.op("pe", lambda e: e.matmul(pb[0:64, HB(h)], sel8[0:8, HB(d * 4 + h)], B8[:, cs], start=True, stop=True), [r8, self.rconst], [rpb])
                yield
                for h in range(4):
                    S.op("dve", lambda e: e.tensor_scalar(out=Xt[:, HB(h)], in0=pg[0:64, HB(h)], scalar1=tk(0, h), scalar2=None, op0=ALU.subtract), [rpg, rtok], [rXt])
                S.op("dve", lambda e: e.tensor_scalar(out=Xp[:], in0=Xt[:], scalar1=0.0, scalar2=None, op0=ALU.max), [rXt], [rXp])
                S.op("pool", lambda e: e.tensor_tensor(out=Xt[:], in0=Xt[:], in1=Xp[:], op=ALU.subtract), [rXt, rXp], [rXt])
                yield
                S.op("act", lambda e: e.activation(out=Dt[:], in_=Xp[:], func=AF.Exp, scale=-1.0), [rXp], [rDt])
                S.op("act", lambda e: e.activation(out=DTt[:], in_=Xt[:], func=AF.Exp), [rXt], [rDTt])
                yield
                (M, rM), (N, rN) = Mt[0], Nt[0]
                S.op("dve", lambda e: e.tensor_tensor(out=Dt[:], in0=Dt[:], in1=pkk[0:64, 0:256], op=ALU.mult), [rDt, rpkk], [rDt])
                for h in range(4):
                    S.op("dve", lambda e: e.scalar_tensor_tensor(out=M[:, HB(h)], in0=Dt[:, HB(h)], scalar=tk(1, h), in1=ms[:, HB(h)], op0=ALU.mult, op1=ALU.mult),
                         [rDt, rtok, self.rconst], [rM])
                S.op("dve", lambda e: e.tensor_tensor(out=qkT[:], in0=DTt[:], in1=pqk[0:64, 0:256], op=ALU.mult), [rDTt, rpqk], [rqk])
                S.op("pool", lambda e: e.tensor_tensor(out=qkT[:], in0=qkT[:], in1=qi, op=ALU.mult), [rqk, self.rconst], [rqk])
                S.op("dve", lambda e: e.tensor_tensor(out=DTt[:], in0=DTt[:], in1=pkk[0:64, 0:256], op=ALU.mult), [rDTt, rpkk], [rDTt])
                S.op("dve", lambda e: e.tensor_tensor(out=DTt[:], in0=DTt[:], in1=pb[0:64, 0:256], op=ALU.mult), [rDTt, rpb], [rDTt])
                S.op("pool", lambda e: e.tensor_tensor(out=N[:], in0=DTt[:], in1=nms, op=ALU.mult), [rDTt, self.rconst], [rN])
                S.op("pool", lambda e: e.tensor_tensor(out=Pt[:], in0=N[:], in1=id4, op=ALU.add), [rN, self.rconst], [rPt])
                yield
                for lev in range(5):
                    (M, rM), (N, rN) = Mt[lev % 2], Nt[lev % 2]
                    (M2, rM2), (N2, rN2) = Mt[(lev + 1) % 2], Nt[(lev + 1) % 2]
                    pm, rpm = self.psum_chain(d)
                    for h in range(4):
                        S.op("pe", lambda e: e.matmul(pm[0:64, HB(h)], N[:, HB(h)], M[:, HB(h)], start=True, stop=True), [rM, rN], [rpm])
                    if lev < 4:
                        pn, rpn = self.psum_chain(d)
                        for h in range(4):
                            S.op("pe", lambda e: e.matmul(pn[0:64, HB(h)], M[:, HB(h)], N[:, HB(h)], start=True, stop=True), [rM, rN], [rpn])
                        S.op("dve", lambda e: e.tensor_copy(out=N2[:], in_=pn[0:64, 0:256]), [rpn], [rN2])
                    S.op("act", lambda e: e.activation(out=M2[:], in_=pm[0:64, 0:256], func=AF.Copy), [rpm], [rM2])
                    yield
                    pp, rpp = self.psum_chain(d)
                    for h in range(4):
                        S.op("pe", lambda e: e.matmul(pp[0:64, HB(h)], M2[:, HB(h)], Pt[:, HB(h)], start=True, stop=True), [rM2, rPt], [rpp])
                    S.op("dve", lambda e: e.tensor_tensor(out=Pt[:], in0=Pt[:], in1=pp[0:64, 0:256], op=ALU.add), [rPt, rpp], [rPt])
                    yield
                for h in range(4):
                    ko, vo = (0, 64) if h % 2 == 0 else (64, 0)
                    S.op("dve", lambda e: e.tensor_scalar(out=rhsm[:, h * 128 + ko:h * 128 + ko + 64], in0=Ktok[:, HB(h)], scalar1=tk(2, h), scalar2=None, op0=ALU.mult),
                         [rKt, rtok], [rrh])
                    S.op("pool", lambda e: e.tensor_scalar(out=rhsm[:, h * 128 + vo:h * 128 + vo + 64], in0=Vtok[:, HB(h)], scalar1=tk(3, h), scalar2=None, op0=ALU.mult),
                         [rVt, rtok], [rrh])
                yield
                pst, rpst = self.psum_chain(d)
                pso, rpso = self.psum_chain(d)
                for h in range(4):
                    S.op("pe", lambda e: e.matmul(pst[:, HB(h)], rhsm[:, h * 128:(h + 1) * 128], Pt[:, HB(h)], start=True, stop=True), [rrh, rPt], [rpst])
                    S.op("pe", lambda e: e.matmul(pso[0:64, h * 128:(h + 1) * 128], Pt[:, HB(h)], rhsm[:, h * 128:(h + 1) * 128], start=True, stop=True), [rrh, rPt], [rpso])
                S.op("dve", lambda e: e.tensor_tensor(out=wT[:], in0=pst[:, 0:256], in1=wmask, op=ALU.mult), [rpst, self.rconst], [rwT])
                for h in range(4):
                    vo = 64 if h % 2 == 0 else 0
                    S.op("dve", lambda e: e.tensor_copy(out=u0[:, HB(h)], in_=pso[0:64, h * 128 + vo:h * 128 + vo + 64]), [rpso], [ru0])
                yield
                pw, rpw = self.psum_chain(d)
                po1, rpo1 = self.psum_chain(d)
                for h in range(4):
                    p0 = (h % 2) * 64
                    Sh = St[:, h // 2, p0:p0 + 64]
                    S.op("pe", lambda e: e.matmul(pw[0:64, HB(h)], wT[:, HB(h)], Sh, start=True, stop=True), [rwT, rS], [rpw])
                    S.op("pe", lambda e: e.matmul(po1[0:64, HB(h)], QTm[:, HB(h)], Sh, start=True, stop=True), [rQTm, rS], [rpo1])
                S.op("dve", lambda e: e.tensor_tensor(out=ut[:], in0=u0[:], in1=pw[0:64, 0:256], op=ALU.subtract), [ru0, rpw], [rut])
                yield
                po2, rpo2 = self.psum_chain(d)
                for h in range(4):
                    S.op("pe", lambda e: e.matmul(po2[0:64, HB(h)], qkT[:, HB(h)], ut[:, HB(h)], start=True, stop=True), [rqk, rut], [rpo2])
                S.op("act", lambda e: e.activation(out=o2[:], in_=po2[0:64, 0:256], func=AF.Copy), [rpo2], [ro2])
                yield
                for h in range(4):
                    S.op("dve", lambda e: e.scalar_tensor_tensor(out=otmp[:, HB(h)], in0=po1[0:64, HB(h)], scalar=tk(4, h), in1=o2[:, HB(h)], op0=ALU.mult, op1=ALU.add),
                         [rpo1, ro2, rtok], [rotmp])
                S.op("pool", lambda e: e.tensor_tensor(out=Otok[:, c, :], in0=Otok[:, c, :], in1=otmp[:], op=ALU.add), [rotmp, rOtok[c]], [rOtok[c]])
                yield
                for h in range(4):
                    S.op("pool", lambda e: e.tensor_scalar(out=Kd[:, HB(h)], in0=Ktok[:, HB(h)], scalar1=tk(5, h), scalar2=None, op0=ALU.mult), [rKt, rtok], [rKd])
                for hc in range(2):
                    pS, rpS = self.psum_chain(d)
                    S.op("pe", lambda e: e.matmul(pS[:, 0:128], Kd[:, hc * 128:(hc + 1) * 128], ut[:, hc * 128:(hc + 1) * 128], start=True, stop=True), [rKd, rut], [rpS])
                    S.op("dve", lambda e: e.scalar_tensor_tensor(out=St[:, hc, :], in0=St[:, hc, :], scalar=ETOTP[:, d * 2 + hc, c:c + 1], in1=pS[:, 0:128],
                                                                 op0=ALU.mult, op1=ALU.add), [rS, rpS, rtok], [rS])

            orders = [list(range(32, 36)) + list(range(32)), list(range(35, 31, -1)) + list(range(31, -1, -1))]
            for n in range(36):
                gens = [step(d, orders[d][n]) for d in range(2)]
                while gens:
                    for g in list(gens):
                        try:
                            next(g)
                        except StopIteration:
                            gens.remove(g)
            n = 0
            for c in range(36):
                for hc in range(2):
                    ps, rp = self.psum()
                    S.op("pe", lambda e: e.transpose(ps[:, 0:64], Otok[:, c, hc * 128:(hc + 1) * 128], ident[0:64, 0:64]), [rOtok[c], self.rconst], [rp])
                    self.evac(OT[:, hc, c * 64:(c + 1) * 64], ps[:, 0:64], [rp], [rOT], n)
                    n += 1
            S.barrier()
            es.close()
            self.gated_tail(eso, [OT, None], [rOT, None], l, "c_og", 2064, 512)


    def mixer_d(self, b, l):
        S, A = self.S, self.A
        ident = self.cst("ident")
        with ExitStack() as eso:
            OTs = [self.sb(eso, "dOT%d" % i, [128, 2, T]) for i in range(2)]
            es = ExitStack()
            self.load_mconsts(es)
            gmask = self.cst("gla_mask")
            hm32 = self.cst("hmask32")
            sblk = self.cst("sblk")
            qT, rq = self.load_rows(es, "dq", 2320, 1)
            kT, rk = self.load_rows(es, "dk", 2448, 1)
            Vtok = self.sb(es, "dvtok", [64, 36, 256])
            rVt = Res()
            with ExitStack() as es2:
                vT, rv = self.load_rows(es2, "dv", 2576, 2)
                n = 0
                for c2 in range(2):
                    for c in range(36):
                        ps, rp = self.psum()
                        S.op("pe", lambda e: e.transpose(ps[0:64, 0:128], vT[:, c2, c * 64:(c + 1) * 64], ident), [rv, self.rconst], [rp])
                        self.evac(Vtok[:, c, c2 * 128:(c2 + 1) * 128], ps[0:64, 0:128], [rp], [rVt], n)
                        n += 1
                S.barrier()
            rOTs = [Res() for _ in range(2)]
            cm = self.sb(es, "cm", [128, T])
            rcm = Res()
            S.dma(cm[:], A["cmask"][:, :], writes=[rcm])
            LA = self.sb(es, "dLA", [128, T])
            G = self.sb(es, "dG", [128, T])
            QE = self.sb(es, "dQE", [128, T])
            KE = self.sb(es, "dKE", [128, T])
            KH = self.sb(es, "dKH", [128, T])
            TOT = self.sb(es, "dTOT", [128, 36])
            ETOT = self.sb(es, "dETOT", [128, 36])
            lr = self.sb(es, "dlr", [16, T])
            gw = self.sb(es, "dgw", [16, 128])
            St = self.sb(es, "dS", [128, 256])
            at = [self.sb(es, "dat%d" % i, [64, 256]) for i in range(2)]
            ktok = [self.sb(es, "dktok%d" % i, [64, 128]) for i in range(2)]
            KEm = [self.sb(es, "dKEm%d" % i, [128, 4, 64]) for i in range(2)]
            rKEm = [Res() for _ in range(2)]
            rw = Res()
            rS = Res()
            rat = [Res() for _ in range(2)]
            rkt = [Res() for _ in range(2)]
            gbo = PV["d_gb"][0]
            for d in range(2):
                OT, rOT = OTs[d], rOTs[d]
                S.dma(lr[:], A["projT"][2832 + 16 * d:2848 + 16 * d, :], reads=[self.rprojT], writes=[rw])
                S.dma(gw[:], A["d_gw"][l, d], writes=[rw])
                for t0 in range(0, T, 512):
                    tn = min(512, T - t0)
                    ps, rp = self.psum()
                    S.op("pe", lambda e: e.matmul(ps[:, 0:tn], gw[:], lr[:, t0:t0 + tn], start=True, stop=True), [rw], [rp])
                    S.op("dve", lambda e: e.tensor_scalar(out=LA[:, t0:t0 + tn], in0=ps[:, 0:tn], scalar1=self.pvec[:, l, gbo + d:gbo + d + 1], scalar2=None,
                                                          op0=ALU.add), [rp, self.rconst], [rw])
                S.op("act", lambda e: e.activation(out=LA[:], in_=LA[:], func=AF.Exp, scale=-1.0), [rw], [rw])
                S.op("pool", lambda e: e.tensor_scalar(out=LA[:], in0=LA[:], scalar1=1.0, scalar2=None, op0=ALU.add), [rw], [rw])
                S.op("act", lambda e: e.activation(out=LA[:], in_=LA[:], func=AF.Ln), [rw], [rw])
                S.op("pool", lambda e: e.tensor_scalar(out=LA[:], in0=LA[:], scalar1=-1.0 / 16.0, scalar2=None, op0=ALU.mult), [rw], [rw])
                S.op("dve", lambda e: e.tensor_tensor_scan(out=G[:], data0=cm[:], data1=LA[:], initial=0.0, op0=ALU.mult, op1=ALU.add), [rw, rcm], [rw])
                S.op("dve", lambda e: e.tensor_reduce(out=TOT[:], in_=LA[:].rearrange("p (c i) -> p c i", i=64), axis=AX.X, op=ALU.add), [rw], [rw])
                if d == 1:
                    S.op("pool", lambda e: e.tensor_tensor(out=G[:], in0=LA[:], in1=G[:], op=ALU.subtract), [rw], [rw])
                    for c in range(36):
                        S.op("dve", lambda e: e.tensor_scalar(out=G[:, c * 64:(c + 1) * 64], in0=G[:, c * 64:(c + 1) * 64], scalar1=TOT[:, c:c + 1], scalar2=None,
                                                              op0=ALU.add), [rw], [rw])
                S.op("act", lambda e: e.activation(out=QE[:], in_=G[:], func=AF.Exp), [rw], [rw])
                S.op("dve", lambda e: e.scalar_tensor_tensor(out=QE[:], in0=QE[:], scalar=32.0 ** -0.5, in1=qT[:, 0, :], op0=ALU.mult, op1=ALU.mult), [rw, rq], [rw])
                S.op("act", lambda e: e.activation(out=KE[:], in_=G[:], func=AF.Exp, scale=-1.0), [rw], [rw])
                S.op("pool", lambda e: e.tensor_tensor(out=KE[:], in0=KE[:], in1=kT[:, 0, :], op=ALU.mult), [rw, rk], [rw])
                for c in range(36):
                    S.op("dve", lambda e: e.tensor_scalar(out=KH[:, c * 64:(c + 1) * 64], in0=G[:, c * 64:(c + 1) * 64], scalar1=TOT[:, c:c + 1], scalar2=None,
                                                          op0=ALU.subtract), [rw], [rw])
                S.op("act", lambda e: e.activation(out=KH[:], in_=KH[:], func=AF.Exp, scale=-1.0), [rw], [rw])
                S.op("pool", lambda e: e.tensor_tensor(out=KH[:], in0=KH[:], in1=kT[:, 0, :], op=ALU.mult), [rw, rk], [rw])
                S.op("act", lambda e: e.activation(out=ETOT[:], in_=TOT[:], func=AF.Exp), [rw], [rw])
                S.op("pool", lambda e: e.memset(St[:], 0.0), [], [rS])
                order = (list(range(32, 36)) + list(range(32))) if d == 0 else (list(range(35, 31, -1)) + list(range(31, -1, -1)))
                for n, c in enumerate(order):
                    cs = slice(c * 64, (c + 1) * 64)
                    a_, ra = at[n % 2], rat[n % 2]
                    k_, rk_ = ktok[n % 2], rkt[n % 2]
                    pa, rpa = self.psum()
                    kem, rkem = KEm[n % 2], rKEm[n % 2]
                    for h in range(4):
                        S.op("pool", lambda e: e.tensor_scalar(out=kem[:, h, :], in0=KE[:, cs], scalar1=hm32[:, h:h + 1], scalar2=None, op0=ALU.mult), [rw, self.rconst], [rkem])
                    for h in range(4):
                        S.op("pe", lambda e: e.matmul(pa[0:64, h * 64:(h + 1) * 64], kem[:, h, :], QE[:, cs], start=True, stop=True), [rw, rkem], [rpa])
                    S.op("dve", lambda e: e.tensor_tensor(out=a_[:], in0=pa[0:64, 0:256], in1=gmask[0:64, d * 256:(d + 1) * 256], op=ALU.mult), [rpa, self.rconst], [ra])
                    pk, rpk = self.psum()
                    S.op("pe", lambda e: e.transpose(pk[0:64, 0:128], KH[:, cs], ident), [rw, self.rconst], [rpk])
                    S.op("act", lambda e: e.activation(out=k_[:], in_=pk[0:64, 0:128], func=AF.Copy), [rpk], [rk_])
                    po, rpo = self.psum()
                    for h in range(4):
                        hs = slice(h * 32, (h + 1) * 32)
                        es_ = slice(h * 64, (h + 1) * 64)
                        S.op("pe", lambda e: e.matmul(po[0:64, es_], St[:, es_], QE[:, cs], start=True, stop=False), [rS, rw], [rpo])
                        S.op("pe", lambda e: e.matmul(po[0:64, es_], Vtok[:, c, es_], a_[:, es_], start=False, stop=True), [rVt, ra], [rpo])
                    for h in range(4):
                        p0 = (h % 2) * 64
                        S.op("act", lambda e: e.activation(out=OT[p0:p0 + 64, h // 2, cs], in_=po[0:64, h * 64:(h + 1) * 64], func=AF.Copy), [rpo], [rOT])
                    pS, rpS = self.psum()
                    S.op("pe", lambda e: e.matmul(pS[:, 0:256], k_[:], Vtok[:, c, :], start=True, stop=True), [rk_, rVt], [rpS])
                    S.op("dve", lambda e: e.scalar_tensor_tensor(out=St[:], in0=St[:], scalar=ETOT[:, c:c + 1], in1=pS[:, 0:256], op0=ALU.mult, op1=ALU.add),
                         [rS, rpS, rw], [rS])
                    S.op("dve", lambda e: e.tensor_tensor(out=St[:], in0=St[:], in1=sblk, op=ALU.mult), [rS, self.rconst], [rS])
            S.barrier()
            es.close()
            self.gated_tail(eso, OTs, rOTs, l, "d_og", 2864, 768)

    def gated_tail(self, es, OTs, rOTs, l, gname, gate_row, zrow):
        S, A = self.S, self.A
        OT, rOT = OTs[0], rOTs[0]
        tmps = [(self.sb(es, "gt%d" % i, [128, 512]), Res()) for i in range(2)]
        gate, rg = self.load_rows(es, "gate", gate_row, 2)
        S.op("act", lambda e: e.activation(out=gate[:], in_=gate[:], func=AF.Silu), [rg], [rg])
        go = PV[gname][0]
        for c in range(2):
            if OTs[1] is not None:
                S.op("pool", lambda e: e.tensor_tensor(out=OT[:, c, :], in0=OT[:, c, :], in1=OTs[1][:, c, :], op=ALU.add), [rOT, rOTs[1]], [rOT])
            self.headnorm(OT, rOT, c, tmps, self.cst("bd64"), self.pvec[:, l, go:go + 1])
            S.op("pool", lambda e: e.tensor_tensor(out=OT[:, c, :], in0=OT[:, c, :], in1=gate[:, c, :], op=ALU.mult), [rOT, rg], [rOT])
        S.dma(A["oT"][zrow:zrow + 256, :].rearrange("(c p) t -> p c t", p=128), OT[:], reads=[rOT], writes=[self.roT], eng="pool")


    def layer_norm(self, es_tiles, l, t0, tn, gname, bname):
        S = self.S
        sq, rsq, rstd, rrstd = es_tiles
        onesln = self.cst("onesln")
        mean_ps, rmp = self.psum()
        for k in range(8):
            S.op("pe", lambda e, k=k: e.matmul(mean_ps[:, 0:tn], onesln, self.xT[:, k, t0:t0 + tn], start=(k == 0), stop=(k == 7)),
                 [self.rconst] + self.rxs(k, t0, tn), [rmp])
        for k in range(8):
            S.op("dve", lambda e, k=k: e.tensor_tensor(out=self.xT[:, k, t0:t0 + tn], in0=self.xT[:, k, t0:t0 + tn], in1=mean_ps[:, 0:tn],
                                                     op=ALU.subtract), [rmp] + self.rxs(k, t0, tn), self.rxs(k, t0, tn))
        var_ps, rvp = self.psum()
        for k in range(8):
            s_, r_ = sq[k % 2], rsq[k % 2]
            S.op("pool", lambda e, k=k, s_=s_: e.tensor_tensor(out=s_[:, 0:tn], in0=self.xT[:, k, t0:t0 + tn], in1=self.xT[:, k, t0:t0 + tn], op=ALU.mult),
                 self.rxs(k, t0, tn), [r_])
            S.op("pe", lambda e, k=k, s_=s_: e.matmul(var_ps[:, 0:tn], onesln, s_[:, 0:tn], start=(k == 0), stop=(k == 7)),
                 [r_, self.rconst], [rvp])
        epsc = self.cst("cvec")[:, 0:1]
        S.op("dve", lambda e: e.tensor_scalar(out=rstd[:, 0:tn], in0=var_ps[:, 0:tn], scalar1=EPS, scalar2=None, op0=ALU.add), [rvp], [rrstd])
        S.op("act", lambda e: e.activation(out=rstd[:, 0:tn], in_=rstd[:, 0:tn], func=AF.Sqrt), [rrstd], [rrstd])
        S.op("dve", lambda e: e.reciprocal(out=rstd[:, 0:tn], in_=rstd[:, 0:tn]), [rrstd], [rrstd])
        go, bo = PV[gname][0], PV[bname][0]
        for k in range(8):
            S.op("dve", lambda e, k=k: e.scalar_tensor_tensor(out=self.xT[:, k, t0:t0 + tn], in0=self.xT[:, k, t0:t0 + tn],
                                                            scalar=self.pvec[:, l, go + k:go + k + 1], in1=rstd[:, 0:tn],
                                                            op0=ALU.mult, op1=ALU.mult), [rrstd, self.rconst] + self.rxs(k, t0, tn), self.rxs(k, t0, tn))
            S.op("pool", lambda e, k=k: e.tensor_scalar(out=self.xT[:, k, t0:t0 + tn], in0=self.xT[:, k, t0:t0 + tn],
                                                      scalar1=self.pvec[:, l, bo + k:bo + k + 1], scalar2=None, op0=ALU.add),
                 self.rxs(k, t0, tn) + [self.rconst], self.rxs(k, t0, tn))

    def phase_merge(self, b, l):
        nc, S, A = self.nc, self.S, self.A
        TB = 256
        with ExitStack() as es:
            wbr = self.sb(es, "wbr", [128, 8, D])
            rwbr = Res()
            S.dma(wbr[:], A["w_branch"][l].rearrange("z (c p) d -> p (z c) d", p=128), writes=[rwbr])
            wo = self.sb(es, "wo", [128, 8, D])
            rwo = Res()
            S.dma(wo[:], A["w_out"][l].rearrange("(k p) d -> p k d", p=128), writes=[rwo])
            oTb = [self.sb(es, "oTb%d" % i, [128, 8, TB]) for i in range(2)]
            roTb = [Res() for _ in range(2)]
            G = [self.sb(es, "G%d" % i, [128, 4, TB]) for i in range(2)]
            rG = [Res() for _ in range(2)]
            m = self.sb(es, "m", [128, 8, TB])
            rm = [Res() for _ in range(8)]
            tmp = [self.sb(es, "mtmp%d" % i, [128, TB]) for i in range(2)]
            rtmp = [Res() for _ in range(2)]
            sq = [self.sb(es, "lnsq%d" % i, [128, TB]) for i in range(2)]
            rsq = [Res() for _ in range(2)]
            rstd = self.sb(es, "lnrstd", [128, TB])
            rrstd = Res()
            ng = 0
            nt = 0
            for bi in range(T // TB):
                t0 = bi * TB
                col = b if t0 < TL else 2
                ob, rob = oTb[bi % 2], roTb[bi % 2]
                S.dma(ob[:], A["oT"][:, t0:t0 + TB].rearrange("(c p) t -> p c t", p=128), reads=[self.roT], writes=[rob])
                for dmc in range(8):
                    g, rg = G[ng % 2], rG[ng % 2]
                    ng += 1
                    S.dma(g[:], A["projT"][3120:7216, t0:t0 + TB].rearrange("(z c p) t -> c p z t", z=4, p=128)[dmc],
                          reads=[self.rprojT], writes=[rg])
                    S.op("act", lambda e, g=g: e.activation(out=g[:], in_=g[:], func=AF.Sigmoid), [rg], [rg])
                    for z in range(4):
                        ps, rp = self.psum()
                        for c2 in range(2):
                            S.op("pe", lambda e, ps=ps, z=z, c2=c2, dmc=dmc, ob=ob: e.matmul(ps[:, 0:TB], wbr[:, z * 2 + c2, dmc * 128:(dmc + 1) * 128],
                                                                                       ob[:, z * 2 + c2, :], start=(c2 == 0), stop=(c2 == 1)),
                                 [rwbr, rob], [rp])
                        if z == 0:
                            S.op("dve", lambda e, ps=ps, g=g, dmc=dmc: e.tensor_tensor(out=m[:, dmc, :], in0=ps[:, 0:TB], in1=g[:, 0, :], op=ALU.mult),
                                 [rp, rg], [rm[dmc]])
                        else:
                            tt, rt = tmp[nt % 2], rtmp[nt % 2]
                            nt += 1
                            S.op("dve", lambda e, ps=ps, g=g, z=z, tt=tt: e.tensor_tensor(out=tt[:], in0=ps[:, 0:TB], in1=g[:, z, :], op=ALU.mult),
                                 [rp, rg], [rt])
                            S.op("pool", lambda e, tt=tt, dmc=dmc: e.tensor_tensor(out=m[:, dmc, :], in0=m[:, dmc, :], in1=tt[:], op=ALU.add),
                                 [rt, rm[dmc]], [rm[dmc]])
                for d2 in range(8):
                    ps, rp = self.psum()
                    for k in range(8):
                        S.op("pe", lambda e, ps=ps, k=k, d2=d2: e.matmul(ps[:, 0:TB], wo[:, k, d2 * 128:(d2 + 1) * 128], m[:, k, :],
                                                                       start=(k == 0), stop=(k == 7)), [rwo, rm[k]], [rp])
                    tt, rt = tmp[nt % 2], rtmp[nt % 2]
                    nt += 1
                    S.op("act", lambda e, ps=ps, tt=tt, d2=d2: e.activation(out=tt[:], in_=ps[:, 0:TB], func=AF.Identity, scale=self.mod(l, 2, d2, col)),
                         [rp, self.rmod], [rt])
                    S.op("dve", lambda e, tt=tt, d2=d2: e.scalar_tensor_tensor(out=self.xT[:, d2, t0:t0 + TB], in0=self.xT[:, d2, t0:t0 + TB], scalar=ALPHA,
                                                                            in1=tt[:], op0=ALU.mult, op1=ALU.add),
                         [rt] + self.rxs(d2, t0, TB), self.rxs(d2, t0, TB))
                if self.dbg.get("merge_ln", True):
                    self.layer_norm((sq, rsq, rstd, rrstd), l, t0, TB, "ln1_g", "ln1_b")

    def phase_moe(self, b, l):
        nc, S, A = self.nc, self.S, self.A
        ident = self.cst("ident")
        with ExitStack() as es:
            h2 = self.sb(es, "h2", [128, 8, T], BF16)
            rh2 = Res()
            denseT = self.sb(es, "denseT", [32, T])
            rdT = [Res() for _ in range(5)]
            for k in range(8):
                S.op("dve", lambda e, k=k: e.tensor_scalar(out=h2[:, k, 0:TL], in0=self.xT[:, k, 0:TL], scalar1=self.mod(l, 4, k, b),
                                                         scalar2=self.mod(l, 3, k, b), op0=ALU.mult, op1=ALU.add),
                     [self.rmod] + self.rx[k][0:4], [rh2])
                S.op("pool", lambda e, k=k: e.tensor_scalar(out=h2[:, k, TL:T], in0=self.xT[:, k, TL:T], scalar1=self.mod(l, 4, k, 2),
                                                          scalar2=self.mod(l, 3, k, 2), op0=ALU.mult, op1=ALU.add),
                     [self.rmod, self.rx[k][4]], [rh2])
            with ExitStack() as es2:
                wrt = self.sb(es2, "wrt", [128, 8, 36])
                rwrt = Res()
                S.dma(wrt[:], A["w_rt"][l].rearrange("(k p) c -> p k c", p=128), writes=[rwrt])
                brow = self.sb(es2, "brow", [1, 36])
                S.dma(brow[:], A["b_rt"][l:l + 1, :], writes=[rwrt])
                h2f = [self.sb(es2, "h2f%d" % i, [128, 8, 128]) for i in range(2)]
                rh2f = [Res() for _ in range(2)]
                R = [self.sb(es2, "rt%d" % i, [128, 160]) for i in range(2)]
                rR = [Res() for _ in range(2)]
                ones = self.cst("ones")
                for tt in range(T // 128):
                    t0 = tt * 128
                    col = b if t0 < TL else 2
                    hf, rhf = h2f[tt % 2], rh2f[tt % 2]
                    r_, rr = R[tt % 2], rR[tt % 2]
                    for k in range(8):
                        S.op("dve", lambda e, k=k, hf=hf: e.tensor_scalar(out=hf[:, k, :], in0=self.xT[:, k, t0:t0 + 128], scalar1=self.mod(l, 4, k, col),
                                                                        scalar2=self.mod(l, 3, k, col), op0=ALU.mult, op1=ALU.add),
                             [self.rmod] + self.rxs(k, t0, 128), [rhf])
                    ps, rp = self.psum()
                    for k in range(8):
                        S.op("pe", lambda e, k=k, hf=hf, ps=ps: e.matmul(ps[:, 0:36], hf[:, k, :], wrt[:, k, :], start=(k == 0), stop=False), [rhf, rwrt], [rp])
                    S.op("pe", lambda e, ps=ps: e.matmul(ps[:, 0:36], ones[0:1, :], brow[0:1, :], start=False, stop=True), [rwrt, self.rconst], [rp])
                    L = r_[:, 0:36]
                    sc = lambda i, r_=r_: r_[:, 140 + i:141 + i]
                    MG, NMG, SG, PG, M1, M2, DD, EE, RR, W1, W2 = range(11)
                    S.op("act", lambda e, ps=ps: e.activation(out=L, in_=ps[:, 0:36], func=AF.Copy), [rp], [rr])
                    dv = lambda fn: S.op("dve", fn, [rr], [rr])
                    dv(lambda e: e.tensor_reduce(out=sc(MG), in_=r_[:, 0:4], axis=AX.X, op=ALU.max))
                    dv(lambda e: e.tensor_scalar(out=r_[:, 60:64], in0=r_[:, 0:4], scalar1=sc(MG), scalar2=None, op0=ALU.subtract))
                    S.op("act", lambda e: e.activation(out=r_[:, 60:64], in_=r_[:, 60:64], func=AF.Exp), [rr], [rr])
                    dv(lambda e: e.tensor_reduce(out=sc(SG), in_=r_[:, 60:64], axis=AX.X, op=ALU.add))
                    dv(lambda e: e.reciprocal(out=sc(PG), in_=sc(SG)))
                    dv(lambda e: e.tensor_scalar(out=r_[:, 40:44], in0=r_[:, 0:4], scalar1=sc(MG), scalar2=None, op0=ALU.is_equal))
                    dv(lambda e: e.tensor_scalar(out=r_[:, 44:52], in0=r_[:, 4:12], scalar1=r_[:, 40:41], scalar2=None, op0=ALU.mult))
                    for g in range(1, 4):
                        dv(lambda e, g=g: e.scalar_tensor_tensor(out=r_[:, 44:52], in0=r_[:, 4 + 8 * g:12 + 8 * g], scalar=r_[:, 40 + g:41 + g],
                                                                 in1=r_[:, 44:52], op0=ALU.mult, op1=ALU.add))
                    dv(lambda e: e.tensor_reduce(out=sc(M1), in_=r_[:, 44:52], axis=AX.X, op=ALU.max))
                    dv(lambda e: e.tensor_scalar(out=r_[:, 52:60], in0=r_[:, 44:52], scalar1=sc(M1), scalar2=None, op0=ALU.is_equal))
                    dv(lambda e: e.scalar_tensor_tensor(out=r_[:, 60:68], in0=r_[:, 52:60], scalar=NEG, in1=r_[:, 44:52], op0=ALU.mult, op1=ALU.add))
                    dv(lambda e: e.tensor_reduce(out=sc(M2), in_=r_[:, 60:68], axis=AX.X, op=ALU.max))
                    dv(lambda e: e.tensor_scalar(out=r_[:, 68:76], in0=r_[:, 60:68], scalar1=sc(M2), scalar2=None, op0=ALU.is_equal))
                    dv(lambda e: e.tensor_tensor(out=sc(DD), in0=sc(M2), in1=sc(M1), op=ALU.subtract))
                    S.op("act", lambda e: e.activation(out=sc(EE), in_=sc(DD), func=AF.Exp), [rr], [rr])
                    dv(lambda e: e.tensor_scalar(out=sc(RR), in0=sc(EE), scalar1=1.0, scalar2=None, op0=ALU.add))
                    dv(lambda e: e.reciprocal(out=sc(RR), in_=sc(RR)))
                    dv(lambda e: e.tensor_tensor(out=sc(W1), in0=sc(RR), in1=sc(PG), op=ALU.mult))
                    dv(lambda e: e.tensor_tensor(out=sc(W2), in0=sc(W1), in1=sc(EE), op=ALU.mult))
                    dv(lambda e: e.tensor_scalar(out=r_[:, 76:84], in0=r_[:, 52:60], scalar1=sc(W1), scalar2=None, op0=ALU.mult))
                    dv(lambda e: e.scalar_tensor_tensor(out=r_[:, 76:84], in0=r_[:, 68:76], scalar=sc(W2), in1=r_[:, 76:84], op0=ALU.mult, op1=ALU.add))
                    for g in range(4):
                        dv(lambda e, g=g: e.tensor_scalar(out=r_[:, 84 + 8 * g:92 + 8 * g], in0=r_[:, 76:84], scalar1=r_[:, 40 + g:41 + g], scalar2=None,
                                                          op0=ALU.mult))
                    ps2, rp2 = self.psum()
                    S.op("pe", lambda e, ps2=ps2: e.transpose(ps2[0:32, 0:128], r_[:, 84:116], ident), [rr, self.rconst], [rp2])
                    S.op("act", lambda e, ps2=ps2: e.activation(out=denseT[:, t0:t0 + 128], in_=ps2[0:32, 0:128], func=AF.Copy), [rp2], [rdT[t0 // 512]])
                S.barrier()
            self.dump("denseT", denseT[:], [32, T], rdT)
            for k in range(8):
                for j in range(5):
                    t0 = j * 512
                    tn = min(512, T - t0)
                    S.op("pool", lambda e, k=k, t0=t0, tn=tn: e.tensor_scalar(out=self.xT[:, k, t0:t0 + tn], in0=self.xT[:, k, t0:t0 + tn], scalar1=ALPHA,
                                                                          scalar2=None, op0=ALU.mult), [self.rx[k][j]], [self.rx[k][j]])
            with ExitStack() as es3:
                NSTG = 2
                stg = [self.sb(es3, "stg%d" % i, [128, 2048]) for i in range(NSTG)]
                rstg = [Res() for _ in range(NSTG)]
                wgu = [self.sb(es3, "wgu%d" % i, [128, 8, 512], BF16) for i in range(3)]
                rwgu = [Res() for _ in range(3)]
                wdn = [self.sb(es3, "wdn%d" % i, [128, 4, D], BF16) for i in range(2)]
                rwdn = [Res() for _ in range(2)]
                abf = [self.sb(es3, "abf%d" % i, [128, 4, 512], BF16) for i in range(2)]
                rabf = [Res() for _ in range(2)]
                sgt = [self.sb(es3, "sgt%d" % i, [128, 512]) for i in range(2)]
                rsgt = [Res() for _ in range(2)]
                tt_ = [self.sb(es3, "ttm%d" % i, [128, 512]) for i in range(2)]
                rtt = [Res() for _ in range(2)]
                dsel = [self.sb(es3, "dsel%d" % i, [32, 512]) for i in range(2)]
                rdsel = [Res() for _ in range(2)]
                dB = [self.sb(es3, "dB%d" % i, [128, 512]) for i in range(2)]
                rdB = [Res() for _ in range(2)]
                ones = self.cst("ones")
                nstg = 0
                ngu = 0
                cnt = 0
                for e_ in range(self.dbg.get("e_start", 0), self.dbg.get("nexp", 32)):
                    ws = []
                    for wi, nm in enumerate(("w_gate", "w_up")):
                        w, rw = wgu[ngu % 3], rwgu[ngu % 3]
                        ngu += 1
                        for hh in range(2):
                            s_, rs_ = stg[nstg % NSTG], rstg[nstg % NSTG]
                            nstg += 1
                            S.dma(s_[:].rearrange("p (k h) -> p k h", k=8), A[nm][l, e_, :, hh * 256:(hh + 1) * 256].rearrange("(k p) h -> p k h", p=128),
                                  writes=[rs_])
                            S.op("act", lambda e, s_=s_, w=w, hh=hh: e.activation(out=w[:, :, hh * 256:(hh + 1) * 256],
                                                                               in_=s_[:].rearrange("p (k h) -> p k h", k=8), func=AF.Copy), [rs_], [rw])
                        ws.append((w, rw))
                    (wg, rwg), (wu, rwu) = ws
                    wd, rwd = wdn[e_ % 2], rwdn[e_ % 2]
                    for hh in range(2):
                        s_, rs_ = stg[nstg % NSTG], rstg[nstg % NSTG]
                        nstg += 1
                        S.dma(s_[:].rearrange("p (c d) -> p c d", c=2), A["w_down"][l, e_, hh * 256:(hh + 1) * 256, :].rearrange("(c p) d -> p c d", p=128),
                              writes=[rs_])
                        S.op("pool", lambda e, s_=s_, wd=wd, hh=hh: e.tensor_copy(out=wd[:, hh * 2:hh * 2 + 2, :],
                                                                               in_=s_[:].rearrange("p (c d) -> p c d", c=2)), [rs_], [rwd])
                    def stage_a(tb):
                        t0 = tb * 512
                        tn = min(512, T - t0)
                        cnt = e_ * 5 + tb
                        ds_, rds = dsel[cnt % 2], rdsel[cnt % 2]
                        db_, rdb = dB[cnt % 2], rdB[cnt % 2]
                        ab, rab = abf[cnt % 2], rabf[cnt % 2]
                        S.op("dve", lambda e: e.tensor_scalar(out=ds_[:, 0:tn], in0=denseT[:, t0:t0 + tn], scalar1=ident[0:32, e_:e_ + 1],
                                                              scalar2=None, op0=ALU.mult), [rdT[tb], self.rconst], [rds])
                        psb, rpb = self.psum()
                        S.op("pe", lambda e: e.matmul(psb[:, 0:tn], ones[0:32, :], ds_[:, 0:tn], start=True, stop=True), [rds, self.rconst], [rpb])
                        S.op("act", lambda e: e.activation(out=db_[:, 0:tn], in_=psb[:, 0:tn], func=AF.Copy), [rpb], [rdb])
                        for hc in range(4):
                            psg, rpg = self.psum()
                            for k in range(8):
                                S.op("pe", lambda e: e.matmul(psg[:, 0:tn], wg[:, k, hc * 128:(hc + 1) * 128], h2[:, k, t0:t0 + tn],
                                                              start=(k == 0), stop=(k == 7)), [rwg, rh2], [rpg])
                            psu, rpu = self.psum()
                            for k in range(8):
                                S.op("pe", lambda e: e.matmul(psu[:, 0:tn], wu[:, k, hc * 128:(hc + 1) * 128], h2[:, k, t0:t0 + tn],
                                                              start=(k == 0), stop=(k == 7)), [rwu, rh2], [rpu])
                            i2 = (cnt * 4 + hc) % 2
                            sg_, rsg = sgt[i2], rsgt[i2]
                            t_, rt_ = tt_[i2], rtt[i2]
                            S.op("act", lambda e: e.activation(out=sg_[:, 0:tn], in_=psg[:, 0:tn], func=AF.Silu), [rpg], [rsg])
                            S.op("dve", lambda e: e.tensor_tensor(out=t_[:, 0:tn], in0=psu[:, 0:tn], in1=sg_[:, 0:tn], op=ALU.mult), [rpu, rsg], [rt_])
                            S.op("pool", lambda e: e.tensor_tensor(out=ab[:, hc, 0:tn], in0=t_[:, 0:tn], in1=db_[:, 0:tn], op=ALU.mult), [rt_, rdb], [rab])

                    def stage_b(tb):
                        t0 = tb * 512
                        tn = min(512, T - t0)
                        col = b if t0 < TL else 2
                        cnt = e_ * 5 + tb
                        ab, rab = abf[cnt % 2], rabf[cnt % 2]
                        for dmc in range(8):
                            psd, rpd = self.psum()
                            for hc in range(4):
                                S.op("pe", lambda e: e.matmul(psd[:, 0:tn], wd[:, hc, dmc * 128:(dmc + 1) * 128], ab[:, hc, 0:tn],
                                                              start=(hc == 0), stop=(hc == 3)), [rwd, rab], [rpd])
                            S.op("dve", lambda e: e.scalar_tensor_tensor(out=self.xT[:, dmc, t0:t0 + tn], in0=psd[:, 0:tn], scalar=self.mod(l, 5, dmc, col),
                                                                         in1=self.xT[:, dmc, t0:t0 + tn], op0=ALU.mult, op1=ALU.add),
                                 [rpd, self.rmod, self.rx[dmc][tb]], [self.rx[dmc][tb]])

                    stage_a(0)
                    for tb in range(5):
                        if tb + 1 < 5:
                            stage_a(tb + 1)
                        stage_b(tb)
                S.barrier()
            with ExitStack() as es4:
                sq = [self.sb(es4, "lnsq%d" % i, [128, 512]) for i in range(2)]
                rsq = [Res() for _ in range(2)]
                rstd = self.sb(es4, "lnrstd", [128, 512])
                rrstd = Res()
                for tb in range(5):
                    t0 = tb * 512
                    tn = min(512, T - t0)
                    self.layer_norm((sq, rsq, rstd, rrstd), l, t0, tn, "ln2_g", "ln2_b")


def make_in_maps(inputs, ncores=8):
    f = lambda a: np.ascontiguousarray(np.asarray(a, np.float32))
    x, c, ctx, c_ctx = inputs["x"], inputs["c"], inputs["ctx"], inputs["c_ctx"]
    pvec = np.zeros((DEPTH, 128, NPV), np.float32)
    for l in range(DEPTH):
        for name in ("b_ada", "ln1_g", "ln1_b", "ln2_g", "ln2_b"):
            o, n = PV[name]
            pvec[l, :, o:o + n] = _fm(inputs[name][l])
        for name, src in (("a_qg", "a_q_gain"), ("a_kg", "a_k_gain"), ("c_og", "c_out_gain"), ("d_og", "d_out_gain")):
            pvec[l, :, PV[name][0]] = np.tile(np.asarray(inputs[src][l], np.float32), 2)
        cw = np.asarray(inputs["c_conv"][l], np.float32)
        for ch in range(6):
            for tap in range(3):
                pvec[l, :, PV["c_conv"][0] + ch * 3 + tap] = cw[tap, ch * 128:(ch + 1) * 128]
        pvec[l, 0:8, PV["c_dtb"][0]] = np.asarray(inputs["c_dt_bias"][l], np.float32).reshape(8)
        pvec[l, 0:8, PV["c_alog"][0]] = np.asarray(inputs["c_a_log"][l], np.float32).reshape(8)
        pvec[l, :, PV["d_gb"][0]:PV["d_gb"][0] + 2] = np.asarray(inputs["d_gate_b"][l], np.float32).T
    w_rt = np.concatenate([inputs["w_router_g"], np.transpose(inputs["w_router_e"], (0, 2, 1, 3)).reshape(DEPTH, D, 32)], axis=2)
    b_rt = np.concatenate([inputs["b_router_g"], inputs["b_router_e"].reshape(DEPTH, 32)], axis=1)
    shared = {"mconsts": MCONSTS, "cmask": CMASK, "d_gw": f(inputs["d_gate_w"]), "rope": ROPE, "nab": _na_bias(np.asarray(inputs["b_rpb"], np.float32)), "consts": CONSTS, "pvec": pvec, "w_ada": f(inputs["w_ada"]), "w_in": f(inputs["w_in"]), "w_branch": f(inputs["w_branch"]),
              "w_out": f(inputs["w_out"]), "w_rt": f(w_rt), "b_rt": f(b_rt), "w_up": f(inputs["w_up"]), "w_gate": f(inputs["w_gate"]),
              "w_down": f(inputs["w_down"])}
    maps = []
    for i in range(ncores):
        bs = slice(2 * i, 2 * i + 2)
        m = dict(shared)
        m["xT_in"] = f(np.transpose(x[bs], (0, 2, 1)))
        m["ctxT_in"] = f(np.transpose(ctx[bs], (0, 2, 1)))
        m["cc"] = f(np.stack([c[2 * i], c[2 * i + 1], c_ctx], axis=1))
        maps.append(m)
    return maps


def kernel(**inputs):
    nc = bass.Bass("TRN2", target_bir_lowering=False)
    Kern(nc).build()
    maps = make_in_maps(inputs)
    res = run_bass_kernel_spmd(nc, maps, core_ids=list(range(8)))
    out = np.zeros((16, TL, D), np.float32)
    for i in range(8):
        o = res.results[i]["outT"]
        out[2 * i:2 * i + 2] = np.transpose(o, (0, 2, 1))
    return out
```

```python
import numpy as np
import concourse.bass as bass
import concourse.mybir as mybir
from concourse.bass_utils import run_bass_kernel_spmd
from contextlib import ExitStack

F32 = mybir.dt.float32
BF16 = mybir.dt.bfloat16
AF = mybir.ActivationFunctionType
ALU = mybir.AluOpType
AX = mybir.AxisListType

D = 1024
TL = 2048
TC = 256
T = TL + TC
DEPTH = 4
INW = 7216
ALPHA = (2.0 * DEPTH) ** 0.25
EPS = 1e-6
NEG = -30000.0


class Res:
    __slots__ = ("w", "rs")

    def __init__(self):
        self.w = None
        self.rs = []


ENGS = ("pe", "act", "dve", "pool", "sp")


class Sched:
    def __init__(self, nc, ndma_sems=16):
        self.nc = nc
        self.eobj = {"pe": nc.tensor, "act": nc.scalar, "dve": nc.vector, "pool": nc.gpsimd, "sp": nc.sync}
        self.esem = {e: nc.alloc_semaphore("prog_" + e) for e in ENGS}
        self.ecount = {e: 0 for e in ENGS}
        self.dsems = {}
        self.dcount = {}
        self.ndma = ndma_sems
        self.seen = {e: {} for e in ENGS}
        self.nops = 0

    def _wait(self, eng, tok):
        sem, val = tok[0], tok[1]
        k = id(sem)
        if self.seen[eng].get(k, 0) >= val:
            return
        self.seen[eng][k] = val
        self.eobj[eng].wait_ge(sem, val)

    def op(self, eng, fn, reads=(), writes=(), dma=False):
        toks = []
        for r in reads:
            if r.w is not None:
                toks.append(r.w)
        for w in writes:
            if w.w is not None:
                toks.append(w.w)
            toks.extend(w.rs)
        for t in toks:
            if t[2] == eng and not t[3] and eng == "pe":
                continue
            self._wait(eng, t)
        if dma:
            if eng not in self.dsems:
                self.dsems[eng] = [self.nc.alloc_semaphore("dma_%s_%d" % (eng, i)) for i in range(self.ndma)]
                self.dcount[eng] = 0
            j = self.dcount[eng]
            self.dcount[eng] = j + 1
            sem = self.dsems[eng][j % self.ndma]
            if j >= self.ndma:
                self._wait(eng, (sem, 16 * (j // self.ndma)))
            val = 16 * (j // self.ndma + 1)
            ins = fn(self.eobj[eng])
            ins.then_inc(sem, 16)
            tok = (sem, val, eng, True)
        else:
            self.ecount[eng] += 1
            ins = fn(self.eobj[eng])
            ins.then_inc(self.esem[eng], 1)
            tok = (self.esem[eng], self.ecount[eng], eng, False)
        for r in reads:
            r.rs.append(tok)
        for w in writes:
            w.w = tok
            w.rs = []
        self.nops += 1
        return tok

    def dma(self, out, in_, reads=(), writes=(), eng="sp", **kw):
        return self.op(eng, lambda e: e.dma_start(out=out, in_=in_, **kw), reads, writes, dma=True)

    def barrier(self):
        toks = []
        for e in ENGS:
            if self.ecount[e] > 0:
                toks.append((self.esem[e], self.ecount[e], e, False))
        for q, sems in self.dsems.items():
            n = self.dcount[q]
            for i, s in enumerate(sems):
                uses = (n - i + self.ndma - 1) // self.ndma if n > i else 0
                if uses > 0:
                    toks.append((s, 16 * uses, q, True))
        for e in ENGS:
            for t in toks:
                if t[2] == e and not t[3]:
                    continue
                self._wait(e, t)

    def finish(self, toks):
        for t in toks:
            self._wait("sp", t)


CONST_COLS = {}


def _build_consts():
    cols = []

    mcols = []

    def add(name, arr):
        arr = np.asarray(arr, np.float32)
        assert arr.shape[0] == 128
        if name in ("gla_mask", "sel8", "selp", "gdn_ms", "gdn_nms", "gdn_qi", "ident4", "hmask32", "sblk", "hm64", "wmask"):
            CONST_COLS[name] = (1, sum(a.shape[1] for a in mcols), arr.shape[1])
            mcols.append(arr)
        else:
            CONST_COLS[name] = (0, sum(a.shape[1] for a in cols), arr.shape[1])
            cols.append(arr)

    add("ident", np.eye(128))
    add("onesln", np.full((128, 128), 1.0 / 1024))
    bd = np.zeros((128, 128))
    bd[:64, :64] = 1.0 / 64
    bd[64:, 64:] = 1.0 / 64
    add("bd64", bd)
    bdo = np.zeros((128, 128))
    bdo[:64, :64] = 1.0
    bdo[64:, 64:] = 1.0
    add("bd64one", bdo)
    add("ones", np.ones((128, 128)))
    cv = np.zeros((128, 8))
    cv[:, 0] = EPS
    cv[:, 1] = 1.0
    cv[0:4, 2] = 1.0
    cv[4:8, 3] = 1.0
    add("cvec", cv)
    gm = np.zeros((128, 512))
    tri = np.tril(np.ones((64, 64)))
    for h in range(4):
        gm[0:64, h * 64:(h + 1) * 64] = tri.T
        gm[0:64, 256 + h * 64:256 + (h + 1) * 64] = tri
    add("gla_mask", gm)
    sel8 = np.zeros((128, 512))
    for dh in range(8):
        sel8[dh, dh * 64:(dh + 1) * 64] = 1.0
    add("sel8", sel8)
    selp = np.zeros((128, 512))
    for d in range(2):
        for hc in range(2):
            o = (d * 2 + hc) * 128
            selp[d * 4 + 2 * hc, o:o + 64] = 1.0
            selp[d * 4 + 2 * hc + 1, o + 64:o + 128] = 1.0
    add("selp", selp)
    lo = np.tril(np.ones((64, 64)), -1)
    up = np.triu(np.ones((64, 64)), 1)
    ms = np.zeros((128, 512)); nms = np.zeros((128, 512)); qi = np.zeros((128, 512)); id4 = np.zeros((128, 256))
    for h in range(4):
        ms[0:64, h * 64:(h + 1) * 64] = lo
        ms[0:64, 256 + h * 64:256 + (h + 1) * 64] = up
        nms[0:64, h * 64:(h + 1) * 64] = -up
        nms[0:64, 256 + h * 64:256 + (h + 1) * 64] = -lo
        qi[0:64, h * 64:(h + 1) * 64] = up + np.eye(64)
        qi[0:64, 256 + h * 64:256 + (h + 1) * 64] = lo + np.eye(64)
        id4[0:64, h * 64:(h + 1) * 64] = np.eye(64)
    add("gdn_ms", ms)
    add("gdn_nms", nms)
    add("gdn_qi", qi)
    add("ident4", id4)
    hm32 = np.zeros((128, 4)); sblk = np.zeros((128, 256)); hm64 = np.zeros((128, 2)); wmask = np.zeros((128, 256))
    for h in range(4):
        hm32[h * 32:(h + 1) * 32, h] = 1.0
        sblk[h * 32:(h + 1) * 32, h * 64:(h + 1) * 64] = 1.0
        wmask[(h % 2) * 64:(h % 2) * 64 + 64, h * 64:(h + 1) * 64] = 1.0
    hm64[0:64, 0] = 1.0
    hm64[64:128, 1] = 1.0
    add("hmask32", hm32)
    add("sblk", sblk)
    add("hm64", hm64)
    add("wmask", wmask)
    return np.concatenate(cols, axis=1), np.concatenate(mcols, axis=1)


CONSTS, MCONSTS = _build_consts()
NMCONST = MCONSTS.shape[1]
CMASK = np.ones((128, T), np.float32)
CMASK[:, ::64] = 0.0
NCONST = CONSTS.shape[1]


def _rope_tables():
    t = np.arange(TL)
    row = (t // 64).astype(np.float32)
    col = (t % 64).astype(np.float32)
    inv = (np.float32(10000.0) ** (-np.arange(16, dtype=np.float32) / np.float32(16))).astype(np.float32)
    ang_r = row[:, None] * inv
    ang_c = col[:, None] * inv
    C = np.zeros((64, TL), np.float32)
    Sg = np.zeros((64, TL), np.float32)
    P = np.zeros((64, 64), np.float32)
    for m in range(64):
        d = m % 64
        ang = ang_r if d < 32 else ang_c
        f = d % 16
        first = (d % 32) < 16
        C[m] = np.cos(ang[:, f])
        Sg[m] = (-1.0 if first else 1.0) * np.sin(ang[:, f])
        src = m + 16 if first else m - 16
        P[src, m] = 1.0
    return np.concatenate([C, Sg, P], axis=1)


ROPE = _rope_tables()


def _rs(r):
    return min(max(r - 4, 0), 24)


def _na_patterns():
    pats = {}
    table = {}
    for t in range(16):
        r0, r1 = 2 * t, 2 * t + 1
        for j in range(_rs(r0) // 2, (_rs(r1) + 7) // 2 + 1):
            key = tuple((2 * j + a - (2 * t + bq), _rs(2 * t + bq) <= 2 * j + a < _rs(2 * t + bq) + 8) for a in (0, 1) for bq in (0, 1))
            if not any(v for _, v in key):
                continue
            table[(t, j)] = pats.setdefault(key, len(pats))
    return pats, table


NA_PATS, NA_TABLE = _na_patterns()
NPAT = len(NA_PATS)


def _na_bias(rpb):
    L = rpb.shape[0]
    out = np.full((L, NPAT, 128, 4, 128), NEG, np.float32)
    qc = np.arange(64)
    kc = np.arange(64)
    cs = np.clip(qc - 8, 0, 48)
    col_ok = (kc[None, :] >= cs[:, None]) & (kc[None, :] < cs[:, None] + 16)
    dc = np.clip(kc[None, :] - qc[:, None] + 15, 0, 30)
    for key, idx in NA_PATS.items():
        n = 0
        for a in (0, 1):
            for bq in (0, 1):
                dr, valid = key[n]
                n += 1
                if not valid:
                    continue
                blk = rpb[:, :, dr + 7, :][:, :, dc]
                blk = np.where(col_ok[None, None], blk, np.float32(NEG))
                out[:, idx, a * 64:(a + 1) * 64, :, bq * 64:(bq + 1) * 64] = np.transpose(blk, (0, 3, 1, 2))
    return out


PV = {}


def _pv_layout():
    off = 0
    for name, n in [("b_ada", 48), ("ln1_g", 8), ("ln1_b", 8), ("ln2_g", 8), ("ln2_b", 8), ("a_qg", 1), ("a_kg", 1), ("c_og", 1), ("d_og", 1), ("d_gb", 2), ("c_conv", 18), ("c_dtb", 1), ("c_alog", 1)]:
        PV[name] = (off, n)
        off += n
    return off


NPV = _pv_layout()


def _fm(v):
    return np.ascontiguousarray(np.asarray(v, np.float32).reshape(-1, 128).T)


class Kern:
    def __init__(self, nc, dbg=None):
        self.nc = nc
        self.S = Sched(nc)
        self.dbg = dbg or {}
        self.es = ExitStack()
        self.nps = 0

    def sb(self, es, name, shape, dt=F32):
        self.nsb = getattr(self, "nsb", 0) + 1
        return es.enter_context(self.nc.sbuf_tensor("%s_%d" % (name, self.nsb), shape, dt))

    def psum_chain(self, d):
        self.npc = getattr(self, "npc", [0, 0])
        i = d * 4 + self.npc[d] % 4
        self.npc[d] += 1
        return self.ps[i], self.rps[i]

    def psum(self):
        i = self.nps % 8
        self.nps += 1
        return self.ps[i], self.rps[i]

    def dump(self, name, ap, shape, reads):
        if not self.dbg.get("dump"):
            return
        d = self.nc.dram_tensor("dump_" + name, list(shape), F32, kind="ExternalOutput").ap()
        self.S.barrier()
        self.S.dma(d, ap, reads=reads)
        self.S.barrier()

    def cst(self, name, rows=128):
        w, o, n = CONST_COLS[name]
        return (self.mconsts if w else self.consts)[0:rows, o:o + n]

    def load_mconsts(self, es):
        self.mconsts = self.sb(es, "mconsts", [128, NMCONST])
        self.S.dma(self.mconsts[:], self.A["mconsts"][:, :], writes=[self.rconst])

    def evac(self, out, in_, reads, writes, i):
        if i % 2 == 0:
            self.S.op("act", lambda e: e.activation(out=out, in_=in_, func=AF.Copy), reads, writes)
        else:
            self.S.op("dve", lambda e: e.tensor_copy(out=out, in_=in_), reads, writes)

    def build(self):
        nc, S = self.nc, self.S
        dt = nc.dram_tensor
        A = {}
        A["xT"] = dt("xT_in", [2, D, TL], F32, kind="ExternalInput").ap()
        A["ctxT"] = dt("ctxT_in", [2, D, TC], F32, kind="ExternalInput").ap()
        A["cc"] = dt("cc", [D, 3], F32, kind="ExternalInput").ap()
        A["consts"] = dt("consts", [128, NCONST], F32, kind="ExternalInput").ap()
        A["rope"] = dt("rope", [64, 2 * TL + 64], F32, kind="ExternalInput").ap()
        A["nab"] = dt("nab", [DEPTH, NPAT, 128, 4, 128], F32, kind="ExternalInput").ap()
        A["mconsts"] = dt("mconsts", [128, NMCONST], F32, kind="ExternalInput").ap()
        A["pvec"] = dt("pvec", [DEPTH, 128, NPV], F32, kind="ExternalInput").ap()
        A["w_ada"] = dt("w_ada", [DEPTH, D, 6 * D], F32, kind="ExternalInput").ap()
        A["w_in"] = dt("w_in", [DEPTH if self.dbg.get("do_proj", True) else 1, D, INW], F32, kind="ExternalInput").ap()
        A["w_branch"] = dt("w_branch", [DEPTH, 4, 256, D], F32, kind="ExternalInput").ap()
        A["w_out"] = dt("w_out", [DEPTH, D, D], F32, kind="ExternalInput").ap()
        A["w_rt"] = dt("w_rt", [DEPTH, D, 36], F32, kind="ExternalInput").ap()
        A["b_rt"] = dt("b_rt", [DEPTH, 36], F32, kind="ExternalInput").ap()
        nlw = DEPTH if self.dbg.get("do_moe", True) else 1
        nex = self.dbg.get("nexp", 32) if self.dbg.get("do_moe", True) else 1
        A["w_up"] = dt("w_up", [nlw, nex, D, 512], F32, kind="ExternalInput").ap()
        A["w_gate"] = dt("w_gate", [nlw, nex, D, 512], F32, kind="ExternalInput").ap()
        A["w_down"] = dt("w_down", [nlw, nex, 512, D], F32, kind="ExternalInput").ap()
        A["out"] = dt("outT", [2, D, TL], F32, kind="ExternalOutput").ap()
        A["projT"] = dt("projT", [INW, T], F32, kind=self.dbg.get("proj_kind", "Internal")).ap()
        A["oT"] = dt("oT", [D, T], F32, kind=self.dbg.get("oT_kind", "Internal")).ap()
        self.A = A
        A["xsave"] = dt("xsave", [D, T], F32, kind="Internal").ap()
        self.rxsave = Res()
        A["cmask"] = dt("cmask", [128, T], F32, kind="ExternalInput").ap()
        A["d_gw"] = dt("d_gw", [DEPTH, 2, 16, 128], F32, kind="ExternalInput").ap()
        self.rprojT = Res()
        self.roT = Res()

        es = self.es
        self.ps = [nc.alloc_psum_tensor("ps%d" % i, [128, 512], F32) for i in range(8)]
        self.rps = [Res() for _ in range(8)]
        self.consts = self.sb(es, "consts", [128, NCONST])
        self.rconst = Res()
        S.dma(self.consts[:], A["consts"][:, :], writes=[self.rconst])
        self.pvec = self.sb(es, "pvec", [128, DEPTH, NPV])
        for l in range(DEPTH):
            S.dma(self.pvec[:, l, :], A["pvec"][l], writes=[self.rconst])
        self.modT = self.sb(es, "modT", [128, DEPTH, 48, 3])
        self.rmod = Res()
        self.x_es = ExitStack()
        self.xT = self.sb(self.x_es, "xT", [128, 8, T])
        self.rx = [[Res() for _ in range(5)] for _ in range(8)]

        self.phase_mods()
        S.barrier()
        outs = []
        nitems = self.dbg.get("nitems", 2)
        nlayers = self.dbg.get("nlayers", DEPTH)
        for b in range(nitems):
            self.load_x(b)
            for l in range(nlayers):
                if self.dbg.get("do_proj", True):
                    self.phase_proj(b, l)
                    S.barrier()
                if self.dbg.get("do_mix", True):
                    if not self.dbg.get("nospill"):
                        self.spill_x()
                    self.phase_mixers(b, l)
                    S.barrier()
                    if not self.dbg.get("nospill"):
                        self.restore_x()
                if self.dbg.get("do_merge", True):
                    self.phase_merge(b, l)
                    S.barrier()
                if self.dbg.get("do_moe", True):
                    self.phase_moe(b, l)
                    S.barrier()
            for k in range(8):
                outs.append(S.dma(A["out"][b, k * 128:(k + 1) * 128, :], self.xT[:, k, 0:TL],
                                  reads=[self.rx[k][j] for j in range(4)], eng="sp"))
            S.barrier()
        S.finish(outs)
        self.x_es.close()
        self.es.close()

    def rxs(self, k, t0, n):
        return [self.rx[k][j] for j in range(t0 // 512, (t0 + n - 1) // 512 + 1)]

    def spill_x(self):
        for k in range(8):
            self.S.dma(self.A["xsave"][k * 128:(k + 1) * 128, :], self.xT[:, k, :], reads=self.rx[k], writes=[self.rxsave])
        self.S.barrier()
        self.x_es.close()

    def restore_x(self):
        self.x_es = ExitStack()
        self.xT = self.sb(self.x_es, "xT", [128, 8, T])
        for k in range(8):
            self.S.dma(self.xT[:, k, :], self.A["xsave"][k * 128:(k + 1) * 128, :], reads=[self.rxsave], writes=self.rx[k])

    def load_x(self, b):
        S, A = self.S, self.A
        for k in range(8):
            S.dma(self.xT[:, k, 0:TL], A["xT"][b, k * 128:(k + 1) * 128, :], writes=self.rx[k][0:4])
            S.dma(self.xT[:, k, TL:T], A["ctxT"][b, k * 128:(k + 1) * 128, :], writes=[self.rx[k][4]])

    def phase_mods(self):
        nc, S, A = self.nc, self.S, self.A
        with ExitStack() as es:
            scT = self.sb(es, "scT", [128, 8, 3])
            rsc = Res()
            S.dma(scT[:], A["cc"].rearrange("(k p) j -> p k j", p=128), writes=[rsc])
            S.op("act", lambda e: e.activation(out=scT[:], in_=scT[:], func=AF.Silu), [rsc], [rsc])
            wt = [self.sb(es, "wada%d" % i, [128, 8, 128]) for i in range(3)]
            rw = [Res() for _ in range(3)]
            n = 0
            for l in range(DEPTH):
                bo = PV["b_ada"][0]
                for c in range(48):
                    w, r = wt[n % 3], rw[n % 3]
                    n += 1
                    S.dma(w[:], A["w_ada"][l, :, c * 128:(c + 1) * 128].rearrange("(k p) c -> p k c", p=128), writes=[r])
                    ps, rp = self.psum()
                    for k in range(8):
                        S.op("pe", lambda e, w=w, ps=ps, k=k: e.matmul(ps[:, 0:3], w[:, k, :], scT[:, k, :], start=(k == 0), stop=(k == 7)),
                             [r, rsc], [rp])
                    S.op("dve", lambda e, ps=ps, l=l, c=c: e.tensor_scalar(out=self.modT[:, l, c, :], in0=ps[:, 0:3],
                                                                         scalar1=self.pvec[:, l, bo + c:bo + c + 1], scalar2=None, op0=ALU.add),
                         [rp, self.rconst], [self.rmod])
                for c0 in (8, 32):
                    S.op("dve", lambda e, l=l, c0=c0: e.tensor_scalar(out=self.modT[:, l, c0:c0 + 8, :], in0=self.modT[:, l, c0:c0 + 8, :],
                                                                    scalar1=1.0, scalar2=None, op0=ALU.add), [self.rmod], [self.rmod])

    def mod(self, l, grp, k, col):
        return self.modT[:, l, grp * 8 + k, col:col + 1]

    def phase_proj(self, b, l):
        nc, S, A = self.nc, self.S, self.A
        groups = [(0, 256), (256, 128), (384, 128), (512, 256), (768, 256), (1024, 256), (1280, 768), (2048, 16), (2064, 256),
                  (2320, 128), (2448, 128), (2576, 256), (2832, 32), (2864, 256), (3120, 4096)]
        chunks = []
        for (o, n) in groups:
            for c in range(0, n, 128):
                chunks.append((o + c, min(128, n - c)))
        with ExitStack() as es:
            hT = self.sb(es, "hT", [128, 8, T], BF16)
            rh = Res()
            for k in range(8):
                S.op("dve", lambda e, k=k: e.tensor_scalar(out=hT[:, k, 0:TL], in0=self.xT[:, k, 0:TL], scalar1=self.mod(l, 1, k, b),
                                                         scalar2=self.mod(l, 0, k, b), op0=ALU.mult, op1=ALU.add),
                     [self.rmod] + self.rx[k][0:4], [rh])
                S.op("pool", lambda e, k=k: e.tensor_scalar(out=hT[:, k, TL:T], in0=self.xT[:, k, TL:T], scalar1=self.mod(l, 1, k, 2),
                                                          scalar2=self.mod(l, 0, k, 2), op0=ALU.mult, op1=ALU.add),
                     [self.rmod, self.rx[k][4]], [rh])
            NW = 3
            wst = [self.sb(es, "wins%d" % i, [128, 8, 128]) for i in range(NW)]
            rwst = [Res() for _ in range(NW)]
            wt = [self.sb(es, "win%d" % i, [128, 8, 128], BF16) for i in range(NW)]
            rw = [Res() for _ in range(NW)]
            NST = 4
            st = [self.sb(es, "pst%d" % i, [128, 512]) for i in range(NST)]
            rst = [Res() for _ in range(NST)]
            ns = 0
            for ci, (c0, cn) in enumerate(chunks):
                w, r = wt[ci % NW], rw[ci % NW]
                ws_, rws = wst[ci % NW], rwst[ci % NW]
                S.dma(ws_[:, :, 0:cn], A["w_in"][l, :, c0:c0 + cn].rearrange("(k p) c -> p k c", p=128), writes=[rws])
                S.op("pool", lambda e: e.tensor_copy(out=w[:, :, 0:cn], in_=ws_[:, :, 0:cn]), [rws], [r])
                for tb in range(5):
                    t0 = tb * 512
                    tn = min(512, T - t0)
                    ps, rp = self.psum()
                    for k in range(8):
                        S.op("pe", lambda e, w=w, ps=ps, k=k, cn=cn, t0=t0, tn=tn: e.matmul(ps[0:cn, 0:tn], w[:, k, 0:cn], hT[:, k, t0:t0 + tn],
                                                                                       start=(k == 0), stop=(k == 7)), [r, rh], [rp])
                    s_, rs_ = st[ns % NST], rst[ns % NST]
                    self.evac(s_[0:cn, 0:tn], ps[0:cn, 0:tn], [rp], [rs_], ns)
                    ns += 1
                    S.dma(A["projT"][c0:c0 + cn, t0:t0 + tn], s_[0:cn, 0:tn], reads=[rs_], writes=[self.rprojT], eng="pool")

    def phase_mixers(self, b, l):
        which = self.dbg.get("mixers", "abcd")
        if "a" in which:
            self.mixer_a(b, l)
            self.S.barrier()
        if "b" in which:
            self.mixer_b(b, l)
            self.S.barrier()
        if "d" in which:
            self.mixer_d(b, l)
            self.S.barrier()
        if "c" in which:
            self.mixer_c(b, l)
            self.S.barrier()

    def load_rows(self, es, name, r0, nch):
        t = self.sb(es, name, [128, nch, T])
        r = Res()
        self.S.dma(t[:], self.A["projT"][r0:r0 + 128 * nch, :].rearrange("(c p) t -> p c t", p=128), reads=[self.rprojT], writes=[r])
        return t, r

    def load_heads(self, es, name, r0, nh):
        t = self.sb(es, name, [64, nh, T])
        r = Res()
        self.S.dma(t[:], self.A["projT"][r0:r0 + 64 * nh, :].rearrange("(h p) t -> p h t", p=64), reads=[self.rprojT], writes=[r])
        return t, r

    def headnorm(self, X, rX, c, tmps, mat, gain, P=128):
        S = self.S
        (s1, r1), (s2, r2) = tmps
        for t0 in range(0, T, 512):
            tn = min(512, T - t0)
            xs = X[0:P, c, t0:t0 + tn]
            S.op("pool", lambda e: e.tensor_tensor(out=s1[0:P, 0:tn], in0=xs, in1=xs, op=ALU.mult), [rX], [r1])
            ps, rp = self.psum()
            S.op("pe", lambda e: e.matmul(ps[0:P, 0:tn], mat[0:P, 0:P], s1[0:P, 0:tn], start=True, stop=True), [r1, self.rconst], [rp])
            S.op("dve", lambda e: e.tensor_scalar(out=s2[0:P, 0:tn], in0=ps[0:P, 0:tn], scalar1=EPS, scalar2=None, op0=ALU.add), [rp], [r2])
            S.op("act", lambda e: e.activation(out=s2[0:P, 0:tn], in_=s2[0:P, 0:tn], func=AF.Sqrt), [r2], [r2])
            S.op("dve", lambda e: e.reciprocal(out=s2[0:P, 0:tn], in_=s2[0:P, 0:tn]), [r2], [r2])
            S.op("dve", lambda e: e.scalar_tensor_tensor(out=xs, in0=xs, scalar=gain, in1=s2[0:P, 0:tn], op0=ALU.mult, op1=ALU.mult),
                 [r2, self.rconst, rX], [rX])

    def rope(self, X, rX, c, tmps, ropet, rrope):
        S = self.S
        (s1, r1), (s2, r2) = tmps
        for t0 in range(0, TL, 512):
            xs = X[0:64, c, t0:t0 + 512]
            ps, rp = self.psum()
            S.op("pe", lambda e: e.matmul(ps[0:64, 0:512], ropet[0:64, 2 * TL:2 * TL + 64], xs, start=True, stop=True), [rX, rrope], [rp])
            S.op("pool", lambda e: e.tensor_tensor(out=s1[0:64, 0:512], in0=xs, in1=ropet[0:64, t0:t0 + 512], op=ALU.mult), [rX, rrope], [r1])
            S.op("dve", lambda e: e.tensor_tensor(out=s2[0:64, 0:512], in0=ps[0:64, 0:512], in1=ropet[0:64, TL + t0:TL + t0 + 512], op=ALU.mult), [rp, rrope], [r2])
            S.op("pool", lambda e: e.tensor_tensor(out=xs, in0=s1[0:64, 0:512], in1=s2[0:64, 0:512], op=ALU.add), [r1, r2, rX], [rX])

    def build_vaug(self, Vaug, vT, rv, H):
        S = self.S
        rV = Res()
        S.op("pool", lambda e: e.memset(Vaug[:, :, :, 64:128], 1.0), [], [rV])
        n = 0
        for h in range(H):
            for j in range(18):
                ps, rp = self.psum()
                S.op("pe", lambda e: e.transpose(ps[:, 0:64], vT[0:64, h, j * 128:(j + 1) * 128], self.cst("ident")[0:64, 0:64]), [rv, self.rconst], [rp])
                self.evac(Vaug[:, j, h, 0:64], ps[:, 0:64], [rp], [rV], n)
                n += 1
        return Vaug, rV

    def attention(self, es, l, qT, rq, kfun, rk, Vaug, rV, hv, keylist, zrow):
        S, A = self.S, self.A
        H = 4
        ident = self.cst("ident")
        OUT = self.sb(es, "attn_out", [64, 4, T])
        rOUT = Res()
        PT = [self.sb(es, "PT%d" % i, [128, 512]) for i in range(3)]
        rPT = [Res() for _ in range(3)]
        BT = [self.sb(es, "BT%d" % i, [128, 512]) for i in range(3)]
        rBT = [Res() for _ in range(3)]
        Rr = [self.sb(es, "Rr%d" % i, [64, 128]) for i in range(2)]
        rRr = [Res() for _ in range(2)]
        seq = []
        for t in range(18):
            keys = keylist(t)
            for idx, (j, pat) in enumerate(keys):
                seq.append((t, idx, j, pat, len(keys)))
        nr = [0]

        def scores(n):
            t, idx, j, pat, nk = seq[n]
            sp, rsp = self.ps[4 + n % 4], self.rps[4 + n % 4]
            pt, rpt = PT[n % 3], rPT[n % 3]
            if pat is not None:
                bt, rbt = BT[n % 3], rBT[n % 3]
                S.dma(bt[:], A["nab"][l, pat].rearrange("k h q -> k (h q)"), writes=[rbt])
                S.op("pe", lambda e: e.matmul(sp[:, 0:512], ident, bt[:], start=True, stop=False), [rbt, self.rconst], [rsp])
            for h in range(H):
                S.op("pe", lambda e: e.matmul(sp[:, h * 128:(h + 1) * 128], kfun(h, j), qT[0:64, h, t * 128:(t + 1) * 128],
                                              start=(pat is None), stop=True), [rk, rq], [rsp])
            S.op("act", lambda e: e.activation(out=pt[:], in_=sp[:, 0:512], func=AF.Exp), [rsp], [rpt])

        def pv(n):
            t, idx, j, pat, nk = seq[n]
            pt, rpt = PT[n % 3], rPT[n % 3]
            for h in range(H):
                S.op("pe", lambda e: e.matmul(self.ps[h][:, 0:128], Vaug[:, j, hv(h), :], pt[:, h * 128:(h + 1) * 128],
                                              start=(idx == 0), stop=(idx == nk - 1)), [rV, rpt], [self.rps[h]])
            if idx == nk - 1:
                for h in range(H):
                    rr_, rrr = Rr[nr[0] % 2], rRr[nr[0] % 2]
                    nr[0] += 1
                    S.op("dve", lambda e: e.reciprocal(out=rr_[0:64, :], in_=self.ps[h][64:128, 0:128]), [self.rps[h]], [rrr])
                    S.op("dve", lambda e: e.tensor_tensor(out=OUT[0:64, h, t * 128:(t + 1) * 128], in0=self.ps[h][0:64, 0:128], in1=rr_[0:64, :], op=ALU.mult),
                         [self.rps[h], rrr], [rOUT])

        scores(0)
        for n in range(len(seq)):
            if n + 1 < len(seq):
                scores(n + 1)
            pv(n)
        S.dma(A["oT"][zrow:zrow + 256, :].rearrange("(h p) t -> p h t", p=64), OUT[:], reads=[rOUT], writes=[self.roT], eng="pool")

    def mixer_a(self, b, l):
        S, A = self.S, self.A
        with ExitStack() as es:
            qT, rq = self.load_heads(es, "aq", 0, 4)
            kT, rk = self.load_heads(es, "ak", 256, 2)
            ropet = self.sb(es, "ropet", [64, 2 * TL + 64])
            rrope = Res()
            S.dma(ropet[:], A["rope"][:, :], writes=[rrope])
            Vaug = self.sb(es, "avaug", [128, 18, 2, 128])
            with ExitStack() as es2:
                vT, rv = self.load_heads(es2, "av", 384, 2)
                Vaug, rV = self.build_vaug(Vaug, vT, rv, 2)
                tmps = [(self.sb(es2, "nt%d" % i, [64, 512]), Res()) for i in range(2)]
                bd64 = self.cst("bd64")
                for h in range(4):
                    self.headnorm(qT, rq, h, tmps, bd64, self.pvec[0:64, l, PV["a_qg"][0]:PV["a_qg"][0] + 1], P=64)
                    self.rope(qT, rq, h, tmps, ropet, rrope)
                    S.op("pool", lambda e: e.tensor_scalar(out=qT[:, h, :], in0=qT[:, h, :], scalar1=0.125, scalar2=None, op0=ALU.mult), [rq], [rq])
                for h in range(2):
                    self.headnorm(kT, rk, h, tmps, bd64, self.pvec[0:64, l, PV["a_kg"][0]:PV["a_kg"][0] + 1], P=64)
                    self.rope(kT, rk, h, tmps, ropet, rrope)
                self.S.barrier()
            kfun = lambda h, j: kT[0:64, h // 2, j * 128:(j + 1) * 128]
            keylist = lambda t: [(j, None) for j in (range(18) if t < 16 else (16, 17))]
            self.attention(es, l, qT, rq, kfun, rk, Vaug, rV, lambda h: h // 2, keylist, 0)

    def mixer_b(self, b, l):
        S, A = self.S, self.A
        with ExitStack() as es:
            qT, rq = self.load_heads(es, "bq", 512, 4)
            kT, rk = self.load_heads(es, "bk", 768, 4)
            for h in range(4):
                S.op("pool", lambda e: e.tensor_scalar(out=qT[:, h, :], in0=qT[:, h, :], scalar1=0.125, scalar2=None, op0=ALU.mult), [rq], [rq])
            Vaug = self.sb(es, "bvaug", [128, 18, 4, 128])
            with ExitStack() as es2:
                vT, rv = self.load_heads(es2, "bv", 1024, 4)
                Vaug, rV = self.build_vaug(Vaug, vT, rv, 4)
                self.S.barrier()
            kfun = lambda h, j: kT[0:64, h, j * 128:(j + 1) * 128]

            def keylist(t):
                if t >= 16:
                    return [(16, None), (17, None)]
                ks = [(j, NA_TABLE[(t, j)]) for j in range(16) if (t, j) in NA_TABLE]
                return ks + [(16, None), (17, None)]

            self.attention(es, l, qT, rq, kfun, rk, Vaug, rV, lambda h: h, keylist, 256)

    def mixer_c(self, b, l):
        S, A = self.S, self.A
        ident = self.cst("ident")
        cvec = self.cst("cvec")
        with ExitStack() as eso:
            OT = self.sb(eso, "cOT", [128, 2, T])
            rOT = Res()
            es = ExitStack()
            self.load_mconsts(es)
            sel8 = self.cst("sel8")
            selp = self.cst("selp")
            QKV, rqkv = self.load_rows(es, "cqkv", 1280, 6)
            with ExitStack() as es2:
                Y = self.sb(es2, "cY", [128, T])
                rY = Res()
                tmps = [(self.sb(es2, "cnt%d" % i, [128, 512]), Res()) for i in range(2)]
                co = PV["c_conv"][0]
                for ch in range(6):
                    w = lambda tap: self.pvec[:, l, co + ch * 3 + tap:co + ch * 3 + tap + 1]
                    X = QKV[:, ch, :]
                    S.op("dve", lambda e: e.tensor_scalar(out=Y[:], in0=X, scalar1=w(1), scalar2=None, op0=ALU.mult), [rqkv, self.rconst], [rY])
                    for (a, n) in ((0, TL), (TL, TC)):
                        S.op("dve", lambda e: e.scalar_tensor_tensor(out=Y[:, a + 1:a + n], in0=QKV[:, ch, a:a + n - 1], scalar=w(0), in1=Y[:, a + 1:a + n],
                                                                     op0=ALU.mult, op1=ALU.add), [rqkv, rY, self.rconst], [rY])
                        S.op("dve", lambda e: e.scalar_tensor_tensor(out=Y[:, a:a + n - 1], in0=QKV[:, ch, a + 1:a + n], scalar=w(2), in1=Y[:, a:a + n - 1],
                                                                     op0=ALU.mult, op1=ALU.add), [rqkv, rY, self.rconst], [rY])
                    S.op("act", lambda e: e.activation(out=QKV[:, ch, :], in_=Y[:], func=AF.Silu), [rY, rqkv], [rqkv])
                for ch in range(4):
                    self.headnorm(QKV, rqkv, ch, tmps, self.cst("bd64one"), 0.125 if ch < 2 else 1.0)
                S.barrier()
            B8 = self.sb(es, "cB8", [8, T])
            G8 = self.sb(es, "cG8", [8, T])
            TOT8 = self.sb(es, "cTOT8", [8, 36])
            ETOT8 = self.sb(es, "cETOT8", [8, 36])
            nal = self.sb(es, "cnal", [8, 1])
            TOK = self.sb(es, "cTOK", [64, 36, 48])
            ETOTP = self.sb(es, "cETOTP", [128, 4, 36])
            es3 = ExitStack()
            cm = self.sb(es3, "ccm", [8, T])
            rcm = Res()
            S.dma(cm[:], A["cmask"][0:8, :], writes=[rcm])
            A8 = self.sb(es3, "cA8", [8, T])
            X8 = self.sb(es3, "cX8", [8, T])
            STK = self.sb(es3, "cSTK", [48, T])
            r8 = Res()
            S.dma(B8[:], A["projT"][2048:2056, :], reads=[self.rprojT], writes=[r8])
            S.dma(A8[:], A["projT"][2056:2064, :], reads=[self.rprojT], writes=[r8])
            dtb = self.pvec[0:8, l, PV["c_dtb"][0]:PV["c_dtb"][0] + 1]
            alog = self.pvec[0:8, l, PV["c_alog"][0]:PV["c_alog"][0] + 1]
            o8 = lambda eng, fn: S.op(eng, fn, [r8, self.rconst, rcm], [r8])
            o8("act", lambda e: e.activation(out=B8[:], in_=B8[:], func=AF.Sigmoid))
            o8("act", lambda e: e.activation(out=nal[:], in_=alog, func=AF.Exp))
            o8("dve", lambda e: e.tensor_scalar(out=nal[:], in0=nal[:], scalar1=-1.0, scalar2=None, op0=ALU.mult))
            o8("dve", lambda e: e.tensor_scalar(out=A8[:], in0=A8[:], scalar1=dtb, scalar2=None, op0=ALU.add))
            o8("act", lambda e: e.activation(out=A8[:], in_=A8[:], func=AF.Exp))
            o8("dve", lambda e: e.tensor_scalar(out=A8[:], in0=A8[:], scalar1=1.0, scalar2=None, op0=ALU.add))
            o8("act", lambda e: e.activation(out=A8[:], in_=A8[:], func=AF.Ln))
            o8("dve", lambda e: e.tensor_scalar(out=A8[:], in0=A8[:], scalar1=nal[:, 0:1], scalar2=None, op0=ALU.mult))
            o8("dve", lambda e: e.tensor_tensor_scan(out=G8[:], data0=cm[:], data1=A8[:], initial=0.0, op0=ALU.mult, op1=ALU.add))
            o8("dve", lambda e: e.tensor_reduce(out=TOT8[:], in_=A8[:].rearrange("p (c i) -> p c i", i=64), axis=AX.X, op=ALU.add))
            o8("dve", lambda e: e.tensor_tensor(out=X8[:], in0=A8[:], in1=G8[:], op=ALU.subtract))
            for c in range(36):
                o8("dve", lambda e: e.tensor_scalar(out=X8[:, c * 64:(c + 1) * 64], in0=X8[:, c * 64:(c + 1) * 64], scalar1=TOT8[:, c:c + 1], scalar2=None, op0=ALU.add))
            o8("dve", lambda e: e.tensor_scalar(out=G8[:], in0=G8[:], scalar1=cvec[0:8, 2:3], scalar2=None, op0=ALU.mult))
            o8("dve", lambda e: e.scalar_tensor_tensor(out=G8[:], in0=X8[:], scalar=cvec[0:8, 3:4], in1=G8[:], op0=ALU.mult, op1=ALU.add))
            o8("act", lambda e: e.activation(out=ETOT8[:], in_=TOT8[:], func=AF.Exp))
            o8("act", lambda e: e.activation(out=A8[:], in_=G8[:], func=AF.Exp))
            S.dma(STK[0:8, :], G8[:], reads=[r8], writes=[r8])
            S.dma(STK[24:32, :], B8[:], reads=[r8], writes=[r8])
            S.dma(STK[32:40, :], A8[:], reads=[r8], writes=[r8])
            o8("dve", lambda e: e.tensor_scalar(out=X8[:], in0=B8[:], scalar1=-1.0, scalar2=None, op0=ALU.mult))
            S.dma(STK[8:16, :], X8[:], reads=[r8], writes=[r8])
            o8("dve", lambda e: e.tensor_tensor(out=X8[:], in0=B8[:], in1=A8[:], op=ALU.mult))
            S.dma(STK[16:24, :], X8[:], reads=[r8], writes=[r8])
            for c in range(36):
                o8("dve", lambda e: e.tensor_scalar(out=X8[:, c * 64:(c + 1) * 64], in0=G8[:, c * 64:(c + 1) * 64], scalar1=TOT8[:, c:c + 1], scalar2=None, op0=ALU.subtract))
            o8("act", lambda e: e.activation(out=X8[:], in_=X8[:], func=AF.Exp, scale=-1.0))
            S.dma(STK[40:48, :], X8[:], reads=[r8], writes=[r8])
            rtok = Res()
            for c in range(36):
                ps, rp = self.psum()
                S.op("pe", lambda e: e.transpose(ps[0:64, 0:48], STK[0:48, c * 64:(c + 1) * 64], ident[0:48, 0:48]), [r8, self.rconst], [rp])
                self.evac(TOK[:, c, :], ps[0:64, 0:48], [rp], [rtok], c)
            for i in range(4):
                ps, rp = self.psum()
                S.op("pe", lambda e: e.matmul(ps[:, 0:36], selp[0:8, i * 128:(i + 1) * 128], ETOT8[:], start=True, stop=True), [r8, self.rconst], [rp])
                S.op("act", lambda e: e.activation(out=ETOTP[:, i, :], in_=ps[:, 0:36], func=AF.Copy), [rp], [rtok])
            S.barrier()
            es3.close()
            Otok = self.sb(es, "cOtok", [64, 36, 256])
            rOtok = [Res() for _ in range(36)]
            mk = lambda nm, shape=(64, 256): (self.sb(es, nm, list(shape)), Res())
            def mktiles():
                t = {}
                t['Ktok, rKt'] = mk("cKtok")
                t['Vtok, rVt'] = mk("cVtok")
                t['Xt, rXt'] = mk("cXt")
                t['Xp, rXp'] = mk("cXp")
                t['Dt, rDt'] = mk("cDt")
                t['DTt, rDTt'] = mk("cDTt")
                t['Mt'] = [mk("cM%d" % i) for i in range(2)]
                t['Nt'] = [mk("cN%d" % i) for i in range(2)]
                t['Pt, rPt'] = mk("cP")
                t['qkT, rqk'] = mk("cqkT")
                t['rhsm, rrh'] = mk("crhs", (64, 512))
                t['wT, rwT'] = mk("cwT", (128, 256))
                t['u0, ru0'] = mk("cu0")
                t['ut, rut'] = mk("cu")
                t['o2, ro2'] = mk("co2")
                t['otmp, rotmp'] = mk("cotmp")
                t['Kd, rKd'] = mk("cKd")
                t['KTm, rKTm'] = mk("cKTm", (128, 256))
                t['QTm, rQTm'] = mk("cQTm", (128, 256))
                return t
            TD = [mktiles() for _ in range(2)]
            Sts = [self.sb(es, "cS%d" % i, [128, 2, 128]) for i in range(2)]
            rSs = [Res() for _ in range(2)]
            hm64 = self.cst("hm64")
            wmask = self.cst("wmask")
            HB = lambda h: slice(h * 64, (h + 1) * 64)
            id4 = self.cst("ident4")[0:64, :]
            S.op("pool", lambda e: e.memset(Otok[:], 0.0), [], rOtok)
            for d in range(2):
                S.op("pool", lambda e: e.memset(Sts[d][:], 0.0), [], [rSs[d]])

            def step(d, c):
                t = TD[d]
                Ktok, rKt = t['Ktok, rKt']
                Vtok, rVt = t['Vtok, rVt']
                Xt, rXt = t['Xt, rXt']
                Xp, rXp = t['Xp, rXp']
                Dt, rDt = t['Dt, rDt']
                DTt, rDTt = t['DTt, rDTt']
                Mt = t['Mt']
                Nt = t['Nt']
                Pt, rPt = t['Pt, rPt']
                qkT, rqk = t['qkT, rqk']
                rhsm, rrh = t['rhsm, rrh']
                wT, rwT = t['wT, rwT']
                u0, ru0 = t['u0, ru0']
                ut, rut = t['ut, rut']
                o2, ro2 = t['o2, ro2']
                otmp, rotmp = t['otmp, rotmp']
                Kd, rKd = t['Kd, rKd']
                KTm, rKTm = t['KTm, rKTm']
                QTm, rQTm = t['QTm, rQTm']
                St, rS = Sts[d], rSs[d]
                ms = self.cst("gdn_ms")[0:64, d * 256:(d + 1) * 256]
                nms = self.cst("gdn_nms")[0:64, d * 256:(d + 1) * 256]
                qi = self.cst("gdn_qi")[0:64, d * 256:(d + 1) * 256]
                cs = slice(c * 64, (c + 1) * 64)
                tk = lambda grp, h: TOK[:, c, grp * 8 + d * 4 + h:grp * 8 + d * 4 + h + 1]
                pk, rpk = self.psum_chain(d)
                pv, rpv = self.psum_chain(d)
                for hc in range(2):
                    S.op("pe", lambda e: e.transpose(pk[0:64, hc * 128:(hc + 1) * 128], QKV[:, 2 + hc, cs], ident), [rqkv, self.rconst], [rpk])
                    S.op("pe", lambda e: e.transpose(pv[0:64, hc * 128:(hc + 1) * 128], QKV[:, 4 + hc, cs], ident), [rqkv, self.rconst], [rpv])
                S.op("act", lambda e: e.activation(out=Ktok[:], in_=pk[0:64, 0:256], func=AF.Copy), [rpk], [rKt])
                S.op("dve", lambda e: e.tensor_copy(out=Vtok[:], in_=pv[0:64, 0:256]), [rpv], [rVt])
                yield
                pkk, rpkk = self.psum_chain(d)
                pqk, rpqk = self.psum_chain(d)
                pg, rpg = self.psum_chain(d)
                pb, rpb = self.psum_chain(d)
                for h in range(4):
                    S.op("pool", lambda e: e.tensor_scalar(out=KTm[:, HB(h)], in0=QKV[:, 2 + h // 2, cs], scalar1=hm64[:, h % 2:h % 2 + 1], scalar2=None, op0=ALU.mult),
                         [rqkv, self.rconst], [rKTm])
                    S.op("pool", lambda e: e.tensor_scalar(out=QTm[:, HB(h)], in0=QKV[:, h // 2, cs], scalar1=hm64[:, h % 2:h % 2 + 1], scalar2=None, op0=ALU.mult),
                         [rqkv, self.rconst], [rQTm])
                for h in range(4):
                    S.op("pe", lambda e: e.matmul(pkk[0:64, HB(h)], KTm[:, HB(h)], QKV[:, 2 + h // 2, cs], start=True, stop=True), [rqkv, rKTm], [rpkk])
                    S.op("pe", lambda e: e.matmul(pqk[0:64, HB(h)], KTm[:, HB(h)], QKV[:, h // 2, cs], start=True, stop=True), [rqkv, rKTm], [rpqk])
                    S.op("pe", lambda e: e.matmul(pg[0:64, HB(h)], sel8[0:8, HB(d * 4 + h)], G8[:, cs], start=True, stop=True), [r8, self.rconst], [rpg])
                    S.op("pe", lambda e: e.matmul(pb[0:64, HB(h)], sel8[0:8, HB(d * 4 + h)], B8[:, cs], start=True, stop=True), [r8, self.rconst], [rpb])
                yield
                for h in range(4):
                    S.op("dve", lambda e: e.tensor_scalar(out=Xt[:, HB(h)], in0=pg[0:64, HB(h)], scalar1=tk(0, h), scalar2=None, op0=ALU.subtract), [rpg, rtok], [rXt])
                S.op("dve", lambda e: e.tensor_scalar(out=Xp[:], in0=Xt[:], scalar1=0.0, scalar2=None, op0=ALU.max), [rXt], [rXp])
                S.op("pool", lambda e: e.tensor_tensor(out=Xt[:], in0=Xt[:], in1=Xp[:], op=ALU.subtract), [rXt, rXp], [rXt])
                yield
                S.op("act", lambda e: e.activation(out=Dt[:], in_=Xp[:], func=AF.Exp, scale=-1.0), [rXp], [rDt])
                S.op("act", lambda e: e.activation(out=DTt[:], in_=Xt[:], func=AF.Exp), [rXt], [rDTt])
                yield
                (M, rM), (N, rN) = Mt[0], Nt[0]
                S.op("dve", lambda e: e.tensor_tensor(out=Dt[:], in0=Dt[:], in1=pkk[0:64, 0:256], op=ALU.mult), [rDt, rpkk], [rDt])
                for h in range(4):
                    S.op("dve", lambda e: e.scalar_tensor_tensor(out=M[:, HB(h)], in0=Dt[:, HB(h)], scalar=tk(1, h), in1=ms[:, HB(h)], op0=ALU.mult, op1=ALU.mult),
                         [rDt, rtok, self.rconst], [rM])
                S.op("dve", lambda e: e.tensor_tensor(out=qkT[:], in0=DTt[:], in1=pqk[0:64, 0:256], op=ALU.mult), [rDTt, rpqk], [rqk])
                S.op("pool", lambda e: e.tensor_tensor(out=qkT[:], in0=qkT[:], in1=qi, op=ALU.mult), [rqk, self.rconst], [rqk])
                S.op("dve", lambda e: e.tensor_tensor(out=DTt[:], in0=DTt[:], in1=pkk[0:64, 0:256], op=ALU.mult), [rDTt, rpkk], [rDTt])
                S.op("dve", lambda e: e.tensor_tensor(out=DTt[:], in0=DTt[:], in1=pb[0:64, 0:256], op=ALU.mult), [rDTt, rpb], [rDTt])
                S.op("pool", lambda e: e.tensor_tensor(out=N[:], in0=DTt[:], in1=nms, op=ALU.mult), [rDTt, self.rconst], [rN])
                S.op("pool", lambda e: e.tensor_tensor(out=Pt[:], in0=N[:], in1=id4, op=ALU.add), [rN, self.rconst], [rPt])
                yield
                for lev in range(5):
                    (M, rM), (N, rN) = Mt[lev % 2], Nt[lev % 2]
                    (M2, rM2), (N2, rN2) = Mt[(lev + 1) % 2], Nt[(lev + 1) % 2]
                    pm, rpm = self.psum_chain(d)
                    for h in range(4):
                        S.op("pe", lambda e: e.matmul(pm[0:64, HB(h)], N[:, HB(h)], M[:, HB(h)], start=True, stop=True), [rM, rN], [rpm])
                    if lev < 4:
                        pn, rpn = self.psum_chain(d)
                        for h in range(4):
                            S.op("pe", lambda e: e.matmul(pn[0:64, HB(h)], M[:, HB(h)], N[:, HB(h)], start=True, stop=True), [rM, rN], [rpn])
                        S.op("dve", lambda e: e.tensor_copy(out=N2[:], in_=pn[0:64, 0:256]), [rpn], [rN2])
                    S.op("act", lambda e: e.activation(out=M2[:], in_=pm[0:64, 0:256], func=AF.Copy), [rpm], [rM2])
                    yield
                    pp, rpp = self.psum_chain(d)
                    for h in range(4):
                        S.op("pe", lambda e: e.matmul(pp[0:64, HB(h)], M2[:, HB(h)], Pt[:, HB(h)], start=True, stop=True), [rM2, rPt], [rpp])
                    S.op("dve", lambda e: e.tensor_tensor(out=Pt[:], in0=Pt[:], in1=pp[0:64, 0:256], op=ALU.add), [rPt, rpp], [rPt])
                    yield
                for h in range(4):
                    ko, vo = (0, 64) if h % 2 == 0 else (64, 0)
                    S.op("dve", lambda e: e.tensor_scalar(out=rhsm[:, h * 128 + ko:h * 128 + ko + 64], in0=Ktok[:, HB(h)], scalar1=tk(2, h), scalar2=None, op0=ALU.mult),
                         [rKt, rtok], [rrh])
                    S.op("pool", lambda e: e.tensor_scalar(out=rhsm[:, h * 128 + vo:h * 128 + vo + 64], in0=Vtok[:, HB(h)], scalar1=tk(3, h), scalar2=None, op0=ALU.mult),
                         [rVt, rtok], [rrh])
                yield
                pst, rpst = self.psum_chain(d)
                pso, rpso = self.psum_chain(d)
                for h in range(4):
                    S.op("pe", lambda e: e.matmul(pst[:, HB(h)], rhsm[:, h * 128:(h + 1) * 128], Pt[:, HB(h)], start=True, stop=True), [rrh, rPt], [rpst])
                    S.op("pe", lambda e: e.matmul(pso[0:64, h * 128:(h + 1) * 128], Pt[:, HB(h)], rhsm[:, h * 128:(h + 1) * 128], start=True, stop=True), [rrh, rPt], [rpso])
                S.op("dve", lambda e: e.tensor_tensor(out=wT[:], in0=pst[:, 0:256], in1=wmask, op=ALU.mult), [rpst, self.rconst], [rwT])
                for h in range(4):
                    vo = 64 if h % 2 == 0 else 0
                    S.op("dve", lambda e: e.tensor_copy(out=u0[:, HB(h)], in_=pso[0:64, h * 128 + vo:h * 128 + vo + 64]), [rpso], [ru0])
                yield
                pw, rpw = self.psum_chain(d)
                po1, rpo1 = self.psum_chain(d)
                for h in range(4):
                    p0 = (h % 2) * 64
                    Sh = St[:, h // 2, p0:p0 + 64]
                    S.op("pe", lambda e: e.matmul(pw[0:64, HB(h)], wT[:, HB(h)], Sh, start=True, stop=True), [rwT, rS], [rpw])
                    S.op("pe", lambda e: e.matmul(po1[0:64, HB(h)], QTm[:, HB(h)], Sh, start=True, stop=True), [rQTm, rS], [rpo1])
                S.op("dve", lambda e: e.tensor_tensor(out=ut[:], in0=u0[:], in1=pw[0:64, 0:256], op=ALU.subtract), [ru0, rpw], [rut])
                yield
                po2, rpo2 = self.psum_chain(d)
                for h in range(4):
                    S.op("pe", lambda e: e.matmul(po2[0:64, HB(h)], qkT[:, HB(h)], ut[:, HB(h)], start=True, stop=True), [rqk, rut], [rpo2])
                S.op("act", lambda e: e.activation(out=o2[:], in_=po2[0:64, 0:256], func=AF.Copy), [rpo2], [ro2])
                yield
                for h in range(4):
                    S.op("dve", lambda e: e.scalar_tensor_tensor(out=otmp[:, HB(h)], in0=po1[0:64, HB(h)], scalar=tk(4, h), in1=o2[:, HB(h)], op0=ALU.mult, op1=ALU.add),
                         [rpo1, ro2, rtok], [rotmp])
                S.op("pool", lambda e: e.tensor_tensor(out=Otok[:, c, :], in0=Otok[:, c, :], in1=otmp[:], op=ALU.add), [rotmp, rOtok[c]], [rOtok[c]])
                yield
                for h in range(4):
                    S.op("pool", lambda e: e.tensor_scalar(out=Kd[:, HB(h)], in0=Ktok[:, HB(h)], scalar1=tk(5, h), scalar2=None, op0=ALU.mult), [rKt, rtok], [rKd])
                for hc in range(2):
                    pS, rpS = self.psum_chain(d)
                    S.op("pe", lambda e: e.matmul(pS[:, 0:128], Kd[:, hc * 128:(hc + 1) * 128], ut[:, hc * 128:(hc + 1) * 128], start=True, stop=True), [rKd, rut], [rpS])
                    S.op("dve", lambda e: e.scalar_tensor_tensor(out=St[:, hc, :], in0=St[:, hc, :], scalar=ETOTP[:, d * 2 + hc, c:c + 1], in1=pS[:, 0:128],
                                                                 op0=ALU.mult, op1=ALU.add), [rS, rpS, rtok], [rS])

            orders = [list(range(32, 36)) + list(range(32)), list(range(35, 31, -1)) + list(range(31, -1, -1))]
            for n in range(36):
                gens = [step(d, orders[d][n]) for d in range(2)]
                while gens:
                    for g in list(gens):
                        try:
                            next(g)
                        except StopIteration:
                            gens.remove(g)
            n = 0
            for c in range(36):
                for hc in range(2):
                    ps, rp = self.psum()
                    S.op("pe", lambda e: e.transpose(ps[:, 0:64], Otok[:, c, hc * 128:(hc + 1) * 128], ident[0:64, 0:64]), [rOtok[c], self.rconst], [rp])
                    self.evac(OT[:, hc, c * 64:(c + 1) * 64], ps[:, 0:64], [rp], [rOT], n)
                    n += 1
            S.barrier()
            es.close()
            self.gated_tail(eso, [OT, None], [rOT, None], l, "c_og", 2064, 512)


    def mixer_d(self, b, l):
        S, A = self.S, self.A
        ident = self.cst("ident")
        with ExitStack() as eso:
            OTs = [self.sb(eso, "dOT%d" % i, [128, 2, T]) for i in range(2)]
            es = ExitStack()
            self.load_mconsts(es)
            gmask = self.cst("gla_mask")
            hm32 = self.cst("hmask32")
            sblk = self.cst("sblk")
            qT, rq = self.load_rows(es, "dq", 2320, 1)
            kT, rk = self.load_rows(es, "dk", 2448, 1)
            Vtok = self.sb(es, "dvtok", [64, 36, 256])
            rVt = Res()
            with ExitStack() as es2:
                vT, rv = self.load_rows(es2, "dv", 2576, 2)
                n = 0
                for c2 in range(2):
                    for c in range(36):
                        ps, rp = self.psum()
                        S.op("pe", lambda e: e.transpose(ps[0:64, 0:128], vT[:, c2, c * 64:(c + 1) * 64], ident), [rv, self.rconst], [rp])
                        self.evac(Vtok[:, c, c2 * 128:(c2 + 1) * 128], ps[0:64, 0:128], [rp], [rVt], n)
                        n += 1
                S.barrier()
            rOTs = [Res() for _ in range(2)]
            cm = self.sb(es, "cm", [128, T])
            rcm = Res()
            S.dma(cm[:], A["cmask"][:, :], writes=[rcm])
            LA = self.sb(es, "dLA", [128, T])
            G = self.sb(es, "dG", [128, T])
            QE = self.sb(es, "dQE", [128, T])
            KE = self.sb(es, "dKE", [128, T])
            KH = self.sb(es, "dKH", [128, T])
            TOT = self.sb(es, "dTOT", [128, 36])
            ETOT = self.sb(es, "dETOT", [128, 36])
            lr = self.sb(es, "dlr", [16, T])
            gw = self.sb(es, "dgw", [16, 128])
            St = self.sb(es, "dS", [128, 256])
            at = [self.sb(es, "dat%d" % i, [64, 256]) for i in range(2)]
            ktok = [self.sb(es, "dktok%d" % i, [64, 128]) for i in range(2)]
            KEm = [self.sb(es, "dKEm%d" % i, [128, 4, 64]) for i in range(2)]
            rKEm = [Res() for _ in range(2)]
            rw = Res()
            rS = Res()
            rat = [Res() for _ in range(2)]
            rkt = [Res() for _ in range(2)]
            gbo = PV["d_gb"][0]
            for d in range(2):
                OT, rOT = OTs[d], rOTs[d]
                S.dma(lr[:], A["projT"][2832 + 16 * d:2848 + 16 * d, :], reads=[self.rprojT], writes=[rw])
                S.dma(gw[:], A["d_gw"][l, d], writes=[rw])
                for t0 in range(0, T, 512):
                    tn = min(512, T - t0)
                    ps, rp = self.psum()
                    S.op("pe", lambda e: e.matmul(ps[:, 0:tn], gw[:], lr[:, t0:t0 + tn], start=True, stop=True), [rw], [rp])
                    S.op("dve", lambda e: e.tensor_scalar(out=LA[:, t0:t0 + tn], in0=ps[:, 0:tn], scalar1=self.pvec[:, l, gbo + d:gbo + d + 1], scalar2=None,
                                                          op0=ALU.add), [rp, self.rconst], [rw])
                S.op("act", lambda e: e.activation(out=LA[:], in_=LA[:], func=AF.Exp, scale=-1.0), [rw], [rw])
                S.op("pool", lambda e: e.tensor_scalar(out=LA[:], in0=LA[:], scalar1=1.0, scalar2=None, op0=ALU.add), [rw], [rw])
                S.op("act", lambda e: e.activation(out=LA[:], in_=LA[:], func=AF.Ln), [rw], [rw])
                S.op("pool", lambda e: e.tensor_scalar(out=LA[:], in0=LA[:], scalar1=-1.0 / 16.0, scalar2=None, op0=ALU.mult), [rw], [rw])
                S.op("dve", lambda e: e.tensor_tensor_scan(out=G[:], data0=cm[:], data1=LA[:], initial=0.0, op0=ALU.mult, op1=ALU.add), [rw, rcm], [rw])
                S.op("dve", lambda e: e.tensor_reduce(out=TOT[:], in_=LA[:].rearrange("p (c i) -> p c i", i=64), axis=AX.X, op=ALU.add), [rw], [rw])
                if d == 1:
                    S.op("pool", lambda e: e.tensor_tensor(out=G[:], in0=LA[:], in1=G[:], op=ALU.subtract), [rw], [rw])
                    for c in range(36):
                        S.op("dve", lambda e: e.tensor_scalar(out=G[:, c * 64:(c + 1) * 64], in0=G[:, c * 64:(c + 1) * 64], scalar1=TOT[:, c:c + 1], scalar2=None,
                                                              op0=ALU.add), [rw], [rw])
                S.op("act", lambda e: e.activation(out=QE[:], in_=G[:], func=AF.Exp), [rw], [rw])
                S.op("dve", lambda e: e.scalar_tensor_tensor(out=QE[:], in0=QE[:], scalar=32.0 ** -0.5, in1=qT[:, 0, :], op0=ALU.mult, op1=ALU.mult), [rw, rq], [rw])
                S.op("act", lambda e: e.activation(out=KE[:], in_=G[:], func=AF.Exp, scale=-1.0), [rw], [rw])
                S.op("pool", lambda e: e.tensor_tensor(out=KE[:], in0=KE[:], in1=kT[:, 0, :], op=ALU.mult), [rw, rk], [rw])
                for c in range(36):
                    S.op("dve", lambda e: e.tensor_scalar(out=KH[:, c * 64:(c + 1) * 64], in0=G[:, c * 64:(c + 1) * 64], scalar1=TOT[:, c:c + 1], scalar2=None,
                                                          op0=ALU.subtract), [rw], [rw])
                S.op("act", lambda e: e.activation(out=KH[:], in_=KH[:], func=AF.Exp, scale=-1.0), [rw], [rw])
                S.op("pool", lambda e: e.tensor_tensor(out=KH[:], in0=KH[:], in1=kT[:, 0, :], op=ALU.mult), [rw, rk], [rw])
                S.op("act", lambda e: e.activation(out=ETOT[:], in_=TOT[:], func=AF.Exp), [rw], [rw])
                S.op("pool", lambda e: e.memset(St[:], 0.0), [], [rS])
                order = (list(range(32, 36)) + list(range(32))) if d == 0 else (list(range(35, 31, -1)) + list(range(31, -1, -1)))
                for n, c in enumerate(order):
                    cs = slice(c * 64, (c + 1) * 64)
                    a_, ra = at[n % 2], rat[n % 2]
                    k_, rk_ = ktok[n % 2], rkt[n % 2]
                    pa, rpa = self.psum()
                    kem, rkem = KEm[n % 2], rKEm[n % 2]
                    for h in range(4):
                        S.op("pool", lambda e: e.tensor_scalar(out=kem[:, h, :], in0=KE[:, cs], scalar1=hm32[:, h:h + 1], scalar2=None, op0=ALU.mult), [rw, self.rconst], [rkem])
                    for h in range(4):
                        S.op("pe", lambda e: e.matmul(pa[0:64, h * 64:(h + 1) * 64], kem[:, h, :], QE[:, cs], start=True, stop=True), [rw, rkem], [rpa])
                    S.op("dve", lambda e: e.tensor_tensor(out=a_[:], in0=pa[0:64, 0:256], in1=gmask[0:64, d * 256:(d + 1) * 256], op=ALU.mult), [rpa, self.rconst], [ra])
                    pk, rpk = self.psum()
                    S.op("pe", lambda e: e.transpose(pk[0:64, 0:128], KH[:, cs], ident), [rw, self.rconst], [rpk])
                    S.op("act", lambda e: e.activation(out=k_[:], in_=pk[0:64, 0:128], func=AF.Copy), [rpk], [rk_])
                    po, rpo = self.psum()
                    for h in range(4):
                        hs = slice(h * 32, (h + 1) * 32)
                        es_ = slice(h * 64, (h + 1) * 64)
                        S.op("pe", lambda e: e.matmul(po[0:64, es_], St[:, es_], QE[:, cs], start=True, stop=False), [rS, rw], [rpo])
                        S.op("pe", lambda e: e.matmul(po[0:64, es_], Vtok[:, c, es_], a_[:, es_], start=False, stop=True), [rVt, ra], [rpo])
                    for h in range(4):
                        p0 = (h % 2) * 64
                        S.op("act", lambda e: e.activation(out=OT[p0:p0 + 64, h // 2, cs], in_=po[0:64, h * 64:(h + 1) * 64], func=AF.Copy), [rpo], [rOT])
                    pS, rpS = self.psum()
                    S.op("pe", lambda e: e.matmul(pS[:, 0:256], k_[:], Vtok[:, c, :], start=True, stop=True), [rk_, rVt], [rpS])
                    S.op("dve", lambda e: e.scalar_tensor_tensor(out=St[:], in0=St[:], scalar=ETOT[:, c:c + 1], in1=pS[:, 0:256], op0=ALU.mult, op1=ALU.add),
                         [rS, rpS, rw], [rS])
                    S.op("dve", lambda e: e.tensor_tensor(out=St[:], in0=St[:], in1=sblk, op=ALU.mult), [rS, self.rconst], [rS])
            S.barrier()
            es.close()
            self.gated_tail(eso, OTs, rOTs, l, "d_og", 2864, 768)

    def gated_tail(self, es, OTs, rOTs, l, gname, gate_row, zrow):
        S, A = self.S, self.A
        OT, rOT = OTs[0], rOTs[0]
        tmps = [(self.sb(es, "gt%d" % i, [128, 512]), Res()) for i in range(2)]
        gate, rg = self.load_rows(es, "gate", gate_row, 2)
        S.op("act", lambda e: e.activation(out=gate[:], in_=gate[:], func=AF.Silu), [rg], [rg])
        go = PV[gname][0]
        for c in range(2):
            if OTs[1] is not None:
                S.op("pool", lambda e: e.tensor_tensor(out=OT[:, c, :], in0=OT[:, c, :], in1=OTs[1][:, c, :], op=ALU.add), [rOT, rOTs[1]], [rOT])
            self.headnorm(OT, rOT, c, tmps, self.cst("bd64"), self.pvec[:, l, go:go + 1])
            S.op("pool", lambda e: e.tensor_tensor(out=OT[:, c, :], in0=OT[:, c, :], in1=gate[:, c, :], op=ALU.mult), [rOT, rg], [rOT])
        S.dma(A["oT"][zrow:zrow + 256, :].rearrange("(c p) t -> p c t", p=128), OT[:], reads=[rOT], writes=[self.roT], eng="pool")


    def layer_norm(self, es_tiles, l, t0, tn, gname, bname):
        S = self.S
        sq, rsq, rstd, rrstd = es_tiles
        onesln = self.cst("onesln")
        mean_ps, rmp = self.psum()
        for k in range(8):
            S.op("pe", lambda e, k=k: e.matmul(mean_ps[:, 0:tn], onesln, self.xT[:, k, t0:t0 + tn], start=(k == 0), stop=(k == 7)),
                 [self.rconst] + self.rxs(k, t0, tn), [rmp])
        for k in range(8):
            S.op("dve", lambda e, k=k: e.tensor_tensor(out=self.xT[:, k, t0:t0 + tn], in0=self.xT[:, k, t0:t0 + tn], in1=mean_ps[:, 0:tn],
                                                     op=ALU.subtract), [rmp] + self.rxs(k, t0, tn), self.rxs(k, t0, tn))
        var_ps, rvp = self.psum()
        for k in range(8):
            s_, r_ = sq[k % 2], rsq[k % 2]
            S.op("pool", lambda e, k=k, s_=s_: e.tensor_tensor(out=s_[:, 0:tn], in0=self.xT[:, k, t0:t0 + tn], in1=self.xT[:, k, t0:t0 + tn], op=ALU.mult),
                 self.rxs(k, t0, tn), [r_])
            S.op("pe", lambda e, k=k, s_=s_: e.matmul(var_ps[:, 0:tn], onesln, s_[:, 0:tn], start=(k == 0), stop=(k == 7)),
                 [r_, self.rconst], [rvp])
        epsc = self.cst("cvec")[:, 0:1]
        S.op("dve", lambda e: e.tensor_scalar(out=rstd[:, 0:tn], in0=var_ps[:, 0:tn], scalar1=EPS, scalar2=None, op0=ALU.add), [rvp], [rrstd])
        S.op("act", lambda e: e.activation(out=rstd[:, 0:tn], in_=rstd[:, 0:tn], func=AF.Sqrt), [rrstd], [rrstd])
        S.op("dve", lambda e: e.reciprocal(out=rstd[:, 0:tn], in_=rstd[:, 0:tn]), [rrstd], [rrstd])
        go, bo = PV[gname][0], PV[bname][0]
        for k in range(8):
            S.op("dve", lambda e, k=k: e.scalar_tensor_tensor(out=self.xT[:, k, t0:t0 + tn], in0=self.xT[:, k, t0:t0 + tn],
                                                            scalar=self.pvec[:, l, go + k:go + k + 1], in1=rstd[:, 0:tn],
                                                            op0=ALU.mult, op1=ALU.mult), [rrstd, self.rconst] + self.rxs(k, t0, tn), self.rxs(k, t0, tn))
            S.op("pool", lambda e, k=k: e.tensor_scalar(out=self.xT[:, k, t0:t0 + tn], in0=self.xT[:, k, t0:t0 + tn],
                                                      scalar1=self.pvec[:, l, bo + k:bo + k + 1], scalar2=None, op0=ALU.add),
                 self.rxs(k, t0, tn) + [self.rconst], self.rxs(k, t0, tn))

    def phase_merge(self, b, l):
        nc, S, A = self.nc, self.S, self.A
        TB = 256
        with ExitStack() as es:
            wbr = self.sb(es, "wbr", [128, 8, D])
            rwbr = Res()
            S.dma(wbr[:], A["w_branch"][l].rearrange("z (c p) d -> p (z c) d", p=128), writes=[rwbr])
            wo = self.sb(es, "wo", [128, 8, D])
            rwo = Res()
            S.dma(wo[:], A["w_out"][l].rearrange("(k p) d -> p k d", p=128), writes=[rwo])
            oTb = [self.sb(es, "oTb%d" % i, [128, 8, TB]) for i in range(2)]
            roTb = [Res() for _ in range(2)]
            G = [self.sb(es, "G%d" % i, [128, 4, TB]) for i in range(2)]
            rG = [Res() for _ in range(2)]
            m = self.sb(es, "m", [128, 8, TB])
            rm = [Res() for _ in range(8)]
            tmp = [self.sb(es, "mtmp%d" % i, [128, TB]) for i in range(2)]
            rtmp = [Res() for _ in range(2)]
            sq = [self.sb(es, "lnsq%d" % i, [128, TB]) for i in range(2)]
            rsq = [Res() for _ in range(2)]
            rstd = self.sb(es, "lnrstd", [128, TB])
            rrstd = Res()
            ng = 0
            nt = 0
            for bi in range(T // TB):
                t0 = bi * TB
                if l == self.dbg.get("ctx_skip_layer", DEPTH - 1) and t0 >= TL:
                    continue
                col = b if t0 < TL else 2
                ob, rob = oTb[bi % 2], roTb[bi % 2]
                S.dma(ob[:], A["oT"][:, t0:t0 + TB].rearrange("(c p) t -> p c t", p=128), reads=[self.roT], writes=[rob])
                for dmc in range(8):
                    g, rg = G[ng % 2], rG[ng % 2]
                    ng += 1
                    S.dma(g[:], A["projT"][3120:7216, t0:t0 + TB].rearrange("(z c p) t -> c p z t", z=4, p=128)[dmc],
                          reads=[self.rprojT], writes=[rg])
                    S.op("act", lambda e, g=g: e.activation(out=g[:], in_=g[:], func=AF.Sigmoid), [rg], [rg])
                    for z in range(4):
                        ps, rp = self.psum()
                        for c2 in range(2):
                            S.op("pe", lambda e, ps=ps, z=z, c2=c2, dmc=dmc, ob=ob: e.matmul(ps[:, 0:TB], wbr[:, z * 2 + c2, dmc * 128:(dmc + 1) * 128],
                                                                                       ob[:, z * 2 + c2, :], start=(c2 == 0), stop=(c2 == 1)),
                                 [rwbr, rob], [rp])
                        if z == 0:
                            S.op("dve", lambda e, ps=ps, g=g, dmc=dmc: e.tensor_tensor(out=m[:, dmc, :], in0=ps[:, 0:TB], in1=g[:, 0, :], op=ALU.mult),
                                 [rp, rg], [rm[dmc]])
                        else:
                            tt, rt = tmp[nt % 2], rtmp[nt % 2]
                            nt += 1
                            S.op("dve", lambda e, ps=ps, g=g, z=z, tt=tt: e.tensor_tensor(out=tt[:], in0=ps[:, 0:TB], in1=g[:, z, :], op=ALU.mult),
                                 [rp, rg], [rt])
                            S.op("pool", lambda e, tt=tt, dmc=dmc: e.tensor_tensor(out=m[:, dmc, :], in0=m[:, dmc, :], in1=tt[:], op=ALU.add),
                                 [rt, rm[dmc]], [rm[dmc]])
                for d2 in range(8):
                    ps, rp = self.psum()
                    for k in range(8):
                        S.op("pe", lambda e, ps=ps, k=k, d2=d2: e.matmul(ps[:, 0:TB], wo[:, k, d2 * 128:(d2 + 1) * 128], m[:, k, :],
                                                                       start=(k == 0), stop=(k == 7)), [rwo, rm[k]], [rp])
                    tt, rt = tmp[nt % 2], rtmp[nt % 2]
                    nt += 1
                    S.op("act", lambda e, ps=ps, tt=tt, d2=d2: e.activation(out=tt[:], in_=ps[:, 0:TB], func=AF.Identity, scale=self.mod(l, 2, d2, col)),
                         [rp, self.rmod], [rt])
                    S.op("dve", lambda e, tt=tt, d2=d2: e.scalar_tensor_tensor(out=self.xT[:, d2, t0:t0 + TB], in0=self.xT[:, d2, t0:t0 + TB], scalar=ALPHA,
                                                                            in1=tt[:], op0=ALU.mult, op1=ALU.add),
                         [rt] + self.rxs(d2, t0, TB), self.rxs(d2, t0, TB))
                if self.dbg.get("merge_ln", True):
                    self.layer_norm((sq, rsq, rstd, rrstd), l, t0, TB, "ln1_g", "ln1_b")

    def phase_moe(self, b, l):
        nc, S, A = self.nc, self.S, self.A
        ident = self.cst("ident")
        with ExitStack() as es:
            h2 = self.sb(es, "h2", [128, 8, T], BF16)
            rh2 = Res()
            denseT = self.sb(es, "denseT", [32, T])
            rdT = [Res() for _ in range(5)]
            for k in range(8):
                S.op("dve", lambda e, k=k: e.tensor_scalar(out=h2[:, k, 0:TL], in0=self.xT[:, k, 0:TL], scalar1=self.mod(l, 4, k, b),
                                                         scalar2=self.mod(l, 3, k, b), op0=ALU.mult, op1=ALU.add),
                     [self.rmod] + self.rx[k][0:4], [rh2])
                S.op("pool", lambda e, k=k: e.tensor_scalar(out=h2[:, k, TL:T], in0=self.xT[:, k, TL:T], scalar1=self.mod(l, 4, k, 2),
                                                          scalar2=self.mod(l, 3, k, 2), op0=ALU.mult, op1=ALU.add),
                     [self.rmod, self.rx[k][4]], [rh2])
            with ExitStack() as es2:
                wrt = self.sb(es2, "wrt", [128, 8, 36])
                rwrt = Res()
                S.dma(wrt[:], A["w_rt"][l].rearrange("(k p) c -> p k c", p=128), writes=[rwrt])
                brow = self.sb(es2, "brow", [1, 36])
                S.dma(brow[:], A["b_rt"][l:l + 1, :], writes=[rwrt])
                h2f = [self.sb(es2, "h2f%d" % i, [128, 8, 128]) for i in range(2)]
                rh2f = [Res() for _ in range(2)]
                R = [self.sb(es2, "rt%d" % i, [128, 160]) for i in range(2)]
                rR = [Res() for _ in range(2)]
                ones = self.cst("ones")
                skipc = (l == self.dbg.get("ctx_skip_layer", DEPTH - 1))
                for tt in range((TL if skipc else T) // 128):
                    t0 = tt * 128
                    col = b if t0 < TL else 2
                    hf, rhf = h2f[tt % 2], rh2f[tt % 2]
                    r_, rr = R[tt % 2], rR[tt % 2]
                    for k in range(8):
                        S.op("dve", lambda e, k=k, hf=hf: e.tensor_scalar(out=hf[:, k, :], in0=self.xT[:, k, t0:t0 + 128], scalar1=self.mod(l, 4, k, col),
                                                                        scalar2=self.mod(l, 3, k, col), op0=ALU.mult, op1=ALU.add),
                             [self.rmod] + self.rxs(k, t0, 128), [rhf])
                    ps, rp = self.psum()
                    for k in range(8):
                        S.op("pe", lambda e, k=k, hf=hf, ps=ps: e.matmul(ps[:, 0:36], hf[:, k, :], wrt[:, k, :], start=(k == 0), stop=False), [rhf, rwrt], [rp])
                    S.op("pe", lambda e, ps=ps: e.matmul(ps[:, 0:36], ones[0:1, :], brow[0:1, :], start=False, stop=True), [rwrt, self.rconst], [rp])
                    L = r_[:, 0:36]
                    sc = lambda i, r_=r_: r_[:, 140 + i:141 + i]
                    MG, NMG, SG, PG, M1, M2, DD, EE, RR, W1, W2 = range(11)
                    S.op("act", lambda e, ps=ps: e.activation(out=L, in_=ps[:, 0:36], func=AF.Copy), [rp], [rr])
                    dv = lambda fn: S.op("dve", fn, [rr], [rr])
                    dv(lambda e: e.tensor_reduce(out=sc(MG), in_=r_[:, 0:4], axis=AX.X, op=ALU.max))
                    dv(lambda e: e.tensor_scalar(out=r_[:, 60:64], in0=r_[:, 0:4], scalar1=sc(MG), scalar2=None, op0=ALU.subtract))
                    S.op("act", lambda e: e.activation(out=r_[:, 60:64], in_=r_[:, 60:64], func=AF.Exp), [rr], [rr])
                    dv(lambda e: e.tensor_reduce(out=sc(SG), in_=r_[:, 60:64], axis=AX.X, op=ALU.add))
                    dv(lambda e: e.reciprocal(out=sc(PG), in_=sc(SG)))
                    dv(lambda e: e.tensor_scalar(out=r_[:, 40:44], in0=r_[:, 0:4], scalar1=sc(MG), scalar2=None, op0=ALU.is_equal))
                    dv(lambda e: e.tensor_scalar(out=r_[:, 44:52], in0=r_[:, 4:12], scalar1=r_[:, 40:41], scalar2=None, op0=ALU.mult))
                    for g in range(1, 4):
                        dv(lambda e, g=g: e.scalar_tensor_tensor(out=r_[:, 44:52], in0=r_[:, 4 + 8 * g:12 + 8 * g], scalar=r_[:, 40 + g:41 + g],
                                                                 in1=r_[:, 44:52], op0=ALU.mult, op1=ALU.add))
                    dv(lambda e: e.tensor_reduce(out=sc(M1), in_=r_[:, 44:52], axis=AX.X, op=ALU.max))
                    dv(lambda e: e.tensor_scalar(out=r_[:, 52:60], in0=r_[:, 44:52], scalar1=sc(M1), scalar2=None, op0=ALU.is_equal))
                    dv(lambda e: e.scalar_tensor_tensor(out=r_[:, 60:68], in0=r_[:, 52:60], scalar=NEG, in1=r_[:, 44:52], op0=ALU.mult, op1=ALU.add))
                    dv(lambda e: e.tensor_reduce(out=sc(M2), in_=r_[:, 60:68], axis=AX.X, op=ALU.max))
                    dv(lambda e: e.tensor_scalar(out=r_[:, 68:76], in0=r_[:, 60:68], scalar1=sc(M2), scalar2=None, op0=ALU.is_equal))
                    dv(lambda e: e.tensor_tensor(out=sc(DD), in0=sc(M2), in1=sc(M1), op=ALU.subtract))
                    S.op("act", lambda e: e.activation(out=sc(EE), in_=sc(DD), func=AF.Exp), [rr], [rr])
                    dv(lambda e: e.tensor_scalar(out=sc(RR), in0=sc(EE), scalar1=1.0, scalar2=None, op0=ALU.add))
                    dv(lambda e: e.reciprocal(out=sc(RR), in_=sc(RR)))
                    dv(lambda e: e.tensor_tensor(out=sc(W1), in0=sc(RR), in1=sc(PG), op=ALU.mult))
                    dv(lambda e: e.tensor_tensor(out=sc(W2), in0=sc(W1), in1=sc(EE), op=ALU.mult))
                    dv(lambda e: e.tensor_scalar(out=r_[:, 76:84], in0=r_[:, 52:60], scalar1=sc(W1), scalar2=None, op0=ALU.mult))
                    dv(lambda e: e.scalar_tensor_tensor(out=r_[:, 76:84], in0=r_[:, 68:76], scalar=sc(W2), in1=r_[:, 76:84], op0=ALU.mult, op1=ALU.add))
                    for g in range(4):
                        dv(lambda e, g=g: e.tensor_scalar(out=r_[:, 84 + 8 * g:92 + 8 * g], in0=r_[:, 76:84], scalar1=r_[:, 40 + g:41 + g], scalar2=None,
                                                          op0=ALU.mult))
                    ps2, rp2 = self.psum()
                    S.op("pe", lambda e, ps2=ps2: e.transpose(ps2[0:32, 0:128], r_[:, 84:116], ident), [rr, self.rconst], [rp2])
                    S.op("act", lambda e, ps2=ps2: e.activation(out=denseT[:, t0:t0 + 128], in_=ps2[0:32, 0:128], func=AF.Copy), [rp2], [rdT[t0 // 512]])
                S.barrier()
            self.dump("denseT", denseT[:], [32, T], rdT)
            for k in range(8):
                for j in range(5):
                    t0 = j * 512
                    tn = min(512, T - t0)
                    S.op("pool", lambda e, k=k, t0=t0, tn=tn: e.tensor_scalar(out=self.xT[:, k, t0:t0 + tn], in0=self.xT[:, k, t0:t0 + tn], scalar1=ALPHA,
                                                                          scalar2=None, op0=ALU.mult), [self.rx[k][j]], [self.rx[k][j]])
            with ExitStack() as es3:
                NSTG = 2
                stg = [self.sb(es3, "stg%d" % i, [128, 2048]) for i in range(NSTG)]
                rstg = [Res() for _ in range(NSTG)]
                wgu = [self.sb(es3, "wgu%d" % i, [128, 8, 512], BF16) for i in range(3)]
                rwgu = [Res() for _ in range(3)]
                wdn = [self.sb(es3, "wdn%d" % i, [128, 4, D], BF16) for i in range(2)]
                rwdn = [Res() for _ in range(2)]
                abf = [self.sb(es3, "abf%d" % i, [128, 4, 512], BF16) for i in range(2)]
                rabf = [Res() for _ in range(2)]
                sgt = [self.sb(es3, "sgt%d" % i, [128, 512]) for i in range(2)]
                rsgt = [Res() for _ in range(2)]
                tt_ = [self.sb(es3, "ttm%d" % i, [128, 512]) for i in range(2)]
                rtt = [Res() for _ in range(2)]
                dsel = [self.sb(es3, "dsel%d" % i, [32, 512]) for i in range(2)]
                rdsel = [Res() for _ in range(2)]
                dB = [self.sb(es3, "dB%d" % i, [128, 512]) for i in range(2)]
                rdB = [Res() for _ in range(2)]
                ones = self.cst("ones")
                nstg = 0
                ngu = 0
                cnt = 0
                for e_ in range(self.dbg.get("e_start", 0), self.dbg.get("nexp", 32)):
                    ws = []
                    for wi, nm in enumerate(("w_gate", "w_up")):
                        w, rw = wgu[ngu % 3], rwgu[ngu % 3]
                        ngu += 1
                        for hh in range(2):
                            s_, rs_ = stg[nstg % NSTG], rstg[nstg % NSTG]
                            nstg += 1
                            S.dma(s_[:].rearrange("p (k h) -> p k h", k=8), A[nm][l, e_, :, hh * 256:(hh + 1) * 256].rearrange("(k p) h -> p k h", p=128),
                                  writes=[rs_])
                            S.op("act", lambda e, s_=s_, w=w, hh=hh: e.activation(out=w[:, :, hh * 256:(hh + 1) * 256],
                                                                               in_=s_[:].rearrange("p (k h) -> p k h", k=8), func=AF.Copy), [rs_], [rw])
                        ws.append((w, rw))
                    (wg, rwg), (wu, rwu) = ws
                    wd, rwd = wdn[e_ % 2], rwdn[e_ % 2]
                    for hh in range(2):
                        s_, rs_ = stg[nstg % NSTG], rstg[nstg % NSTG]
                        nstg += 1
                        S.dma(s_[:].rearrange("p (c d) -> p c d", c=2), A["w_down"][l, e_, hh * 256:(hh + 1) * 256, :].rearrange("(c p) d -> p c d", p=128),
                              writes=[rs_])
                        S.op("pool", lambda e, s_=s_, wd=wd, hh=hh: e.tensor_copy(out=wd[:, hh * 2:hh * 2 + 2, :],
                                                                               in_=s_[:].rearrange("p (c d) -> p c d", c=2)), [rs_], [rwd])
                    def stage_a(tb):
                        t0 = tb * 512
                        tn = min(512, T - t0)
                        cnt = e_ * 5 + tb
                        ds_, rds = dsel[cnt % 2], rdsel[cnt % 2]
                        db_, rdb = dB[cnt % 2], rdB[cnt % 2]
                        ab, rab = abf[cnt % 2], rabf[cnt % 2]
                        S.op("dve", lambda e: e.tensor_scalar(out=ds_[:, 0:tn], in0=denseT[:, t0:t0 + tn], scalar1=ident[0:32, e_:e_ + 1],
                                                              scalar2=None, op0=ALU.mult), [rdT[tb], self.rconst], [rds])
                        psb, rpb = self.psum()
                        S.op("pe", lambda e: e.matmul(psb[:, 0:tn], ones[0:32, :], ds_[:, 0:tn], start=True, stop=True), [rds, self.rconst], [rpb])
                        S.op("act", lambda e: e.activation(out=db_[:, 0:tn], in_=psb[:, 0:tn], func=AF.Copy), [rpb], [rdb])
                        for hc in range(4):
                            psg, rpg = self.psum()
                            for k in range(8):
                                S.op("pe", lambda e: e.matmul(psg[:, 0:tn], wg[:, k, hc * 128:(hc + 1) * 128], h2[:, k, t0:t0 + tn],
                                                              start=(k == 0), stop=(k == 7)), [rwg, rh2], [rpg])
                            psu, rpu = self.psum()
                            for k in range(8):
                                S.op("pe", lambda e: e.matmul(psu[:, 0:tn], wu[:, k, hc * 128:(hc + 1) * 128], h2[:, k, t0:t0 + tn],
                                                              start=(k == 0), stop=(k == 7)), [rwu, rh2], [rpu])
                            i2 = (cnt * 4 + hc) % 2
                            sg_, rsg = sgt[i2], rsgt[i2]
                            t_, rt_ = tt_[i2], rtt[i2]
                            S.op("act", lambda e: e.activation(out=sg_[:, 0:tn], in_=psg[:, 0:tn], func=AF.Silu), [rpg], [rsg])
                            S.op("dve", lambda e: e.tensor_tensor(out=t_[:, 0:tn], in0=psu[:, 0:tn], in1=sg_[:, 0:tn], op=ALU.mult), [rpu, rsg], [rt_])
                            S.op("pool", lambda e: e.tensor_tensor(out=ab[:, hc, 0:tn], in0=t_[:, 0:tn], in1=db_[:, 0:tn], op=ALU.mult), [rt_, rdb], [rab])

                    def stage_b(tb):
                        t0 = tb * 512
                        tn = min(512, T - t0)
                        col = b if t0 < TL else 2
                        cnt = e_ * 5 + tb
                        ab, rab = abf[cnt % 2], rabf[cnt % 2]
                        for dmc in range(8):
                            psd, rpd = self.psum()
                            for hc in range(4):
                                S.op("pe", lambda e: e.matmul(psd[:, 0:tn], wd[:, hc, dmc * 128:(dmc + 1) * 128], ab[:, hc, 0:tn],
                                                              start=(hc == 0), stop=(hc == 3)), [rwd, rab], [rpd])
                            S.op("dve", lambda e: e.scalar_tensor_tensor(out=self.xT[:, dmc, t0:t0 + tn], in0=psd[:, 0:tn], scalar=self.mod(l, 5, dmc, col),
                                                                         in1=self.xT[:, dmc, t0:t0 + tn], op0=ALU.mult, op1=ALU.add),
                                 [rpd, self.rmod, self.rx[dmc][tb]], [self.rx[dmc][tb]])

                    ntb = 4 if l == self.dbg.get("ctx_skip_layer", DEPTH - 1) else 5
                    stage_a(0)
                    for tb in range(ntb):
                        if tb + 1 < ntb:
                            stage_a(tb + 1)
                        stage_b(tb)
                S.barrier()
            with ExitStack() as es4:
                sq = [self.sb(es4, "lnsq%d" % i, [128, 512]) for i in range(2)]
                rsq = [Res() for _ in range(2)]
                rstd = self.sb(es4, "lnrstd", [128, 512])
                rrstd = Res()
                for tb in range(4 if l == self.dbg.get("ctx_skip_layer", DEPTH - 1) else 5):
                    t0 = tb * 512
                    tn = min(512, T - t0)
                    self.layer_norm((sq, rsq, rstd, rrstd), l, t0, tn, "ln2_g", "ln2_b")


def make_in_maps(inputs, ncores=8):
    f = lambda a: np.ascontiguousarray(np.asarray(a, np.float32))
    x, c, ctx, c_ctx = inputs["x"], inputs["c"], inputs["ctx"], inputs["c_ctx"]
    pvec = np.zeros((DEPTH, 128, NPV), np.float32)
    for l in range(DEPTH):
        for name in ("b_ada", "ln1_g", "ln1_b", "ln2_g", "ln2_b"):
            o, n = PV[name]
            pvec[l, :, o:o + n] = _fm(inputs[name][l])
        for name, src in (("a_qg", "a_q_gain"), ("a_kg", "a_k_gain"), ("c_og", "c_out_gain"), ("d_og", "d_out_gain")):
            pvec[l, :, PV[name][0]] = np.tile(np.asarray(inputs[src][l], np.float32), 2)
        cw = np.asarray(inputs["c_conv"][l], np.float32)
        for ch in range(6):
            for tap in range(3):
                pvec[l, :, PV["c_conv"][0] + ch * 3 + tap] = cw[tap, ch * 128:(ch + 1) * 128]
        pvec[l, 0:8, PV["c_dtb"][0]] = np.asarray(inputs["c_dt_bias"][l], np.float32).reshape(8)
        pvec[l, 0:8, PV["c_alog"][0]] = np.asarray(inputs["c_a_log"][l], np.float32).reshape(8)
        pvec[l, :, PV["d_gb"][0]:PV["d_gb"][0] + 2] = np.asarray(inputs["d_gate_b"][l], np.float32).T
    w_rt = np.concatenate([inputs["w_router_g"], np.transpose(inputs["w_router_e"], (0, 2, 1, 3)).reshape(DEPTH, D, 32)], axis=2)
    b_rt = np.concatenate([inputs["b_router_g"], inputs["b_router_e"].reshape(DEPTH, 32)], axis=1)
    shared = {"mconsts": MCONSTS, "cmask": CMASK, "d_gw": f(inputs["d_gate_w"]), "rope": ROPE, "nab": _na_bias(np.asarray(inputs["b_rpb"], np.float32)), "consts": CONSTS, "pvec": pvec, "w_ada": f(inputs["w_ada"]), "w_in": f(inputs["w_in"]), "w_branch": f(inputs["w_branch"]),
              "w_out": f(inputs["w_out"]), "w_rt": f(w_rt), "b_rt": f(b_rt), "w_up": f(inputs["w_up"]), "w_gate": f(inputs["w_gate"]),
              "w_down": f(inputs["w_down"])}
    maps = []
    for i in range(ncores):
        bs = slice(2 * i, 2 * i + 2)
        m = dict(shared)
        m["xT_in"] = f(np.transpose(x[bs], (0, 2, 1)))
        m["ctxT_in"] = f(np.transpose(ctx[bs], (0, 2, 1)))
        m["cc"] = f(np.stack([c[2 * i], c[2 * i + 1], c_ctx], axis=1))
        maps.append(m)
    return maps


def kernel(**inputs):
    nc = bass.Bass("TRN2", target_bir_lowering=False)
    Kern(nc).build()
    maps = make_in_maps(inputs)
    res = run_bass_kernel_spmd(nc, maps, core_ids=list(range(8)))
    out = np.zeros((16, TL, D), np.float32)
    for i in range(8):
        o = res.results[i]["outT"]
        out[2 * i:2 * i + 2] = np.transpose(o, (0, 2, 1))
    return out
```
